# Optimizing a Trainium2 kernel written in Bass

```python
import math
import jax, jax.numpy as jnp
from jax import lax
import numpy as np

D_MODEL = 1024
BATCH = 8
SEQ = 2048
DEPTH = 4

CTX_LEN = 256
GRID_W = 64

D_GMLP = D_MODEL // 4
D_NA = D_MODEL // 2
D_HYENA = D_MODEL // 4
D_MIX = D_GMLP + D_NA + D_HYENA
D_IN = 2 * D_GMLP + 3 * D_NA + 3 * D_HYENA
KV_START = 2 * D_GMLP + D_NA
HY_START = 2 * D_GMLP + 3 * D_NA

GMLP_GROUPS = 4
GMLP_GROUP_DIM = D_GMLP // GMLP_GROUPS
GMLP_CHUNK = 128

NA_HEAD_DIM = 64
NA_HEADS = D_NA // NA_HEAD_DIM
NA_SCALE = NA_HEAD_DIM ** -0.5
NA_WIN_ROWS = 8
NA_WIN_COLS = 16
NA_COL_BLOCK = 16
NA_ROW_BLOCK_CHOICES = (8, 4, 2, 1)

HYENA_ORDER = 2
HYENA_SHORT = 3
HYENA_POS_BANDS = 16
HYENA_EMB = 1 + 2 * HYENA_POS_BANDS
HYENA_FILTER_HIDDEN = 64
HYENA_DECAY_TARGET = 1e-2
HYENA_FAST_DECAY = 0.3
HYENA_SLOW_DECAY = 1.5

MOE_GROUPS = 4
MOE_EXPERTS_PER_GROUP = 8
MOE_TOP_K = 2
MOE_HIDDEN = 256

N_MOD = 6
RMS_EPS = 1e-6
LN_EPS = 1e-5

kernel_name = "hybrid_gmlp_natten_hyena_hmoe_dit"


def rms_norm(x, gain):
    xf = x.astype(jnp.float32)
    xf = xf * lax.rsqrt(jnp.mean(jnp.square(xf), axis=-1, keepdims=True) + RMS_EPS)
    return (xf * gain.astype(jnp.float32)).astype(x.dtype)


def layer_norm(x, gain):
    xf = x.astype(jnp.float32)
    xf = xf - jnp.mean(xf, axis=-1, keepdims=True)
    xf = xf * lax.rsqrt(jnp.mean(jnp.square(xf), axis=-1, keepdims=True) + LN_EPS)
    return (xf * gain.astype(jnp.float32)).astype(x.dtype)


def adaln(x, gain, shift, scale):
    return rms_norm(x, gain) * (1 + scale) + shift


def to_heads(t):
    return t.reshape(*t.shape[:-1], NA_HEADS, NA_HEAD_DIM)


def qk_norm_heads(t, gain):
    return rms_norm(to_heads(t), gain)


def gmlp_mix(a_u, a_v, v_gain, w_s, b_s):
    bsz, length, _ = a_u.shape
    u = jax.nn.gelu(a_u)
    v = jax.nn.gelu(a_v).reshape(bsz, length // GMLP_CHUNK, GMLP_CHUNK, GMLP_GROUPS, GMLP_GROUP_DIM)
    v = layer_norm(v, v_gain.reshape(GMLP_GROUPS, GMLP_GROUP_DIM))
    s = jnp.einsum('gij,bnjgd->bnigd', w_s, v) + b_s.T[:, :, None]
    return u * s.reshape(bsz, length, D_GMLP)


def na_geometry(rows):
    kr = min(NA_WIN_ROWS, rows)
    kc = NA_WIN_COLS
    qr = next(b for b in NA_ROW_BLOCK_CHOICES if rows % b == 0)
    qc = NA_COL_BLOCK
    br = min(kr + qr, rows)
    bc = min(kc + qc, GRID_W)
    n_rb, n_cb = rows // qr, GRID_W // qc
    row_idx = np.clip(np.arange(n_rb) * qr - kr // 2, 0, rows - br)[:, None] + np.arange(br)
    col_idx = np.clip(np.arange(n_cb) * qc - kc // 2, 0, GRID_W - bc)[:, None] + np.arange(bc)
    q_row = np.arange(n_rb)[:, None] * qr + np.arange(qr)
    q_col = np.arange(n_cb)[:, None] * qc + np.arange(qc)
    win_r = np.clip(q_row - kr // 2, 0, rows - kr)
    win_c = np.clip(q_col - kc // 2, 0, GRID_W - kc)
    qrow_b = q_row[:, None, :, None, None, None]
    wr_b = win_r[:, None, :, None, None, None]
    krow_b = row_idx[:, None, None, None, :, None]
    qcol_b = q_col[None, :, None, :, None, None]
    wc_b = win_c[None, :, None, :, None, None]
    kcol_b = col_idx[None, :, None, None, None, :]
    full = (n_rb, n_cb, qr, qc, br, bc)
    mask = np.broadcast_to((krow_b >= wr_b) & (krow_b < wr_b + kr) & (kcol_b >= wc_b) & (kcol_b < wc_b + kc), full)
    dr = np.broadcast_to(np.clip(krow_b - qrow_b + NA_WIN_ROWS - 1, 0, 2 * NA_WIN_ROWS - 2), full)
    dc = np.broadcast_to(np.clip(kcol_b - qcol_b + NA_WIN_COLS - 1, 0, 2 * NA_WIN_COLS - 2), full)
    return qr, br, bc, n_rb, n_cb, row_idx, col_idx, mask, dr, dc


def neighbourhood_attention(q, k, v, k_ctx, v_ctx, rpb):
    bsz, seq, n_heads, dh = q.shape
    rows = seq // GRID_W
    qr, br, bc, n_rb, n_cb, row_idx, col_idx, mask, dr, dc = na_geometry(rows)
    qg = q.reshape(bsz, n_rb, qr, n_cb, NA_COL_BLOCK, n_heads, dh)
    kg = k.reshape(bsz, rows, GRID_W, n_heads, dh)[:, :, col_idx][:, row_idx]
    vg = v.reshape(bsz, rows, GRID_W, n_heads, dh)[:, :, col_idx][:, row_idx]
    s_win = jnp.einsum('bipjqhd,biajchd->bijhpqac', qg, kg).astype(jnp.float32)
    bias = jnp.moveaxis(rpb[:, dr, dc], 0, 2).astype(jnp.float32)
    s_win = jnp.where(mask[:, :, None], s_win + bias, -jnp.inf)
    s_win = s_win.reshape(*s_win.shape[:6], br * bc)
    s_ctx = jnp.einsum('bipjqhd,bkhd->bijhpqk', qg, k_ctx).astype(jnp.float32)
    p = jax.nn.softmax(jnp.concatenate([s_win, s_ctx], axis=-1), axis=-1).astype(v.dtype)
    p_win = p[..., :br * bc].reshape(*p.shape[:6], br, bc)
    p_ctx = p[..., br * bc:]
    o = jnp.einsum('bijhpqac,biajchd->bipjqhd', p_win, vg) + jnp.einsum('bijhpqk,bkhd->bipjqhd', p_ctx, v_ctx)
    return o.reshape(bsz, seq, n_heads * dh)


def context_attention(q, k, v):
    bsz, length = q.shape[:2]
    s = jnp.einsum('bqhd,bkhd->bhqk', q, k).astype(jnp.float32)
    p = jax.nn.softmax(s, axis=-1).astype(v.dtype)
    return jnp.einsum('bhqk,bkhd->bqhd', p, v).reshape(bsz, length, NA_HEADS * NA_HEAD_DIM)


def hyena_positional_features(length):
    t = jnp.linspace(0.0, 1.0, length, dtype=jnp.float32)[:, None]
    w = 2.0 * math.pi * jnp.arange(length, dtype=jnp.float32)[:, None] / length
    f = jnp.linspace(1e-4, HYENA_POS_BANDS - 1, HYENA_POS_BANDS, dtype=jnp.float32)[None, :]
    return jnp.concatenate([t, jnp.cos(f * w), -jnp.sin(f * w)], axis=-1)


def hyena_filters(length, w1, b1, w2, b2, w3, freq):
    f32 = jnp.float32
    z = hyena_positional_features(length)
    hdn = jnp.sin(freq[0].astype(f32) * (z @ w1.astype(f32) + b1.astype(f32)))
    hdn = jnp.sin(freq[1].astype(f32) * (hdn @ w2.astype(f32) + b2.astype(f32)))
    h = (hdn @ w3.astype(f32)).reshape(length, HYENA_ORDER, 2, D_HYENA)
    t = jnp.linspace(0.0, 1.0, length, dtype=f32)[:, None]
    min_decay = math.log(HYENA_DECAY_TARGET) / HYENA_SLOW_DECAY
    max_decay = math.log(HYENA_DECAY_TARGET) / HYENA_FAST_DECAY
    deltas = jnp.abs(jnp.linspace(min_decay, max_decay, D_HYENA, dtype=f32))[None, :]
    h = h * jnp.exp(-t * deltas)[:, None, None, :]
    return h / jnp.sum(jnp.abs(h), axis=(0, 2), keepdims=True)


def bidirectional_long_conv(u, h_fwd, h_bwd):
    length = u.shape[1]
    k = jnp.concatenate([h_fwd, jnp.zeros_like(h_fwd[:1]), h_bwd[:0:-1]], axis=0)
    kf = jnp.fft.rfft(k, n=2 * length, axis=0)
    uf = jnp.fft.rfft(u, n=2 * length, axis=1)
    return jnp.fft.irfft(uf * kf[None], n=2 * length, axis=1)[:, :length]


def hyena_mix(a, short_w, short_b, h, d_bias):
    n_ch = a.shape[-1]
    pad = HYENA_SHORT // 2
    a = lax.conv_general_dilated(a, short_w[:, None, :].astype(a.dtype), window_strides=(1,),
                                 padding=((pad, pad),), dimension_numbers=('NWC', 'WIO', 'NWC'),
                                 feature_group_count=n_ch) + short_b
    v, x1, x2 = jnp.split(a, 3, axis=-1)
    z = v.astype(jnp.float32)
    for n, gate in enumerate((x1, x2)):
        z = gate.astype(jnp.float32) * (bidirectional_long_conv(z, h[:, n, 0], h[:, n, 1])
                                        + d_bias[n].astype(jnp.float32) * z)
    return z.astype(a.dtype)


def hier_moe(h, w_rg, b_rg, w_re, b_re, w_gate, w_up, w_down):
    bsz, length, d = h.shape
    t = h.reshape(bsz * length, d)
    g_prob = jax.nn.softmax((t @ w_rg + b_rg).astype(jnp.float32), axis=-1)
    g_p, g_idx = lax.top_k(g_prob, 1)
    g_onehot = jax.nn.one_hot(g_idx[:, 0], MOE_GROUPS, dtype=jnp.float32)
    e_logits = (t @ w_re + b_re).astype(jnp.float32).reshape(-1, MOE_GROUPS, MOE_EXPERTS_PER_GROUP)
    e_prob = jax.nn.softmax(jnp.einsum('tge,tg->te', e_logits, g_onehot), axis=-1)
    e_p, e_idx = lax.top_k(e_prob, MOE_TOP_K)
    e_p = e_p / jnp.sum(e_p, axis=-1, keepdims=True)
    e_w = jnp.einsum('tk,tke->te', e_p, jax.nn.one_hot(e_idx, MOE_EXPERTS_PER_GROUP, dtype=jnp.float32))
    combine = (g_p[:, :, None] * g_onehot[:, :, None] * e_w[:, None, :]).astype(h.dtype)
    out = jnp.zeros_like(t)
    for g in range(MOE_GROUPS):
        act = jax.nn.silu(jnp.einsum('td,edf->tef', t, w_gate[g])) * jnp.einsum('td,edf->tef', t, w_up[g])
        out = out + jnp.einsum('tef,efd->td', act * combine[:, g, :, None], w_down[g])
    return out.reshape(bsz, length, d)


def setup_inputs(seed: int = 0) -> dict:
    key = jax.random.key(seed)
    ks = jax.random.split(key, 32)
    f32 = jnp.float32

    def nrm(k, shape, scale):
        return scale * jax.random.normal(k, shape, f32)

    D, L_ = D_MODEL, DEPTH
    G, E, F = MOE_GROUPS, MOE_EXPERTS_PER_GROUP, MOE_HIDDEN
    return {
        'x': nrm(ks[0], (BATCH, SEQ, D), 1.0),
        'c': nrm(ks[1], (BATCH, D), 1.0),
        'ctx': nrm(ks[2], (BATCH, CTX_LEN, D), 1.0),
        'c_ctx': nrm(ks[3], (D,), 1.0),
        'w_ada': nrm(ks[4], (L_, D, N_MOD * D), 0.5 * D ** -0.5),
        'b_ada': nrm(ks[5], (L_, N_MOD * D), 0.02),
        'g_mix': 1.0 + nrm(ks[6], (L_, D), 0.05),
        'g_ffn': 1.0 + nrm(ks[7], (L_, D), 0.05),
        'w_in': nrm(ks[8], (L_, D, D_IN), D ** -0.5),
        'w_out': nrm(ks[9], (L_, D_MIX, D), D_MIX ** -0.5),
        'gmlp_v_gain': 1.0 + nrm(ks[10], (L_, D_GMLP), 0.05),
        'gmlp_ws': nrm(ks[11], (L_, GMLP_GROUPS, GMLP_CHUNK, GMLP_CHUNK), GMLP_CHUNK ** -0.5),
        'gmlp_bs': 1.0 + nrm(ks[12], (L_, GMLP_GROUPS, GMLP_CHUNK), 0.05),
        'na_q_gain': 1.0 + nrm(ks[13], (L_, NA_HEAD_DIM), 0.05),
        'na_k_gain': 1.0 + nrm(ks[14], (L_, NA_HEAD_DIM), 0.05),
        'na_rpb': nrm(ks[15], (L_, NA_HEADS, 2 * NA_WIN_ROWS - 1, 2 * NA_WIN_COLS - 1), 0.2),
        'hy_short_w': nrm(ks[16], (L_, HYENA_SHORT, 3 * D_HYENA), HYENA_SHORT ** -0.5),
        'hy_short_b': nrm(ks[17], (L_, 3 * D_HYENA), 0.02),
        'hy_w1': nrm(ks[18], (L_, HYENA_EMB, HYENA_FILTER_HIDDEN), HYENA_EMB ** -0.5),
        'hy_b1': nrm(ks[19], (L_, HYENA_FILTER_HIDDEN), 0.02),
        'hy_w2': nrm(ks[20], (L_, HYENA_FILTER_HIDDEN, HYENA_FILTER_HIDDEN), HYENA_FILTER_HIDDEN ** -0.5),
        'hy_b2': nrm(ks[21], (L_, HYENA_FILTER_HIDDEN), 0.02),
        'hy_w3': nrm(ks[22], (L_, HYENA_FILTER_HIDDEN, HYENA_ORDER * 2 * D_HYENA), HYENA_FILTER_HIDDEN ** -0.5),
        'hy_freq': 1.0 + nrm(ks[23], (L_, 2, HYENA_FILTER_HIDDEN), 0.05),
        'hy_bias': nrm(ks[24], (L_, HYENA_ORDER, D_HYENA), 0.1),
        'moe_w_rg': nrm(ks[25], (L_, D, G), D ** -0.5),
        'moe_b_rg': nrm(ks[26], (L_, G), 0.01),
        'moe_w_re': nrm(ks[27], (L_, D, G * E), D ** -0.5),
        'moe_b_re': nrm(ks[28], (L_, G * E), 0.01),
        'moe_w_gate': nrm(ks[29], (L_, G, E, D, F), D ** -0.5),
        'moe_w_up': nrm(ks[30], (L_, G, E, D, F), D ** -0.5),
        'moe_w_down': nrm(ks[31], (L_, G, E, F, D), F ** -0.5),
    }


def reference(x, c, ctx, c_ctx, w_ada, b_ada, g_mix, g_ffn, w_in, w_out,
              gmlp_v_gain, gmlp_ws, gmlp_bs, na_q_gain, na_k_gain, na_rpb,
              hy_short_w, hy_short_b, hy_w1, hy_b1, hy_w2, hy_b2, hy_w3, hy_freq, hy_bias,
              moe_w_rg, moe_b_rg, moe_w_re, moe_b_re, moe_w_gate, moe_w_up, moe_w_down):
    seq = x.shape[1]
    silu_c = jax.nn.silu(c)[:, None, :]
    silu_cc = jax.nn.silu(c_ctx)
    xc = ctx
    for l in range(DEPTH):
        last = l == DEPTH - 1
        m_lat = jnp.split(silu_c @ w_ada[l] + b_ada[l], N_MOD, axis=-1)
        m_ctx = jnp.split(silu_cc @ w_ada[l] + b_ada[l], N_MOD, axis=-1)
        filt = (hy_w1[l], hy_b1[l], hy_w2[l], hy_b2[l], hy_w3[l], hy_freq[l])
        moe_p = (moe_w_rg[l], moe_b_rg[l], moe_w_re[l], moe_b_re[l], moe_w_gate[l], moe_w_up[l], moe_w_down[l])

        hl = adaln(x, g_mix[l], m_lat[0], m_lat[1])
        hc = adaln(xc, g_mix[l], m_ctx[0], m_ctx[1])
        pl = hl @ w_in[l]
        if last:
            pc_kv = hc @ w_in[l][:, KV_START:HY_START]
        else:
            pc = hc @ w_in[l]
            pc_kv = pc[..., KV_START:HY_START]
        k_ctx = qk_norm_heads(pc_kv[..., :D_NA], na_k_gain[l])
        v_ctx = to_heads(pc_kv[..., D_NA:])

        q = qk_norm_heads(pl[..., 2 * D_GMLP:KV_START], na_q_gain[l]) * NA_SCALE
        k = qk_norm_heads(pl[..., KV_START:KV_START + D_NA], na_k_gain[l])
        v = to_heads(pl[..., KV_START + D_NA:HY_START])
        mix_lat = jnp.concatenate([
            gmlp_mix(pl[..., :D_GMLP], pl[..., D_GMLP:2 * D_GMLP], gmlp_v_gain[l], gmlp_ws[l], gmlp_bs[l]),
            neighbourhood_attention(q, k, v, k_ctx, v_ctx, na_rpb[l]),
            hyena_mix(pl[..., HY_START:], hy_short_w[l], hy_short_b[l], hyena_filters(seq, *filt), hy_bias[l]),
        ], axis=-1)
        x = x + m_lat[2] * (mix_lat @ w_out[l])

        if not last:
            q_ctx = qk_norm_heads(pc[..., 2 * D_GMLP:KV_START], na_q_gain[l]) * NA_SCALE
            mix_ctx = jnp.concatenate([
                gmlp_mix(pc[..., :D_GMLP], pc[..., D_GMLP:2 * D_GMLP], gmlp_v_gain[l], gmlp_ws[l], gmlp_bs[l]),
                context_attention(q_ctx, k_ctx, v_ctx),
                hyena_mix(pc[..., HY_START:], hy_short_w[l], hy_short_b[l],
                          hyena_filters(xc.shape[1], *filt), hy_bias[l]),
            ], axis=-1)
            xc = xc + m_ctx[2] * (mix_ctx @ w_out[l])

        x = x + m_lat[5] * hier_moe(adaln(x, g_ffn[l], m_lat[3], m_lat[4]), *moe_p)
        if not last:
            xc = xc + m_ctx[5] * hier_moe(adaln(xc, g_ffn[l], m_ctx[3], m_ctx[4]), *moe_p)
    return x
```

```python
import math
import os
import numpy as np
import ml_dtypes
import concourse.bass as bass
import concourse.mybir as mybir
from concourse.bass_utils import run_bass_kernel_spmd
from contextlib import ExitStack, contextmanager

F32 = mybir.dt.float32
BF16 = mybir.dt.bfloat16
ALU = mybir.AluOpType
AF = mybir.ActivationFunctionType
AX = mybir.AxisListType

ENGS = ('pe', 'act', 'dve', 'pool', 'sp')
WRITE_KW = ('out', 'accum_out', 'ap')


class Buf:
    def __init__(self, mk, t, name, kind):
        self.mk, self.t, self.name, self.kind = mk, t, name, kind
        self.last_w = None
        self.readers = []
        self.sems = {}
        self.lastw_kind = {}

    view = None

    def __getitem__(self, idx):
        return self.v()[idx]

    def v(self):
        if self.view is not None:
            return V(self, self.t[0:self.view[0], 0:self.view[1]])
        return V(self, self.t[:])

    def custom(self, offset, ap):
        return V(self, bass.AP(self.t, offset, ap))


class V:
    def __init__(self, buf, ap):
        self.buf, self.ap = buf, ap

    def __getitem__(self, idx):
        return V(self.buf, self.ap[idx])

    def rearrange(self, pat, **kw):
        return V(self.buf, self.ap.rearrange(pat, **kw))

    def bc(self, shape):
        return V(self.buf, self.ap.to_broadcast(list(shape)))

    def unsq(self, axis):
        return V(self.buf, self.ap.unsqueeze(axis))

    def pbc(self, n):
        return V(self.buf, self.ap.partition_broadcast(n))


class Ins:
    __slots__ = ('eng', 'fn', 'deps', 'is_dma', 'sem', 'semval', 'signals', 'sigidx')

    def __init__(self, eng, fn, is_dma=False):
        self.eng, self.fn, self.is_dma = eng, fn, is_dma
        self.deps = []
        self.sem = None
        self.semval = 0
        self.signals = False
        self.sigidx = None


class SemSlot:
    def __init__(self, sem, kind):
        self.sem = sem
        self.kind = kind
        self.cnt = 0
        self.last = None


class MK:
    def __init__(self, nc, n_pool_sems=70):
        self.nc = nc
        self.es = ExitStack()
        self.lists = {e: [] for e in ENGS}
        self.esem = {}
        n_hw = (n_pool_sems * 3) // 5
        self.free_slots = {
            'hw': [SemSlot(self.es.enter_context(nc.semaphore(f"dqh{i}")), 'hw') for i in range(n_hw)],
            'sw': [SemSlot(self.es.enter_context(nc.semaphore(f"dqs{i}")), 'sw') for i in range(n_pool_sems - n_hw)],
        }
        self.all_slots = self.free_slots['hw'] + self.free_slots['sw']
        self.stack = [self.es]
        self.phase_slots = [[]]
        self.uid = 0

    def sb(self, name, shape, dtype=F32):
        self.uid += 1
        t = self.stack[-1].enter_context(self.nc.sbuf_tensor(f"{name}_{self.uid}", list(shape), dtype))
        return Buf(self, t, name, 'sb')

    def ps(self, name, shape, dtype=F32):
        self.uid += 1
        full = 512 if dtype == F32 else 1024
        t = self.stack[-1].enter_context(self.nc.psum_tensor(f"{name}_{self.uid}", [128, full], dtype))
        b = Buf(self, t, name, 'ps')
        b.view = (shape[0], shape[1])
        return b

    def dram(self, name, shape, dtype=F32, kind="Internal"):
        t = self.nc.dram_tensor(name, list(shape), dtype, kind=kind)
        return Buf(self, t, name, 'dram')

    def _slot_for(self, buf, q):
        kind = 'sw' if q == 'pool' else 'hw'
        if kind not in buf.sems:
            slot = self.free_slots[kind].pop()
            buf.sems[kind] = slot
            if buf.kind != 'dram':
                self.phase_slots[-1].append(slot)
        return buf.sems[kind]

    @contextmanager
    def phase(self):
        st = ExitStack()
        self.stack.append(st)
        self.phase_slots.append([])
        try:
            yield
        finally:
            self.barrier()
            self.stack.pop()
            for s in self.phase_slots.pop():
                self.free_slots[s.kind].append(s)
            st.close()

    def _track(self, ins, reads, writes):
        deps = []
        for b in reads:
            if b.last_w is not None:
                deps.append(('raw', b.last_w))
            for d in b.lastw_kind.values():
                deps.append(('raw', d))
        for b in writes:
            dram_store = False
            if b.last_w is not None and not dram_store:
                deps.append(('waw', b.last_w))
                for d in b.lastw_kind.values():
                    deps.append(('waw', d))
            for r in b.readers:
                deps.append(('war', r))
        seen = set()
        for kind, d in deps:
            if d is ins or id(d) in seen:
                continue
            if not d.is_dma and not ins.is_dma and d.eng == ins.eng:
                if kind != 'raw' or ins.eng == 'pe':
                    continue
            seen.add(id(d))
            ins.deps.append(d)
            if not d.is_dma:
                d.signals = True
        for b in reads:
            b.readers.append(ins)
        for b in writes:
            b.last_w = ins
            b.readers = []

    def I(self, eng, meth, **kw):
        rb, wb, kk = [], [], {}
        for k, v in kw.items():
            if isinstance(v, V):
                (wb if k in WRITE_KW else rb).append(v.buf)
                kk[k] = v.ap
            else:
                kk[k] = v
        ins = Ins(eng, lambda e: getattr(e, meth)(**kk))
        self._track(ins, rb, wb)
        self.lists[eng].append(ins)
        return ins

    def dma(self, q, out, in_, force=False, **kw):
        if q == 'sp' and out.buf.kind == 'dram' and not force:
            q = 'pool'
        ins = Ins(q, lambda e: e.dma_start(out=out.ap, in_=in_.ap, **kw), is_dma=True)
        slot = self._slot_for(out.buf, q)
        slot.cnt += 16
        ins.sem, ins.semval = slot.sem, slot.cnt
        slot.last = ins
        out.buf.lastw_kind[slot.kind] = ins
        self._track(ins, [in_.buf], [out.buf])
        self.lists[q].append(ins)
        return ins

    def barrier(self):
        last = {}
        for e in ENGS:
            for ins in reversed(self.lists[e]):
                if not ins.is_dma and ins.fn is not None:
                    last[e] = ins
                    ins.signals = True
                    break
        dmas = [s.last for s in self.all_slots if s.last is not None]
        for e in ENGS:
            w = Ins(e, None)
            w.deps = [d for ee, d in last.items() if ee != e] + dmas
            self.lists[e].append(w)

    def wait_bufs(self, eng, bufs):
        w = Ins(eng, None)
        self._track(w, list(bufs), [])
        self.lists[eng].append(w)

    def build(self):
        nc = self.nc
        for e in ENGS:
            self.esem[e] = self.es.enter_context(nc.semaphore("eng_" + e))
            n = 0
            for ins in self.lists[e]:
                if ins.signals and not ins.is_dma and ins.fn is not None:
                    n += 1
                    ins.sigidx = n
        lists, esem = self.lists, self.esem
        stats = {}

        def replay(ename, e):
            waited = {}
            nw = 0
            for ins in lists[ename]:
                for d in ins.deps:
                    if d.is_dma:
                        key, val, sem = ('d', id(d.sem)), d.semval, d.sem
                    else:
                        key, val, sem = ('e', d.eng), d.sigidx, esem[d.eng]
                    if waited.get(key, 0) >= val:
                        continue
                    waited[key] = val
                    e.wait_ge(sem, val)
                    nw += 1
                if ins.fn is None:
                    continue
                r = ins.fn(e)
                if ins.is_dma:
                    r.then_inc(ins.sem, 16)
                elif ins.signals:
                    r.then_inc(esem[ename], 1)
            stats[ename] = (len(lists[ename]), nw)

        with nc.Block() as block:
            @block.tensor
            def _(e):
                replay('pe', e)

            @block.scalar
            def _(e):
                replay('act', e)

            @block.vector
            def _(e):
                replay('dve', e)

            @block.gpsimd
            def _(e):
                replay('pool', e)

            @block.sync
            def _(e):
                replay('sp', e)
        self.stats = stats
        self.es.close()
        return nc


D = 1024
S = 2048
CTX = 256
NTOK = S + CTX
DEPTH = 4
D_IN = 2816
GRID_W = 64
NH = 8
DH = 64
NEXP = 32
FH = 256
RMS_EPS = 1e-6
LN_EPS = 1e-5
TWO_PI = 2.0 * math.pi
NEG_MASK = -30000.0


def _na_tables():
    rows = S // GRID_W
    kr, kc, qr, qc, br, bc = 8, 16, 8, 16, 16, 32
    n_rb, n_cb = rows // qr, GRID_W // qc
    r0 = np.clip(np.arange(n_rb) * qr - kr // 2, 0, rows - br)
    c0 = np.clip(np.arange(n_cb) * qc - kc // 2, 0, GRID_W - bc)
    info = []
    cch, rr, cc = np.meshgrid(np.arange(4), np.arange(4), np.arange(32), indexing='ij')
    krow_l = (4 * cch + rr).reshape(4, 128)
    kcol_l = cc.reshape(4, 128)
    qa, qb = np.meshgrid(np.arange(8), np.arange(16), indexing='ij')
    qa, qb = qa.reshape(128), qb.reshape(128)
    for i in range(n_rb):
        for j in range(n_cb):
            krow = r0[i] + krow_l[:, :, None]
            kcol = c0[j] + kcol_l[:, :, None]
            qrow = (i * qr + qa)[None, None, :]
            qcol = (j * qc + qb)[None, None, :]
            wr = np.clip(qrow - kr // 2, 0, rows - kr)
            wc = np.clip(qcol - kc // 2, 0, GRID_W - kc)
            mask = (krow >= wr) & (krow < wr + kr) & (kcol >= wc) & (kcol < wc + kc)
            dr = np.clip(krow - qrow + 7, 0, 14) + 0 * kcol
            dc = np.clip(kcol - qcol + 15, 0, 30) + 0 * krow
            info.append((int(r0[i]), int(c0[j]), i * qr, j * qc, np.broadcast_to(mask, dr.shape), dr, dc))
    return info


_CONST_CACHE = {}


def _consts():
    if _CONST_CACHE:
        return _CONST_CACHE
    c = {}
    c['ident'] = np.eye(128, dtype=np.float32)
    for L in (S, CTX):
        t = np.linspace(0.0, 1.0, L, dtype=np.float32)[:, None]
        w = (2.0 * math.pi * np.arange(L, dtype=np.float32)[:, None] / L).astype(np.float32)
        f = np.linspace(1e-4, 15, 16, dtype=np.float32)[None, :]
        z = np.concatenate([t, np.cos(f * w), -np.sin(f * w)], axis=-1).astype(np.float32)
        c[f'posT{L}'] = np.ascontiguousarray(z.T)
        min_decay = math.log(1e-2) / 1.5
        max_decay = math.log(1e-2) / 0.3
        deltas = np.abs(np.linspace(min_decay, max_decay, 256, dtype=np.float32))[None, :]
        dec = np.exp(-t * deltas).astype(np.float32)
        nT = L // 128
        c[f'decay{L}'] = np.ascontiguousarray(dec.reshape(nT, 128, 256).transpose(1, 0, 2))
        tt = np.arange(L, dtype=np.float64)[:, None]
        ff = np.arange(L, dtype=np.float64)[None, :] + 0.5
        ang = 2.0 * math.pi * tt * ff / (2 * L)
        C = np.cos(ang)
        Sn = np.sin(ang)
        def fwd(M):
            return np.ascontiguousarray(M.reshape(nT, 128, nT, 128).transpose(2, 1, 0, 3)).astype(ml_dtypes.bfloat16)
        def inv(M):
            return np.ascontiguousarray(M.reshape(nT, 128, nT, 128).transpose(0, 3, 2, 1)).astype(ml_dtypes.bfloat16)
        c[f'Cf{L}'] = fwd(C)
        c[f'Sf{L}'] = fwd(Sn)
        c[f'Ci{L}'] = inv(C)
        c[f'Si{L}'] = inv(Sn)
    _CONST_CACHE.update(c)
    return c


def routing_batched(mk, lg, T, cmb, sfx=""):
    I = mk.I
    def t(nm, shape):
        return mk.sb(f"rt_{nm}{sfx}", shape, F32).v()
    lg4 = lg[:, :, 0:4]
    gmax, se, gp = t("gmax", [128, T]), t("se", [128, T]), t("gp", [128, T])
    oh, sh = t("oh", [128, T, 4]), t("sh", [128, T, 4])
    selg, es, es2 = t("selg", [128, T, 32]), t("es", [128, T, 8]), t("es2", [128, T, 8])
    k1, k2 = t("k1", [128, T, 8]), t("k2", [128, T, 8])
    m1, m2, dd, ee, p1, p2 = (t(n, [128, T]) for n in ("m1", "m2", "dd", "ee", "p1", "p2"))
    ew = t("ew", [128, T, 8])
    I('dve', 'reduce_max', out=gmax, in_=lg4, axis=AX.X)
    I('dve', 'tensor_tensor', out=oh, in0=lg4, in1=gmax.unsq(2).bc([128, T, 4]), op=ALU.is_equal)
    I('dve', 'tensor_tensor', out=sh, in0=lg4, in1=gmax.unsq(2).bc([128, T, 4]), op=ALU.subtract)
    I('act', 'activation', out=sh, in_=sh, func=AF.Exp)
    I('dve', 'reduce_sum', out=se, in_=sh, axis=AX.X)
    I('dve', 'reciprocal', out=gp, in_=se)
    el = lg[:, :, 4:36].rearrange("p t (g e) -> p t g e", e=8)
    I('dve', 'tensor_tensor', out=selg.rearrange("p t (g e) -> p t g e", e=8), in0=el, in1=oh.unsq(3).bc([128, T, 4, 8]), op=ALU.mult)
    I('dve', 'reduce_sum', out=es, in_=selg.rearrange("p t (g e) -> p t e g", e=8), axis=AX.X)
    I('dve', 'reduce_max', out=m1, in_=es, axis=AX.X)
    I('dve', 'tensor_tensor', out=k1, in0=es, in1=m1.unsq(2).bc([128, T, 8]), op=ALU.is_equal)
    I('dve', 'scalar_tensor_tensor', out=es2, in0=k1, scalar=-1e30, in1=es, op0=ALU.mult, op1=ALU.add)
    I('dve', 'reduce_max', out=m2, in_=es2, axis=AX.X)
    I('dve', 'tensor_tensor', out=k2, in0=es2, in1=m2.unsq(2).bc([128, T, 8]), op=ALU.is_equal)
    I('dve', 'tensor_tensor', out=dd, in0=m2, in1=m1, op=ALU.subtract)
    I('act', 'activation', out=ee, in_=dd, func=AF.Exp)
    I('dve', 'tensor_scalar', out=p1, in0=ee, scalar1=1.0, scalar2=None, op0=ALU.add)
    I('dve', 'reciprocal', out=p1, in_=p1)
    I('dve', 'tensor_tensor', out=p2, in0=ee, in1=p1, op=ALU.mult)
    I('dve', 'tensor_tensor', out=p1, in0=p1, in1=gp, op=ALU.mult)
    I('dve', 'tensor_tensor', out=p2, in0=p2, in1=gp, op=ALU.mult)
    I('dve', 'tensor_tensor', out=k1, in0=k1, in1=p1.unsq(2).bc([128, T, 8]), op=ALU.mult)
    I('dve', 'tensor_tensor', out=k2, in0=k2, in1=p2.unsq(2).bc([128, T, 8]), op=ALU.mult)
    I('dve', 'tensor_tensor', out=ew, in0=k1, in1=k2, op=ALU.add)
    I('dve', 'tensor_tensor', out=cmb.rearrange("p t (g e) -> p t g e", e=8), in0=oh.unsq(3).bc([128, T, 4, 8]),
      in1=ew.unsq(2).bc([128, T, 4, 8]), op=ALU.mult)


def build_program(n_layers=DEPTH, stop=None, debug=False):
    nc = bass.Bass("TRN2", target_bir_lowering=False)
    mk = MK(nc)
    I = mk.I

    def ext(name, shape, dt=F32):
        return mk.dram(name, shape, dt, kind="ExternalInput")

    xin = ext("xin", [NTOK, D])
    cT = ext("cT", [128, 16])
    w_ada = ext("w_ada", [DEPTH, D, 6 * D])
    b_ada = ext("b_ada", [DEPTH, 6 * D])
    g_mix = ext("g_mix", [DEPTH, D])
    g_ffn = ext("g_ffn", [DEPTH, D])
    w_in = ext("w_in", [DEPTH, D, D_IN])
    w_out = ext("w_out", [DEPTH, D, D])
    gm_vg = ext("gmlp_v_gain", [DEPTH, 256])
    gm_ws = ext("gmlp_ws", [DEPTH, 4, 128, 128])
    gm_bsT = ext("gmlp_bsT", [DEPTH, 128, 4])
    na_qg = ext("na_q_gain", [DEPTH, 64])
    na_kg = ext("na_k_gain", [DEPTH, 64])
    na_bias = ext("na_bias", [DEPTH, 16, 128, NH * 4 * 128])
    hy_sw = ext("hy_short_w", [DEPTH, 3, 768])
    hy_sb = ext("hy_short_b", [DEPTH, 768])
    hy_w1 = ext("hy_w1", [DEPTH, 33, 64])
    hy_w2 = ext("hy_w2", [DEPTH, 64, 64])
    hy_w3 = ext("hy_w3", [DEPTH, 64, 1024])
    hy_fb = ext("hy_fb", [DEPTH, 64, 4])
    hy_bias = ext("hy_bias", [DEPTH, 512])
    moe_wr = ext("moe_wr", [DEPTH, D, 36])
    moe_br = ext("moe_br", [DEPTH, 36])
    moe_wg = ext("moe_w_gate", [DEPTH, NEXP, D, FH])
    moe_wu = ext("moe_w_up", [DEPTH, NEXP, D, FH])
    moe_wd = ext("moe_w_down", [DEPTH, NEXP, FH, D])
    identd = ext("ident", [128, 128])
    cst = {}
    for L in (S, CTX):
        nT = L // 128
        cst[f'posT{L}'] = ext(f"posT{L}", [33, L])
        cst[f'decay{L}'] = ext(f"decay{L}", [128, nT, 256])
        for nm in ('Cf', 'Sf', 'Ci', 'Si'):
            cst[f'{nm}{L}'] = ext(f"{nm}{L}", [nT, 128, nT * 128], BF16)

    out = mk.dram("out", [S, D], F32, kind="ExternalOutput")
    dk = "ExternalOutput" if debug else "Internal"
    xs = mk.dram("xs", [NTOK, D], F32, kind=dk)
    pl = mk.dram("pl", [NTOK, D_IN], F32, kind=dk)
    mix = mk.dram("mix", [NTOK, D], BF16, kind=dk)
    vd = mk.dram("vd", [NTOK, NH * 65], BF16, kind=dk)
    md = mk.dram("md", [2, 6 * D], F32, kind=dk)
    cbd = mk.dram("cbd", [NEXP, NTOK], BF16, kind=dk)
    if debug:
        dbgc = mk.dram("dbgc", [32, NTOK], BF16, kind=dk)
        dbgh = mk.dram("dbgh", [128, 8 * 256], BF16, kind=dk)

    identf = mk.sb("identf", [128, 128], F32)
    identb = mk.sb("identb", [128, 128], BF16)
    onesf = mk.sb("onesf", [128, 128], F32)
    lc = mk.sb("lc", [128, 16, 128], BF16)
    pmask = mk.sb("pmask", [128, 1], F32)
    negpi = mk.sb("negpi", [128, 1], F32)

    def rstd_from_ss(ss_v, scale, eps, n):
        I('dve', 'tensor_scalar', out=ss_v, in0=ss_v, scalar1=scale, scalar2=eps, op0=ALU.mult, op1=ALU.add)
        I('act', 'activation', out=ss_v, in_=ss_v, func=AF.Sqrt)
        I('dve', 'reciprocal', out=ss_v, in_=ss_v)

    with mk.phase():
        ct = mk.sb("ct", [128, 16], F32)
        sct = mk.sb("sct", [128, 16], F32)
        mk.dma('sp', identf.v(), identd.v())
        mk.dma('pool', identb.v(), identd.v())
        mk.dma('sp', ct.v(), cT.v())
        I('act', 'activation', out=sct.v(), in_=ct.v(), func=AF.Silu)
        I('dve', 'tensor_copy', out=lc.v(), in_=sct.v().unsq(2).bc([128, 16, 128]))
        I('dve', 'memset', ap=onesf.v(), constant=1.0)
        I('dve', 'memset', ap=negpi.v(), constant=-math.pi)
        I('dve', 'tensor_scalar', out=pmask.v(), in0=identf[:, 0:1], scalar1=-1.0, scalar2=1.0, op0=ALU.mult, op1=ALU.add)

    def done():
        mk.wait_bufs('sp', [out, xs, pl, mix, vd, md, cbd])
        mk.build()
        return nc, mk

    for l in range(n_layers):
        last = (l == DEPTH - 1)
        xsrc = xin if l == 0 else xs
        n_tiles = 16 if last else 18

        with mk.phase():
            wa = [mk.sb(f"wa{i}", [128, 8, 512], BF16) for i in range(2)]
            mrow = mk.sb("mrow", [1, 2, 6 * D], F32)
            ba = mk.sb("ba", [1, 6 * D], F32)
            gg = mk.sb("gg", [1, 2, D], F32)
            pm = [mk.ps(f"pm{i}", [128, 512], F32) for i in range(4)]
            mk.dma('sp', ba.v(), b_ada[l:l + 1, :])
            mk.dma('sp', gg[:, 0, :], g_mix[l:l + 1, :])
            mk.dma('sp', gg[:, 1, :], g_ffn[l:l + 1, :])
            for n in range(12):
                w = wa[n % 2]
                mk.dma('pool', w.v(), w_ada[l, :, n * 512:(n + 1) * 512].rearrange("(c p) n -> p c n", p=128))
                for s in range(2):
                    p = pm[(2 * n + s) % 4]
                    for c in range(8):
                        I('pe', 'matmul', out=p.v(), lhsT=lc[:, s * 8 + c, :], rhs=w[:, c, :], start=(c == 0), stop=(c == 7))
                    I('dve', 'tensor_tensor', out=mrow[:, s, n * 512:(n + 1) * 512], in0=p[0:1, :],
                      in1=ba[:, n * 512:(n + 1) * 512], op=ALU.add)
            for s in range(2):
                for (slot, g) in ((1, 0), (4, 1)):
                    I('dve', 'scalar_tensor_tensor', out=mrow[:, s, slot * D:(slot + 1) * D],
                      in0=mrow[:, s, slot * D:(slot + 1) * D], scalar=1.0, in1=gg[:, g, :], op0=ALU.add, op1=ALU.mult)
                mk.dma('sp', md[s:s + 1, :], mrow[:, s, :])
        if stop == f"p1_{l}":
            return done()

        def load_mod(tile_v, s, slot):
            mk.dma('sp', tile_v, md[s, slot * D:(slot + 1) * D].pbc(128))

        with mk.phase():
            win = mk.sb("win", [128, 8, D_IN], BF16)
            wst = [mk.sb(f"wst{i}", [128, D_IN], F32) for i in range(2)]
            for c in range(8):
                mk.dma('sp', wst[c % 2].v(), w_in[l, c * 128:(c + 1) * 128, :])
                if c % 4 == 3:
                    I('pool', 'tensor_copy', out=win[:, c, :], in_=wst[c % 2].v())
                elif c % 2 == 0:
                    I('act', 'activation', out=win[:, c, :], in_=wst[c % 2].v(), func=AF.Copy)
                else:
                    I('dve', 'tensor_copy', out=win[:, c, :], in_=wst[c % 2].v())
            gsb = [mk.sb(f"gsb{s}", [128, D], F32) for s in range(2)]
            shb = [mk.sb(f"shb{s}", [128, D], F32) for s in range(2)]
            for s in range(2):
                load_mod(gsb[s].v(), s, 1)
                load_mod(shb[s].v(), s, 0)
            xb = [mk.sb(f"xb{i}", [128, D], F32) for i in range(2)]
            junk = mk.sb("junk", [128, D], BF16)
            ssb = [mk.sb(f"ss{i}", [128, 1], F32) for i in range(2)]
            hf = mk.sb("hf", [128, D], F32)
            hb = [mk.sb(f"hb{i}", [128, D], BF16) for i in range(2)]
            hT = [mk.sb(f"hT{i}", [128, 8, 128], BF16) for i in range(2)]
            plt = [mk.sb(f"plt{i}", [128, D_IN], F32) for i in range(3)]
            pt = [mk.ps(f"pt{i}", [128, 1024], BF16) for i in range(2)]
            po = [mk.ps(f"po{i}", [128, 512], F32) for i in range(4)]
            kctr = [0]

            def p2_front(tt):
                s = 0 if tt < 16 else 1
                x_, ss_, hb_, hT_, pt_ = xb[tt % 2], ssb[tt % 2], hb[tt % 2], hT[tt % 2], pt[tt % 2]
                mk.dma('sp', x_.v(), xsrc[tt * 128:(tt + 1) * 128, :])
                I('act', 'activation', out=junk.v(), in_=x_.v(), func=AF.Square, accum_out=ss_.v())
                rstd_from_ss(ss_.v(), 1.0 / D, RMS_EPS, 1)
                I('dve', 'scalar_tensor_tensor', out=hf.v(), in0=x_.v(), scalar=ss_.v(), in1=gsb[s].v(), op0=ALU.mult, op1=ALU.mult)
                I('dve', 'tensor_tensor', out=hb_.v(), in0=hf.v(), in1=shb[s].v(), op=ALU.add)
                for c in range(8):
                    I('pe', 'transpose', out=pt_[:, c * 128:(c + 1) * 128], in_=hb_[:, c * 128:(c + 1) * 128], identity=identb.v())
                I('act', 'activation', out=hT_.v().rearrange("p c t -> p (c t)"), in_=pt_.v(), func=AF.Copy)

            def p2_back(tt):
                hT_, plt_ = hT[tt % 2], plt[tt % 3]
                for n in range(6):
                    n0 = n * 512
                    wdt = min(512, D_IN - n0)
                    p = po[kctr[0] % 4]
                    kctr[0] += 1
                    for c in range(8):
                        I('pe', 'matmul', out=p[:, 0:wdt], lhsT=hT_[:, c, :], rhs=win[:, c, n0:n0 + wdt], start=(c == 0), stop=(c == 7))
                    if n == 0:
                        I('act', 'activation', out=plt_[:, n0:n0 + wdt], in_=p[:, 0:wdt], func=AF.Gelu_apprx_tanh)
                    elif n % 2 == 0:
                        I('dve', 'tensor_copy', out=plt_[:, n0:n0 + wdt], in_=p[:, 0:wdt])
                    else:
                        I('act', 'activation', out=plt_[:, n0:n0 + wdt], in_=p[:, 0:wdt], func=AF.Copy)
                mk.dma('sp', pl[tt * 128:(tt + 1) * 128, :], plt_.v())

            for tt in range(19):
                if tt < 18:
                    p2_front(tt)
                if tt >= 1:
                    p2_back(tt - 1)
        if stop == f"p2_{l}":
            return done()

        with mk.phase():
            wsf = mk.sb("wsf", [128, 4, 128], F32)
            wsT = mk.sb("wsT", [128, 4, 128], BF16)
            bsT = mk.sb("bsT", [128, 4], F32)
            vg = mk.sb("vg", [128, 256], F32)
            pw = mk.ps("pw", [128, 512], F32)
            mk.dma('sp', wsf.v(), gm_ws[l].rearrange("g i j -> i g j"))
            mk.dma('sp', bsT.v(), gm_bsT[l])
            mk.dma('sp', vg.v(), gm_vg[l, :].pbc(128))
            for g in range(4):
                I('pe', 'transpose', out=pw[:, g * 128:(g + 1) * 128], in_=wsf[:, g, :], identity=identf.v())
            I('dve', 'tensor_copy', out=wsT.v().rearrange("p g i -> p (g i)"), in_=pw.v())
            guall = mk.sb("guall", [128, n_tiles, 512], F32)
            for t0_ in range(0, n_tiles, 6):
                t1_ = min(n_tiles, t0_ + 6)
                mk.dma('sp', guall[:, t0_:t1_, :], pl[t0_ * 128:t1_ * 128, 0:512].rearrange("(t p) n -> p t n", p=128))
            st4 = [mk.sb(f"st4{i}", [128, 4], F32) for i in range(2)]
            vr4 = [mk.sb(f"vr4{i}", [128, 4], F32) for i in range(2)]
            vc = mk.sb("vc", [128, 4, 64], F32)
            sq = mk.sb("sq", [128, 4, 64], F32)
            vnb = [mk.sb(f"vnb{i}", [128, 256], BF16) for i in range(2)]
            mo = [mk.sb(f"mo{i}", [128, 256], BF16) for i in range(3)]
            pg = [mk.ps(f"pg{i}", [128, 256], F32) for i in range(2)]
            for tt in range(n_tiles):
                m4, v4, vn_, mo_, pg_ = st4[tt % 2], vr4[tt % 2], vnb[tt % 2], mo[tt % 3], pg[tt % 2]
                gu = guall[:, tt, :]
                v3 = gu[:, 256:512].rearrange("p (g d) -> p g d", d=64)
                I('dve', 'reduce_sum', out=m4.v(), in_=v3, axis=AX.X)
                I('dve', 'tensor_scalar', out=m4.v(), in0=m4.v(), scalar1=-1.0 / 64, scalar2=None, op0=ALU.mult)
                I('dve', 'tensor_tensor', out=vc.v(), in0=v3, in1=m4.v().unsq(2).bc([128, 4, 64]), op=ALU.add)
                I('dve', 'tensor_tensor', out=sq.v(), in0=vc.v(), in1=vc.v(), op=ALU.mult)
                I('dve', 'reduce_sum', out=v4.v(), in_=sq.v(), axis=AX.X)
                rstd_from_ss(v4.v(), 1.0 / 64, LN_EPS, 4)
                I('dve', 'tensor_tensor', out=vc.v(), in0=vc.v(), in1=v4.v().unsq(2).bc([128, 4, 64]), op=ALU.mult)
                I('dve', 'tensor_tensor', out=vn_.v(), in0=vc.v().rearrange("p g d -> p (g d)"), in1=vg.v(), op=ALU.mult)
                for g in range(4):
                    I('pe', 'matmul', out=pg_[:, g * 64:(g + 1) * 64], lhsT=wsT[:, g, :], rhs=vn_[:, g * 64:(g + 1) * 64], start=True, stop=True)
                for g in range(4):
                    I('dve', 'scalar_tensor_tensor', out=mo_[:, g * 64:(g + 1) * 64], in0=pg_[:, g * 64:(g + 1) * 64],
                      scalar=bsT[:, g:g + 1], in1=gu[:, g * 64:(g + 1) * 64], op0=ALU.add, op1=ALU.mult)
                mk.dma('sp', mix[tt * 128:(tt + 1) * 128, 0:256], mo_.v())
        if stop == f"p3_{l}":
            return done()

        with mk.phase():
            qT = mk.sb("qT", [128, 4, NTOK], BF16)
            kT = mk.sb("kT", [128, 4, NTOK], BF16)
            with mk.phase():
                gqk = mk.sb("gqk", [128, 16, 64], F32)
                mk.dma('sp', gqk[:, 0:8, :], na_qg.custom(l * 64, [[0, 128], [0, 8], [1, 64]]))
                mk.dma('sp', gqk[:, 8:16, :], na_kg.custom(l * 64, [[0, 128], [0, 8], [1, 64]]))
                I('dve', 'tensor_scalar', out=gqk[:, 0:8, :], in0=gqk[:, 0:8, :], scalar1=DH ** -0.5, scalar2=None, op0=ALU.mult)
                qkv = [mk.sb(f"qkv{i}", [128, 1536], F32) for i in range(2)]
                sq16 = mk.sb("sq16", [128, 16, 64], F32)
                r16 = [mk.sb(f"r16{i}", [128, 16], F32) for i in range(2)]
                nb = [mk.sb(f"nb{i}", [128, 1024], BF16) for i in range(2)]
                vb = [mk.sb(f"vb{i}", [128, 8, 65], BF16) for i in range(3)]
                ptq = [mk.ps(f"ptq{i}", [128, 1024], BF16) for i in range(2)]
                for i in range(3):
                    I('dve', 'memset', ap=vb[i][:, :, 64:65], constant=1.0)

                def p4_front(tt):
                    t_, r_, nb_ = qkv[tt % 2], r16[tt % 2], nb[tt % 2]
                    mk.dma('sp', t_.v(), pl[tt * 128:(tt + 1) * 128, 512:2048])
                    t3 = t_[:, 0:1024].rearrange("p (h d) -> p h d", d=64)
                    I('pool', 'tensor_tensor', out=sq16.v(), in0=t3, in1=t3, op=ALU.mult)
                    I('dve', 'reduce_sum', out=r_.v(), in_=sq16.v(), axis=AX.X)
                    rstd_from_ss(r_.v(), 1.0 / DH, RMS_EPS, 16)
                    I('dve', 'tensor_tensor', out=sq16.v(), in0=t3, in1=r_.v().unsq(2).bc([128, 16, 64]), op=ALU.mult)
                    I('dve', 'tensor_tensor', out=nb_.v().rearrange("p (h d) -> p h d", d=64), in0=sq16.v(), in1=gqk.v(), op=ALU.mult)

                def p4_back(tt):
                    t_, nb_, p_, vb_ = qkv[tt % 2], nb[tt % 2], ptq[tt % 2], vb[tt % 3]
                    for c in range(8):
                        I('pe', 'transpose', out=p_[:, c * 128:(c + 1) * 128], in_=nb_[:, c * 128:(c + 1) * 128], identity=identb.v())
                    I('act', 'activation', out=qT[:, :, tt * 128:(tt + 1) * 128], in_=p_[:, 0:512].rearrange("p (c t) -> p c t", t=128), func=AF.Copy)
                    I('act', 'activation', out=kT[:, :, tt * 128:(tt + 1) * 128], in_=p_[:, 512:1024].rearrange("p (c t) -> p c t", t=128), func=AF.Copy)
                    I('act', 'activation', out=vb_[:, :, 0:64], in_=t_[:, 1024:1536].rearrange("p (h d) -> p h d", d=64), func=AF.Copy)
                    mk.dma('sp', vd[tt * 128:(tt + 1) * 128, :], vb_.v().rearrange("p h d -> p (h d)"))

                for tt in range(19):
                    if tt < 18:
                        p4_front(tt)
                    if tt >= 1:
                        p4_back(tt - 1)
            vctx = mk.sb("vctx", [128, 2, NH, 65], BF16)
            mk.dma('sp', vctx.v().rearrange("p c h d -> p c (h d)"), vd[S:NTOK, :].rearrange("(c p) n -> p c n", p=128))
            btb = [mk.sb(f"bt{i}", [128, NH, 4, 128], F32) for i in range(2)]
            vwb = [mk.sb(f"vw{i}", [128, 4, NH, 65], BF16) for i in range(2)]
            obb = [mk.sb(f"ob{i}", [128, 512], BF16) for i in range(2)]
            kbb = [mk.sb(f"kb{i}", [128, 4, 512], BF16) for i in range(2)]
            qbb = [mk.sb(f"qb{i}", [128, 4, 128], BF16) for i in range(2)]
            pTb = [mk.sb(f"pT{i}", [128, 768], BF16) for i in range(3)]
            sTb = [mk.sb(f"sT{i}", [128, 512], F32) for i in range(3)]
            rcp = [mk.sb(f"rcp{i}", [128, 1], F32) for i in range(2)]
            psw = [mk.ps(f"psw{i}", [128, 512], F32) for i in range(3)]
            psc = [mk.ps(f"psc{i}", [128, 256], F32) for i in range(3)]
            pov = [mk.ps(f"pov{i}", [128, 65], F32) for i in range(2)]
            info = _na_tables()
            nblk = 16 + (0 if last else 2)
            vmap = {0: 0, 1: 1, 2: 1, 3: 2}
            border = sorted(range(16), key=lambda b: (vmap[b // 4], vmap[b % 4], b)) + list(range(16, nblk))
            units = [(k, h) for k in range(nblk) for h in range(NH)]
            bias_buf = {}
            nbias = [0]

            def variant(blk):
                return (vmap[blk // 4], vmap[blk % 4])

            def load_block(k):
                blk = border[k]
                r0, c0, qr0, qc0 = info[blk][:4]
                vw = vwb[k % 2]
                if k == 0 or variant(blk) != variant(border[k - 1]):
                    nbias[0] += 1
                    bt = btb[nbias[0] % 2]
                    mk.dma('sp', bt.v().rearrange("p h c q -> p (h c q)"), na_bias[l, blk])
                    bias_buf[k] = bt
                else:
                    bias_buf[k] = bias_buf[k - 1]
                vd3 = vd.v().rearrange("(r c) n -> r c n", c=GRID_W)[r0:r0 + 16, c0:c0 + 32, :]
                vd4 = vd3.rearrange("(ch rr) x n -> rr x ch n", rr=4)
                for rr in range(4):
                    mk.dma('sp', vw[rr * 32:(rr + 1) * 32, :, :, :].rearrange("p c h d -> p c (h d)"), vd4[rr])
                kb, qb = kbb[k % 2], qbb[k % 2]
                for hp_ in range(4):
                    k3 = kT[:, hp_, 0:S].rearrange("p (r c) -> p r c", c=GRID_W)[:, r0:r0 + 16, c0:c0 + 32]
                    if hp_ < 2:
                        I('pool', 'tensor_copy', out=kb[:, hp_, :].rearrange("p (r c) -> p r c", c=32), in_=k3)
                    else:
                        I('act', 'activation', out=kb[:, hp_, :].rearrange("p (r c) -> p r c", c=32), in_=k3, func=AF.Copy)
                q4 = qT[:, :, 0:S].rearrange("p h (r c) -> p h r c", c=GRID_W)[:, :, qr0:qr0 + 8, qc0:qc0 + 16]
                I('pool', 'tensor_copy', out=qb.v().rearrange("p h (r c) -> p h r c", c=16), in_=q4)

            def front(i):
                k, h = units[i]
                blk = border[k]
                hp, base = h // 2, (h % 2) * 64
                pw_, pc_, pT_, sT_ = psw[i % 3], psc[i % 3], pTb[i % 3], sTb[i % 3]
                if blk < 16:
                    kb, qb, bt = kbb[k % 2], qbb[k % 2], bias_buf[k]
                    q3 = qb[base:base + 64, hp, :]
                    for c in range(4):
                        I('pe', 'matmul', out=pw_[:, c * 128:(c + 1) * 128], lhsT=kb[base:base + 64, hp, c * 128:(c + 1) * 128], rhs=q3, start=True, stop=True)
                    for c in range(2):
                        I('pe', 'matmul', out=pc_[:, c * 128:(c + 1) * 128], lhsT=kT[base:base + 64, hp, S + c * 128:S + (c + 1) * 128], rhs=q3, start=True, stop=True)
                    I('dve', 'tensor_tensor', out=sT_.v(), in0=pw_.v(), in1=bt[:, h, :, :].rearrange("p c q -> p (c q)"), op=ALU.add)
                    I('act', 'activation', out=pT_[:, 0:512], in_=sT_.v(), func=AF.Exp)
                    I('act', 'activation', out=pT_[:, 512:768], in_=pc_.v(), func=AF.Exp)
                else:
                    qt = blk - 16
                    q2 = qT[base:base + 64, hp, S + qt * 128:S + (qt + 1) * 128]
                    for c in range(2):
                        I('pe', 'matmul', out=pc_[:, c * 128:(c + 1) * 128], lhsT=kT[base:base + 64, hp, S + c * 128:S + (c + 1) * 128],
                          rhs=q2, start=True, stop=True)
                    I('act', 'activation', out=pT_[:, 512:768], in_=pc_.v(), func=AF.Exp)

            def back(i):
                k, h = units[i]
                blk = border[k]
                po_, pT_, rc_, ob = pov[i % 2], pTb[i % 3], rcp[i % 2], obb[k % 2]
                if blk < 16:
                    vw = vwb[k % 2]
                    for c in range(6):
                        rhs = vw[:, c, h, :] if c < 4 else vctx[:, c - 4, h, :]
                        I('pe', 'matmul', out=po_.v(), lhsT=pT_[:, c * 128:(c + 1) * 128], rhs=rhs, start=(c == 0), stop=(c == 5))
                else:
                    for c in range(2):
                        I('pe', 'matmul', out=po_.v(), lhsT=pT_[:, 512 + c * 128:512 + (c + 1) * 128], rhs=vctx[:, c, h, :], start=(c == 0), stop=(c == 1))
                I('dve', 'reciprocal', out=rc_.v(), in_=po_[:, 64:65])
                I('dve', 'tensor_scalar', out=ob[:, h * 64:(h + 1) * 64], in0=po_[:, 0:64], scalar1=rc_.v(), scalar2=None, op0=ALU.mult)
                if h == NH - 1:
                    if blk < 16:
                        qr0, qc0 = info[blk][2:4]
                        for a in range(8):
                            t0 = (qr0 + a) * GRID_W + qc0
                            mk.dma('sp', mix[t0:t0 + 16, 256:768], ob[a * 16:(a + 1) * 16, :], force=True)
                    else:
                        t0 = S + (blk - 16) * 128
                        mk.dma('sp', mix[t0:t0 + 128, 256:768], ob.v(), force=True)

            load_block(0)
            for i in range(len(units) + 2):
                if i < len(units):
                    front(i)
                if i >= 2:
                    back(i - 2)
                if i < len(units):
                    k, h = units[i]
                    if h == 2 and k + 1 < 16:
                        load_block(k + 1)
        if stop == f"p5_{l}":
            return done()

        streams = [dict(L=S, row0=0, tag='l')]
        if not last:
            streams.append(dict(L=CTX, row0=S, tag='c'))
        for st in streams:
            st['nT'] = st['L'] // 128
        with mk.phase():
            for st in streams:
                st['Kr'] = mk.sb("Kr" + st['tag'], [128, st['nT'], 512], BF16)
                st['Ks'] = mk.sb("Ks" + st['tag'], [128, st['nT'], 512], BF16)
            with mk.phase():
                w1 = mk.sb("w1", [33, 64], F32)
                w2 = mk.sb("w2", [64, 64], F32)
                w3 = mk.sb("w3", [64, 1024], F32)
                fb = mk.sb("fb", [64, 4], F32)
                sc = mk.sb("sc", [64, 2], F32)
                of = mk.sb("of", [64, 2], F32)
                mk.dma('sp', w1.v(), hy_w1[l])
                mk.dma('sp', w2.v(), hy_w2[l])
                mk.dma('sp', w3.v(), hy_w3[l])
                mk.dma('sp', fb.v(), hy_fb[l])
                for st in streams:
                    L, nT, tg = st['L'], st['nT'], st['tag']
                    st['posT'] = mk.sb("posT" + tg, [33, L], F32)
                    st['dec'] = mk.sb("dec" + tg, [128, nT, 256], F32)
                    st['h2T'] = mk.sb("h2T" + tg, [64, L], F32)
                    st['Ab'] = mk.sb("Ab" + tg, [128, nT, 512], BF16)
                    st['Bb'] = mk.sb("Bb" + tg, [128, nT, 512], BF16)
                    st['rn'] = mk.sb("rn" + tg, [128, 2, 256], F32)
                    st['pS'] = [mk.ps(f"pS{tg}{i}", [128, 512], F32) for i in range(2)]
                    mk.dma('sp', st['posT'].v(), cst[f'posT{L}'].v())
                    mk.dma('sp', st['dec'].v(), cst[f'decay{L}'].v())
                I('dve', 'tensor_scalar', out=sc.v(), in0=fb[:, 0:2], scalar1=1.0 / 3.0, scalar2=None, op0=ALU.mult)
                I('dve', 'tensor_tensor', out=of.v(), in0=fb[:, 2:4], in1=sc.v(), op=ALU.mult)
                u1b = [mk.sb(f"u1{i}", [64, 512], F32) for i in range(2)]
                u2b = [mk.sb(f"u2{i}", [64, 512], F32) for i in range(2)]
                hrot = [mk.sb(f"hrot{i}", [128, 1024], F32) for i in range(2)]
                habs = [mk.sb(f"habs{i}", [128, 1024], F32) for i in range(2)]
                ph = [mk.ps(f"ph{i}", [128, 512], F32) for i in range(2)]
                p3 = [mk.ps(f"p3{i}", [128, 512], F32) for i in range(2)]
                chunks = []
                for st in streams:
                    CW = min(512, st['L'])
                    for pc in range(st['L'] // CW):
                        chunks.append((st, slice(pc * CW, (pc + 1) * CW), CW))
                h1c = [mk.sb(f"h1c{i}", [64, 512], F32) for i in range(len(chunks))]
                for k_ in range(2):
                    wk = (w1, w2)[k_]
                    for ci, (st, cs, CW) in enumerate(chunks):
                        u1, u2, p_ = u1b[ci % 2], u2b[ci % 2], ph[ci % 2]
                        src = st['posT'][:, cs] if k_ == 0 else h1c[ci][:, 0:CW]
                        dst = h1c[ci][:, 0:CW] if k_ == 0 else st['h2T'][:, cs]
                        I('pe', 'matmul', out=p_[0:64, 0:CW], lhsT=wk.v(), rhs=src, start=True, stop=True)
                        I('act', 'activation', out=u1[:, 0:CW], in_=p_[0:64, 0:CW], func=AF.Sin, bias=of[:, k_:k_ + 1], scale=sc[:, k_:k_ + 1])
                        I('dve', 'tensor_tensor', out=u2[:, 0:CW], in0=u1[:, 0:CW], in1=u1[:, 0:CW], op=ALU.mult)
                        I('dve', 'tensor_scalar', out=u2[:, 0:CW], in0=u2[:, 0:CW], scalar1=-4.0, scalar2=3.0, op0=ALU.mult, op1=ALU.add)
                        I('dve', 'tensor_tensor', out=dst, in0=u1[:, 0:CW], in1=u2[:, 0:CW], op=ALU.mult)
                kq = 0
                for st in streams:
                    nT = st['nT']
                    for tc in range(nT):
                        hr, ha = hrot[kq % 2], habs[kq % 2]
                        kq += 1
                        for hf_ in range(2):
                            p = p3[hf_]
                            I('pe', 'matmul', out=p.v(), lhsT=st['h2T'][:, tc * 128:(tc + 1) * 128], rhs=w3[:, hf_ * 512:(hf_ + 1) * 512], start=True, stop=True)
                            I('dve', 'tensor_tensor', out=hr[:, hf_ * 512:(hf_ + 1) * 512].rearrange("p (a c) -> p a c", c=256),
                              in0=p.v().rearrange("p (a c) -> p a c", c=256), in1=st['dec'][:, tc, :].unsq(1).bc([128, 2, 256]), op=ALU.mult)
                        I('act', 'activation', out=ha.v(), in_=hr.v(), func=AF.Abs)
                        for hf_ in range(2):
                            I('pe', 'matmul', out=st['pS'][hf_].v(), lhsT=onesf.v(), rhs=ha[:, hf_ * 512:(hf_ + 1) * 512], start=(tc == 0), stop=(tc == nT - 1))
                        h4 = hr.v().rearrange("p (o d c) -> p o d c", o=2, d=2)
                        if tc == 0:
                            I('dve', 'tensor_scalar', out=h4[:, :, 1, :], in0=h4[:, :, 1, :], scalar1=pmask.v(), scalar2=None, op0=ALU.mult)
                        I('dve', 'tensor_tensor', out=st['Ab'][:, tc, :].rearrange("p (o c) -> p o c", o=2), in0=h4[:, :, 0, :], in1=h4[:, :, 1, :], op=ALU.add)
                        I('pool', 'tensor_tensor', out=st['Bb'][:, tc, :].rearrange("p (o c) -> p o c", o=2), in0=h4[:, :, 0, :], in1=h4[:, :, 1, :], op=ALU.subtract)
                for st in streams:
                    rn = st['rn']
                    for o in range(2):
                        I('dve', 'tensor_copy', out=rn[:, o, :], in_=st['pS'][o][:, 0:256])
                        I('dve', 'tensor_tensor', out=rn[:, o, :], in0=rn[:, o, :], in1=st['pS'][o][:, 256:512], op=ALU.add)
                    I('dve', 'reciprocal', out=rn.v(), in_=rn.v())
                cfb = [mk.sb(f"cfk{i}", [128, 16, 128], BF16) for i in range(2)]
                sfb = [mk.sb(f"sfk{i}", [128, 16, 128], BF16) for i in range(2)]
                kq = 0
                for st in streams:
                    L, nT = st['L'], st['nT']
                    rnf = st['rn'].v().rearrange("p o c -> p (o c)")
                    for fc in range(nT):
                        cf, sf = cfb[kq % 2], sfb[kq % 2]
                        kq += 1
                        mk.dma('sp', cf[:, 0:nT, :].rearrange("p t f -> p (t f)"), cst[f'Cf{L}'][fc])
                        mk.dma('sp', sf[:, 0:nT, :].rearrange("p t f -> p (t f)"), cst[f'Sf{L}'][fc])
                        for tc in range(nT):
                            I('pe', 'matmul', out=ph[0].v(), lhsT=cf[:, tc, :], rhs=st['Ab'][:, tc, :], start=(tc == 0), stop=(tc == nT - 1))
                        for tc in range(nT):
                            I('pe', 'matmul', out=ph[1].v(), lhsT=sf[:, tc, :], rhs=st['Bb'][:, tc, :], start=(tc == 0), stop=(tc == nT - 1))
                        I('dve', 'tensor_tensor', out=st['Kr'][:, fc, :], in0=ph[0].v(), in1=rnf, op=ALU.mult)
                        I('dve', 'tensor_tensor', out=st['Ks'][:, fc, :], in0=ph[1].v(), in1=rnf, op=ALU.mult)
            swb = mk.sb("swb", [128, 3, 768], F32)
            sbb = mk.sb("sbb", [128, 768], F32)
            dbb = mk.sb("dbb", [128, 512], F32)
            mk.dma('sp', swb.v().rearrange("p k n -> p (k n)"), hy_sw[l].rearrange("k n -> (k n)").pbc(128))
            mk.dma('sp', sbb.v(), hy_sb[l, :].pbc(128))
            mk.dma('sp', dbb.v(), hy_bias[l, :].pbc(128))
            for st in streams:
                nT, tg = st['nT'], st['tag']
                st['zf'] = mk.sb("zf" + tg, [128, nT, 256], F32)
                st['zb'] = mk.sb("zb" + tg, [128, nT, 256], BF16)
                st['gts'] = mk.sb("gts" + tg, [128, nT, 512], F32)
                st['Yr'] = mk.sb("Yr" + tg, [128, nT, 256], BF16)
                st['Ys'] = mk.sb("Ys" + tg, [128, nT, 256], BF16)
            a3 = [mk.sb(f"a3{i}", [128, 3, 768], F32) for i in range(3)]
            accb = [mk.sb(f"acc{i}", [128, 768], F32) for i in range(2)]
            kq = 0
            for st in streams:
                nT, row0 = st['nT'], st['row0']
                for tc in range(nT):
                    a_, acc = a3[kq % 3], accb[kq % 2]
                    kq += 1
                    g0 = row0 + tc * 128
                    mk.dma('sp', a_[:, 1, :], pl[g0:g0 + 128, 2048:2816])
                    if tc == 0:
                        I('dve', 'memset', ap=a_[0:1, 0, :], constant=0.0)
                        mk.dma('sp', a_[1:128, 0, :], pl[g0:g0 + 127, 2048:2816])
                    else:
                        mk.dma('sp', a_[:, 0, :], pl[g0 - 1:g0 + 127, 2048:2816])
                    if tc == nT - 1:
                        I('dve', 'memset', ap=a_[:, 2, :], constant=0.0)
                        mk.dma('sp', a_[0:127, 2, :], pl[g0 + 1:g0 + 128, 2048:2816])
                    else:
                        mk.dma('sp', a_[:, 2, :], pl[g0 + 1:g0 + 129, 2048:2816])
                    I('dve', 'tensor_tensor', out=acc.v(), in0=a_[:, 1, :], in1=swb[:, 1, :], op=ALU.mult)
                    I('dve', 'tensor_tensor', out=acc.v(), in0=acc.v(), in1=sbb.v(), op=ALU.add)
                    I('pool', 'tensor_tensor', out=a_[:, 0, :], in0=a_[:, 0, :], in1=swb[:, 0, :], op=ALU.mult)
                    I('pool', 'tensor_tensor', out=a_[:, 2, :], in0=a_[:, 2, :], in1=swb[:, 2, :], op=ALU.mult)
                    I('dve', 'tensor_tensor', out=acc.v(), in0=acc.v(), in1=a_[:, 0, :], op=ALU.add)
                    I('dve', 'tensor_tensor', out=acc.v(), in0=acc.v(), in1=a_[:, 2, :], op=ALU.add)
                    I('act', 'activation', out=st['zf'][:, tc, :], in_=acc[:, 0:256], func=AF.Copy)
                    I('act', 'activation', out=st['zb'][:, tc, :], in_=acc[:, 0:256], func=AF.Copy)
                    I('act', 'activation', out=st['gts'][:, tc, :], in_=acc[:, 256:768], func=AF.Copy)
            cfb = [mk.sb(f"cf{i}", [128, 16, 128], BF16) for i in range(2)]
            sfb = [mk.sb(f"sf{i}", [128, 16, 128], BF16) for i in range(2)]
            zr = [mk.sb(f"zr{i}", [128, 256], F32) for i in range(2)]
            zs = [mk.sb(f"zs{i}", [128, 256], F32) for i in range(2)]
            t1 = mk.sb("t1", [128, 256], F32)
            t2 = mk.sb("t2", [128, 256], F32)
            t3 = mk.sb("t3", [128, 256], F32)
            t4 = mk.sb("t4", [128, 256], F32)
            dzb = [mk.sb(f"dz{i}", [128, 256], F32) for i in range(2)]
            pz = [mk.ps(f"pz{i}", [128, 256], F32) for i in range(4)]
            py = [mk.ps(f"py{i}", [128, 256], F32) for i in range(2)]
            kq = 0
            for n in range(2):
                ksl = slice(n * 256, (n + 1) * 256)
                for st in streams:
                    L, nT = st['L'], st['nT']
                    Kr, Ks, zb, Yr, Ys = st['Kr'], st['Ks'], st['zb'], st['Yr'], st['Ys']
                    for fc in range(nT):
                        cf, sf = cfb[kq % 2], sfb[kq % 2]
                        pzr, pzs = pz[(kq % 2) * 2], pz[(kq % 2) * 2 + 1]
                        zr_, zs_ = zr[kq % 2], zs[kq % 2]
                        kq += 1
                        mk.dma('sp', cf[:, 0:nT, :].rearrange("p t f -> p (t f)"), cst[f'Cf{L}'][fc])
                        mk.dma('sp', sf[:, 0:nT, :].rearrange("p t f -> p (t f)"), cst[f'Sf{L}'][fc])
                        for tc in range(nT):
                            I('pe', 'matmul', out=pzr.v(), lhsT=cf[:, tc, :], rhs=zb[:, tc, :], start=(tc == 0), stop=(tc == nT - 1))
                        for tc in range(nT):
                            I('pe', 'matmul', out=pzs.v(), lhsT=sf[:, tc, :], rhs=zb[:, tc, :], start=(tc == 0), stop=(tc == nT - 1))
                        I('act', 'activation', out=zr_.v(), in_=pzr.v(), func=AF.Copy)
                        I('act', 'activation', out=zs_.v(), in_=pzs.v(), func=AF.Copy)
                        I('pool', 'tensor_tensor', out=t1.v(), in0=zr_.v(), in1=Kr[:, fc, ksl], op=ALU.mult)
                        I('pool', 'tensor_tensor', out=t2.v(), in0=zs_.v(), in1=Ks[:, fc, ksl], op=ALU.mult)
                        I('pool', 'tensor_tensor', out=Yr[:, fc, :], in0=t1.v(), in1=t2.v(), op=ALU.subtract)
                        I('dve', 'tensor_tensor', out=t3.v(), in0=zr_.v(), in1=Ks[:, fc, ksl], op=ALU.mult)
                        I('dve', 'tensor_tensor', out=t4.v(), in0=zs_.v(), in1=Kr[:, fc, ksl], op=ALU.mult)
                        I('dve', 'tensor_tensor', out=Ys[:, fc, :], in0=t3.v(), in1=t4.v(), op=ALU.add)
                for st in streams:
                    L, nT = st['L'], st['nT']
                    zf, zb, gts, Yr, Ys = st['zf'], st['zb'], st['gts'], st['Yr'], st['Ys']
                    for tc in range(nT):
                        ci, si = cfb[kq % 2], sfb[kq % 2]
                        p, dz = py[kq % 2], dzb[kq % 2]
                        kq += 1
                        mk.dma('sp', ci[:, 0:nT, :].rearrange("p t f -> p (t f)"), cst[f'Ci{L}'][tc])
                        mk.dma('sp', si[:, 0:nT, :].rearrange("p t f -> p (t f)"), cst[f'Si{L}'][tc])
                        for fc in range(nT):
                            I('pe', 'matmul', out=p.v(), lhsT=ci[:, fc, :], rhs=Yr[:, fc, :], start=(fc == 0), stop=False)
                        for fc in range(nT):
                            I('pe', 'matmul', out=p.v(), lhsT=si[:, fc, :], rhs=Ys[:, fc, :], start=False, stop=(fc == nT - 1))
                        I('pool', 'tensor_tensor', out=dz.v(), in0=zf[:, tc, :], in1=dbb[:, ksl], op=ALU.mult)
                        I('dve', 'scalar_tensor_tensor', out=zf[:, tc, :], in0=p.v(), scalar=1.0 / L, in1=dz.v(), op0=ALU.mult, op1=ALU.add)
                        I('dve', 'tensor_tensor', out=zf[:, tc, :], in0=zf[:, tc, :], in1=gts[:, tc, ksl], op=ALU.mult)
                        I('act', 'activation', out=zb[:, tc, :], in_=zf[:, tc, :], func=AF.Copy)
            for st in streams:
                L, row0 = st['L'], st['row0']
                mk.dma('sp', mix[row0:row0 + L, 768:1024].rearrange("(t p) c -> p t c", p=128), st['zb'].v())
        if stop == f"p6_{l}":
            return done()

        with mk.phase():
            wo = mk.sb("wo", [128, 8, D], BF16)
            wst = [mk.sb(f"wst{i}", [128, 2, D], F32) for i in range(2)]
            for c2 in range(4):
                mk.dma('sp', wst[c2 % 2].v(), w_out[l, c2 * 256:(c2 + 1) * 256, :].rearrange("(c p) n -> p c n", p=128))
                if c2 % 2 == 0:
                    I('act', 'activation', out=wo[:, c2 * 2:(c2 + 1) * 2, :], in_=wst[c2 % 2].v(), func=AF.Copy)
                else:
                    I('dve', 'tensor_copy', out=wo[:, c2 * 2:(c2 + 1) * 2, :], in_=wst[c2 % 2].v())
            g2 = [mk.sb(f"g2{s}", [128, D], F32) for s in range(2)]
            for s in range(2):
                load_mod(g2[s].v(), s, 2)
            xb = [mk.sb(f"xb{i}", [128, D], F32) for i in range(4)]
            mb = [mk.sb(f"mb{i}", [128, D], BF16) for i in range(4)]
            mT = [mk.sb(f"mT{i}", [128, 8, 128], BF16) for i in range(2)]
            tmp = mk.sb("tmp", [128, D], F32)
            pt = [mk.ps(f"pt{i}", [128, 1024], BF16) for i in range(2)]
            po = [mk.ps(f"po{i}", [128, 512], F32) for i in range(4)]
            def p7_front(tt):
                x_, m_, mT_, pt_ = xb[tt % 4], mb[tt % 4], mT[tt % 2], pt[tt % 2]
                mk.dma('sp', x_.v(), xsrc[tt * 128:(tt + 1) * 128, :])
                mk.dma('sp', m_.v(), mix[tt * 128:(tt + 1) * 128, :])
                for c in range(8):
                    I('pe', 'transpose', out=pt_[:, c * 128:(c + 1) * 128], in_=m_[:, c * 128:(c + 1) * 128], identity=identb.v())
                I('act', 'activation', out=mT_.v().rearrange("p c t -> p (c t)"), in_=pt_.v(), func=AF.Copy)

            def p7_back(tt):
                s = 0 if tt < 16 else 1
                x_, mT_ = xb[tt % 4], mT[tt % 2]
                for hf_ in range(2):
                    p = po[(tt % 2) * 2 + hf_]
                    for c in range(8):
                        I('pe', 'matmul', out=p.v(), lhsT=mT_[:, c, :], rhs=wo[:, c, hf_ * 512:(hf_ + 1) * 512], start=(c == 0), stop=(c == 7))
                    hs = slice(hf_ * 512, (hf_ + 1) * 512)
                    I('dve', 'tensor_tensor', out=tmp[:, hs], in0=p.v(), in1=g2[s][:, hs], op=ALU.mult)
                    I('dve', 'tensor_tensor', out=x_[:, hs], in0=x_[:, hs], in1=tmp[:, hs], op=ALU.add)
                mk.dma('sp', xs[tt * 128:(tt + 1) * 128, :], x_.v())

            for tt in range(n_tiles + 1):
                if tt < n_tiles:
                    p7_front(tt)
                if tt >= 1:
                    p7_back(tt - 1)
        if stop == f"p7_{l}":
            return done()

        ntok = n_tiles * 128
        with mk.phase():
            hTa = mk.sb("hTa", [128, 8, ntok], BF16)
            oacc = mk.sb("oacc", [128, n_tiles, D], F32)
            with mk.phase():
                gsb = [mk.sb(f"gsb{s}", [128, D], F32) for s in range(2)]
                shb = [mk.sb(f"shb{s}", [128, D], F32) for s in range(2)]
                for s in range(2):
                    load_mod(gsb[s].v(), s, 4)
                    load_mod(shb[s].v(), s, 3)
                wr = mk.sb("wr", [128, 8, 36], F32)
                brb = mk.sb("brb", [128, 36], F32)
                mk.dma('sp', wr.v(), moe_wr[l].rearrange("(c p) n -> p c n", p=128))
                mk.dma('sp', brb.v(), moe_br[l, :].pbc(128))
                xb = [mk.sb(f"xb{i}", [128, D], F32) for i in range(2)]
                junk = mk.sb("junk", [128, D], BF16)
                ssb = [mk.sb(f"ss{i}", [128, 1], F32) for i in range(2)]
                hf = [mk.sb(f"hf{i}", [128, D], F32) for i in range(2)]
                hTf = [mk.sb(f"hTf{i}", [128, 8, 128], F32) for i in range(2)]
                ptf = [mk.ps(f"ptf{i}", [128, 512], F32) for i in range(4)]
                pr = [mk.ps(f"pr{i}", [128, 36], F32) for i in range(2)]
                pct = [mk.ps(f"pct{i}", [32, 128], F32) for i in range(2)]
                combT = mk.sb("combT", [32, ntok], BF16)
                lgall = mk.sb("lgall", [128, n_tiles, 36], F32)
                cmball = mk.sb("cmball", [128, n_tiles, 32], F32)
                def p8_front(tt):
                    s = 0 if tt < 16 else 1
                    i2 = tt % 2
                    x_, ss_, hf_, hTf_ = xb[i2], ssb[i2], hf[i2], hTf[i2]
                    mk.dma('sp', x_.v(), xs[tt * 128:(tt + 1) * 128, :])
                    I('act', 'activation', out=junk.v(), in_=x_.v(), func=AF.Square, accum_out=ss_.v())
                    rstd_from_ss(ss_.v(), 1.0 / D, RMS_EPS, 1)
                    I('dve', 'scalar_tensor_tensor', out=hf_.v(), in0=x_.v(), scalar=ss_.v(), in1=gsb[s].v(), op0=ALU.mult, op1=ALU.mult)
                    I('pool', 'tensor_tensor', out=hf_.v(), in0=hf_.v(), in1=shb[s].v(), op=ALU.add)
                    for hh in range(2):
                        p = ptf[i2 * 2 + hh]
                        for c in range(4):
                            cc = hh * 4 + c
                            I('pe', 'transpose', out=p[:, c * 128:(c + 1) * 128], in_=hf_[:, cc * 128:(cc + 1) * 128], identity=identf.v())
                        I('act', 'activation', out=hTf_[:, hh * 4:(hh + 1) * 4, :].rearrange("p c t -> p (c t)"), in_=p.v(), func=AF.Copy)
                        I('dve', 'tensor_copy', out=hTa[:, hh * 4:(hh + 1) * 4, tt * 128:(tt + 1) * 128],
                          in_=hTf_[:, hh * 4:(hh + 1) * 4, :])
                def p8_back(tt):
                    i2 = tt % 2
                    hTf_ = hTf[i2]
                    pr_ = pr[i2]
                    for c in range(8):
                        I('pe', 'matmul', out=pr_.v(), lhsT=hTf_[:, c, :], rhs=wr[:, c, :], start=(c == 0), stop=(c == 7))
                    I('dve', 'tensor_tensor', out=lgall[:, tt, :], in0=pr_.v(), in1=brb.v(), op=ALU.add)
                for tt in range(n_tiles + 1):
                    if tt < n_tiles:
                        p8_front(tt)
                    if tt >= 1:
                        p8_back(tt - 1)
                routing_batched(mk, lgall.v(), n_tiles, cmball.v())
                for tt in range(n_tiles):
                    pc_ = pct[tt % 2]
                    I('pe', 'transpose', out=pc_.v(), in_=cmball[:, tt, :], identity=identf.v())
                    I('act', 'activation', out=combT[:, tt * 128:(tt + 1) * 128], in_=pc_.v(), func=AF.Copy)
                mk.dma('sp', cbd[:, 0:ntok], combT.v())
            with mk.phase():
                wgb = [mk.sb(f"wg{i}", [128, 2, 8, FH], BF16) for i in range(2)]
                wub = [mk.sb(f"wu{i}", [128, 2, 8, FH], BF16) for i in range(2)]
                wdb = [mk.sb(f"wd{i}", [128, 2, 2, D], BF16) for i in range(2)]
                actb = [mk.sb(f"act{i}", [128, 2, 2, 512], BF16) for i in range(2)]
                sgb = [mk.sb(f"sg{i}", [128, 512], F32) for i in range(2)]
                tb = [mk.sb(f"tb{i}", [128, 512], F32) for i in range(2)]
                pgp = [mk.ps(f"pgp{i}", [128, 512], F32) for i in range(2)]
                pup = [mk.ps(f"pup{i}", [128, 512], F32) for i in range(2)]
                cbb = [mk.sb(f"cbb{i}", [128, ntok], BF16) for i in range(3)]
                pdp = [mk.ps(f"pdp{i}", [128, 512], F32) for i in range(4)]
                groups = [(g0, min(512, ntok - g0)) for g0 in range(0, ntok, 512)]
                g5 = [mk.sb(f"g5{s}", [128, D], F32) for s in range(2)]
                for s in range(2):
                    load_mod(g5[s].v(), s, 5)
                xrb = [mk.sb(f"xr{i}", [128, D], F32) for i in range(2)]
                res_dst = out if (l == n_layers - 1 and not debug) or last else xs

                def res_load(tt):
                    if tt < n_tiles:
                        mk.dma('sp', xrb[tt % 2].v(), xs[tt * 128:(tt + 1) * 128, :])

                def res_finish(tt):
                    s = 0 if tt < 16 else 1
                    x_ = xrb[tt % 2]
                    I('dve', 'tensor_tensor', out=oacc[:, tt, :], in0=oacc[:, tt, :], in1=g5[s].v(), op=ALU.mult)
                    I('dve', 'tensor_tensor', out=x_.v(), in0=x_.v(), in1=oacc[:, tt, :], op=ALU.add)
                    if not (res_dst is out and tt >= 16):
                        mk.dma('sp', res_dst[tt * 128:(tt + 1) * 128, :], x_.v())
                kk = 0
                kd = 0
                gi = 0
                for pair in range(0 if stop == f"p8a_{l}" else NEXP // 2):
                    wg, wu, wd = wgb[pair % 2], wub[pair % 2], wdb[pair % 2]
                    for e in range(2):
                        ge = pair * 2 + e
                        mk.dma('sp', cbb[ge % 3].v(), cbd[ge, 0:ntok].pbc(128))
                        mk.dma('pool', wg[:, e, :, :], moe_wg[l, ge].rearrange("(c p) f -> p c f", p=128))
                        mk.dma('pool', wu[:, e, :, :], moe_wu[l, ge].rearrange("(c p) f -> p c f", p=128))
                        mk.dma('pool', wd[:, e, :, :], moe_wd[l, ge].rearrange("(c p) n -> p c n", p=128))
                    last_pair = (pair == NEXP // 2 - 1)
                    if last_pair:
                        res_load(0)
                    for (g0, gw) in groups:
                        at = actb[gi % 2]
                        gi += 1
                        for e in range(2):
                            ge = pair * 2 + e
                            for fc in range(2):
                                pg_, pu_, sg_, tb_ = pgp[kk % 2], pup[kk % 2], sgb[kk % 2], tb[kk % 2]
                                cb_ = cbb[ge % 3]
                                kk += 1
                                for c in range(8):
                                    I('pe', 'matmul', out=pg_[:, 0:gw], lhsT=wg[:, e, c, fc * 128:(fc + 1) * 128], rhs=hTa[:, c, g0:g0 + gw], start=(c == 0), stop=(c == 7))
                                for c in range(8):
                                    I('pe', 'matmul', out=pu_[:, 0:gw], lhsT=wu[:, e, c, fc * 128:(fc + 1) * 128], rhs=hTa[:, c, g0:g0 + gw], start=(c == 0), stop=(c == 7))
                                I('act', 'activation', out=sg_[:, 0:gw], in_=pg_[:, 0:gw], func=AF.Silu)
                                I('dve', 'tensor_tensor', out=tb_[:, 0:gw], in0=sg_[:, 0:gw], in1=pu_[:, 0:gw], op=ALU.mult)
                                I('dve', 'tensor_tensor', out=at[:, e, fc, 0:gw], in0=tb_[:, 0:gw], in1=cb_[:, g0:g0 + gw], op=ALU.mult)
                        for ti in range(gw // 128):
                            tt = g0 // 128 + ti
                            if last_pair:
                                res_load(tt + 1)
                            for hf_ in range(2):
                                pd_ = pdp[kd % 4]
                                kd += 1
                                hs = slice(hf_ * 512, (hf_ + 1) * 512)
                                n_ = 0
                                for e in range(2):
                                    for fc in range(2):
                                        I('pe', 'matmul', out=pd_.v(), lhsT=at[:, e, fc, ti * 128:(ti + 1) * 128], rhs=wd[:, e, fc, hs], start=(n_ == 0), stop=(n_ == 3))
                                        n_ += 1
                                if pair == 0:
                                    I('dve', 'tensor_copy', out=oacc[:, tt, hs], in_=pd_.v())
                                else:
                                    I('dve', 'tensor_tensor', out=oacc[:, tt, hs], in0=oacc[:, tt, hs], in1=pd_.v(), op=ALU.add)
                            if last_pair:
                                res_finish(tt)
        if stop in (f"p8_{l}", f"p8a_{l}"):
            return done()
    return done()


def make_in_maps(inputs):
    c = _consts()
    f32 = np.float32
    x = np.asarray(inputs['x'], f32)
    B = x.shape[0]
    ctx = np.asarray(inputs['ctx'], f32)
    cc = np.asarray(inputs['c'], f32)
    c_ctx = np.asarray(inputs['c_ctx'], f32)
    info = _na_tables()
    rpb = np.asarray(inputs['na_rpb'], f32)
    nab = np.empty((DEPTH, 16, 128, NH, 4, 128), f32)
    for b_, (r0, c0, qr0, qc0, mask, dr, dc) in enumerate(info):
        g = rpb[:, :, dr, dc]
        g = np.where(mask[None, None], g, f32(NEG_MASK))
        nab[:, b_] = g.transpose(0, 3, 1, 2, 4)
    nab = nab.reshape(DEPTH, 16, 128, NH * 4 * 128)
    fb = np.stack([np.asarray(inputs['hy_freq'], f32)[:, 0], np.asarray(inputs['hy_freq'], f32)[:, 1],
                   np.asarray(inputs['hy_b1'], f32), np.asarray(inputs['hy_b2'], f32)], axis=-1)
    shared = {
        'w_ada': np.asarray(inputs['w_ada'], f32), 'b_ada': np.asarray(inputs['b_ada'], f32),
        'g_mix': np.asarray(inputs['g_mix'], f32), 'g_ffn': np.asarray(inputs['g_ffn'], f32),
        'w_in': np.asarray(inputs['w_in'], f32), 'w_out': np.asarray(inputs['w_out'], f32),
        'gmlp_v_gain': np.asarray(inputs['gmlp_v_gain'], f32), 'gmlp_ws': np.asarray(inputs['gmlp_ws'], f32),
        'gmlp_bsT': np.ascontiguousarray(np.asarray(inputs['gmlp_bs'], f32).transpose(0, 2, 1)),
        'na_q_gain': np.asarray(inputs['na_q_gain'], f32), 'na_k_gain': np.asarray(inputs['na_k_gain'], f32),
        'na_bias': nab,
        'hy_short_w': np.asarray(inputs['hy_short_w'], f32), 'hy_short_b': np.asarray(inputs['hy_short_b'], f32),
        'hy_w1': np.asarray(inputs['hy_w1'], f32), 'hy_w2': np.asarray(inputs['hy_w2'], f32), 'hy_w3': np.asarray(inputs['hy_w3'], f32),
        'hy_fb': np.ascontiguousarray(fb), 'hy_bias': np.asarray(inputs['hy_bias'], f32).reshape(DEPTH, 512),
        'moe_wr': np.ascontiguousarray(np.concatenate([np.asarray(inputs['moe_w_rg'], f32), np.asarray(inputs['moe_w_re'], f32)], axis=-1)),
        'moe_br': np.ascontiguousarray(np.concatenate([np.asarray(inputs['moe_b_rg'], f32), np.asarray(inputs['moe_b_re'], f32)], axis=-1)),
        'moe_w_gate': np.asarray(inputs['moe_w_gate'], f32).reshape(DEPTH, NEXP, D, FH),
        'moe_w_up': np.asarray(inputs['moe_w_up'], f32).reshape(DEPTH, NEXP, D, FH),
        'moe_w_down': np.asarray(inputs['moe_w_down'], f32).reshape(DEPTH, NEXP, FH, D),
        'ident': c['ident'],
    }
    for L in (S, CTX):
        nT = L // 128
        shared[f'posT{L}'] = c[f'posT{L}']
        shared[f'decay{L}'] = c[f'decay{L}']
        for nm in ('Cf', 'Sf', 'Ci', 'Si'):
            shared[f'{nm}{L}'] = c[f'{nm}{L}'].reshape(nT, 128, nT * 128)
    maps = []
    for b in range(B):
        m = dict(shared)
        m['xin'] = np.ascontiguousarray(np.concatenate([x[b], ctx[b]], axis=0))
        cT = np.concatenate([cc[b].reshape(8, 128).T, c_ctx.reshape(8, 128).T], axis=1)
        m['cT'] = np.ascontiguousarray(cT)
        maps.append(m)
    return maps


_PROG = {}


def kernel(**inputs):
    if 'nc' not in _PROG:
        _PROG['nc'] = build_program()[0]
    maps = make_in_maps(inputs)
    res = run_bass_kernel_spmd(_PROG['nc'], maps, core_ids=list(range(len(maps))))
    return np.stack([np.asarray(r['out'], np.float32) for r in res.results], axis=0)
```

```python
import math
import os
import numpy as np
import ml_dtypes
import concourse.bass as bass
import concourse.mybir as mybir
from concourse.bass_utils import run_bass_kernel_spmd
from contextlib import ExitStack, contextmanager

F32 = mybir.dt.float32
BF16 = mybir.dt.bfloat16
ALU = mybir.AluOpType
AF = mybir.ActivationFunctionType
AX = mybir.AxisListType

ENGS = ('pe', 'act', 'dve', 'pool', 'sp')
WRITE_KW = ('out', 'accum_out', 'ap')


class Buf:
    def __init__(self, mk, t, name, kind):
        self.mk, self.t, self.name, self.kind = mk, t, name, kind
        self.last_w = None
        self.readers = []
        self.sems = {}
        self.lastw_kind = {}

    view = None

    def __getitem__(self, idx):
        return self.v()[idx]

    def v(self):
        if self.view is not None:
            return V(self, self.t[0:self.view[0], 0:self.view[1]])
        return V(self, self.t[:])

    def custom(self, offset, ap):
        return V(self, bass.AP(self.t, offset, ap))


class V:
    def __init__(self, buf, ap):
        self.buf, self.ap = buf, ap

    def __getitem__(self, idx):
        return V(self.buf, self.ap[idx])

    def rearrange(self, pat, **kw):
        return V(self.buf, self.ap.rearrange(pat, **kw))

    def bc(self, shape):
        return V(self.buf, self.ap.to_broadcast(list(shape)))

    def unsq(self, axis):
        return V(self.buf, self.ap.unsqueeze(axis))

    def pbc(self, n):
        return V(self.buf, self.ap.partition_broadcast(n))


class Ins:
    __slots__ = ('eng', 'fn', 'deps', 'is_dma', 'sem', 'semval', 'signals', 'sigidx', 'nowaw')

    def __init__(self, eng, fn, is_dma=False):
        self.eng, self.fn, self.is_dma = eng, fn, is_dma
        self.deps = []
        self.sem = None
        self.semval = 0
        self.signals = False
        self.sigidx = None
        self.nowaw = False


class SemSlot:
    def __init__(self, sem, kind):
        self.sem = sem
        self.kind = kind
        self.cnt = 0
        self.last = None


class MK:
    def __init__(self, nc, n_pool_sems=70):
        self.nc = nc
        self.es = ExitStack()
        self.lists = {e: [] for e in ENGS}
        self.esem = {}
        n_hw = (n_pool_sems * 3) // 5
        self.free_slots = {
            'hw': [SemSlot(self.es.enter_context(nc.semaphore(f"dqh{i}")), 'hw') for i in range(n_hw)],
            'sw': [SemSlot(self.es.enter_context(nc.semaphore(f"dqs{i}")), 'sw') for i in range(n_pool_sems - n_hw)],
        }
        self.all_slots = self.free_slots['hw'] + self.free_slots['sw']
        self.stack = [self.es]
        self.phase_slots = [[]]
        self.uid = 0

    def sb(self, name, shape, dtype=F32):
        self.uid += 1
        t = self.stack[-1].enter_context(self.nc.sbuf_tensor(f"{name}_{self.uid}", list(shape), dtype))
        return Buf(self, t, name, 'sb')

    def ps(self, name, shape, dtype=F32):
        self.uid += 1
        full = 512 if dtype == F32 else 1024
        t = self.stack[-1].enter_context(self.nc.psum_tensor(f"{name}_{self.uid}", [128, full], dtype))
        b = Buf(self, t, name, 'ps')
        b.view = (shape[0], shape[1])
        return b

    def dram(self, name, shape, dtype=F32, kind="Internal"):
        t = self.nc.dram_tensor(name, list(shape), dtype, kind=kind)
        return Buf(self, t, name, 'dram')

    def _slot_for(self, buf, q):
        kind = 'sw' if q == 'pool' else 'hw'
        if kind not in buf.sems:
            slot = self.free_slots[kind].pop()
            buf.sems[kind] = slot
            if buf.kind != 'dram':
                self.phase_slots[-1].append(slot)
        return buf.sems[kind]

    @contextmanager
    def phase(self):
        st = ExitStack()
        self.stack.append(st)
        self.phase_slots.append([])
        try:
            yield
        finally:
            self.barrier()
            self.stack.pop()
            for s in self.phase_slots.pop():
                self.free_slots[s.kind].append(s)
            st.close()

    def _track(self, ins, reads, writes):
        deps = []
        for b in reads:
            if b.last_w is not None:
                deps.append(('raw', b.last_w))
            for d in b.lastw_kind.values():
                deps.append(('raw', d))
        for b in writes:
            dram_store = ins.is_dma and ins.nowaw and b.last_w is not None and b.last_w.is_dma
            if b.last_w is not None and not dram_store:
                deps.append(('waw', b.last_w))
                for d in b.lastw_kind.values():
                    deps.append(('waw', d))
            for r in b.readers:
                deps.append(('war', r))
        seen = set()
        for kind, d in deps:
            if d is ins or id(d) in seen:
                continue
            if not d.is_dma and not ins.is_dma and d.eng == ins.eng:
                if ins.eng == 'pe':
                    continue
            seen.add(id(d))
            ins.deps.append(d)
            if not d.is_dma:
                d.signals = True
        for b in reads:
            b.readers.append(ins)
        for b in writes:
            b.last_w = ins
            b.readers = []

    def I(self, eng, meth, **kw):
        rb, wb, kk = [], [], {}
        for k, v in kw.items():
            if isinstance(v, V):
                (wb if k in WRITE_KW else rb).append(v.buf)
                kk[k] = v.ap
            else:
                kk[k] = v
        ins = Ins(eng, lambda e: getattr(e, meth)(**kk))
        self._track(ins, rb, wb)
        self.lists[eng].append(ins)
        return ins

    def dma(self, q, out, in_, force=False, nowaw=False, **kw):
        if q == 'sp' and out.buf.kind == 'dram' and not force:
            q = 'pool'
        ins = Ins(q, lambda e: e.dma_start(out=out.ap, in_=in_.ap, **kw), is_dma=True)
        ins.nowaw = nowaw
        slot = self._slot_for(out.buf, q)
        slot.cnt += 16
        ins.sem, ins.semval = slot.sem, slot.cnt
        slot.last = ins
        out.buf.lastw_kind[slot.kind] = ins
        self._track(ins, [in_.buf], [out.buf])
        self.lists[q].append(ins)
        return ins

    def barrier(self):
        last = {}
        for e in ENGS:
            for ins in reversed(self.lists[e]):
                if not ins.is_dma and ins.fn is not None:
                    last[e] = ins
                    ins.signals = True
                    break
        dmas = [s.last for s in self.all_slots if s.last is not None]
        for e in ENGS:
            w = Ins(e, None)
            w.deps = [d for ee, d in last.items() if ee != e] + dmas
            self.lists[e].append(w)

    def wait_bufs(self, eng, bufs):
        w = Ins(eng, None)
        self._track(w, list(bufs), [])
        self.lists[eng].append(w)

    def build(self):
        nc = self.nc
        for e in ENGS:
            self.esem[e] = self.es.enter_context(nc.semaphore("eng_" + e))
            n = 0
            for ins in self.lists[e]:
                if ins.signals and not ins.is_dma and ins.fn is not None:
                    n += 1
                    ins.sigidx = n
        lists, esem = self.lists, self.esem
        stats = {}

        def replay(ename, e):
            waited = {}
            nw = 0
            for ins in lists[ename]:
                for d in ins.deps:
                    if d.is_dma:
                        key, val, sem = ('d', id(d.sem)), d.semval, d.sem
                    else:
                        key, val, sem = ('e', d.eng), d.sigidx, esem[d.eng]
                    if waited.get(key, 0) >= val:
                        continue
                    waited[key] = val
                    e.wait_ge(sem, val)
                    nw += 1
                if ins.fn is None:
                    continue
                r = ins.fn(e)
                if ins.is_dma:
                    r.then_inc(ins.sem, 16)
                elif ins.signals:
                    r.then_inc(esem[ename], 1)
            stats[ename] = (len(lists[ename]), nw)

        with nc.Block() as block:
            @block.tensor
            def _(e):
                replay('pe', e)

            @block.scalar
            def _(e):
                replay('act', e)

            @block.vector
            def _(e):
                replay('dve', e)

            @block.gpsimd
            def _(e):
                replay('pool', e)

            @block.sync
            def _(e):
                replay('sp', e)
        self.stats = stats
        self.es.close()
        return nc


D = 1024
S = 2048
CTX = 256
NTOK = S + CTX
DEPTH = 4
D_IN = 2816
GRID_W = 64
NH = 8
DH = 64
NEXP = 32
FH = 256
RMS_EPS = 1e-6
LN_EPS = 1e-5
TWO_PI = 2.0 * math.pi
NEG_MASK = -30000.0


def _na_tables():
    rows = S // GRID_W
    kr, kc, qr, qc, br, bc = 8, 16, 8, 16, 16, 32
    n_rb, n_cb = rows // qr, GRID_W // qc
    r0 = np.clip(np.arange(n_rb) * qr - kr // 2, 0, rows - br)
    c0 = np.clip(np.arange(n_cb) * qc - kc // 2, 0, GRID_W - bc)
    info = []
    cch, rr, cc = np.meshgrid(np.arange(4), np.arange(4), np.arange(32), indexing='ij')
    krow_l = (4 * cch + rr).reshape(4, 128)
    kcol_l = cc.reshape(4, 128)
    qa, qb = np.meshgrid(np.arange(8), np.arange(16), indexing='ij')
    qa, qb = qa.reshape(128), qb.reshape(128)
    for i in range(n_rb):
        for j in range(n_cb):
            krow = r0[i] + krow_l[:, :, None]
            kcol = c0[j] + kcol_l[:, :, None]
            qrow = (i * qr + qa)[None, None, :]
            qcol = (j * qc + qb)[None, None, :]
            wr = np.clip(qrow - kr // 2, 0, rows - kr)
            wc = np.clip(qcol - kc // 2, 0, GRID_W - kc)
            mask = (krow >= wr) & (krow < wr + kr) & (kcol >= wc) & (kcol < wc + kc)
            dr = np.clip(krow - qrow + 7, 0, 14) + 0 * kcol
            dc = np.clip(kcol - qcol + 15, 0, 30) + 0 * krow
            info.append((int(r0[i]), int(c0[j]), i * qr, j * qc, np.broadcast_to(mask, dr.shape), dr, dc))
    return info


_CONST_CACHE = {}


def _consts():
    if _CONST_CACHE:
        return _CONST_CACHE
    c = {}
    c['ident'] = np.eye(128, dtype=np.float32)
    for L in (S, CTX):
        t = np.linspace(0.0, 1.0, L, dtype=np.float32)[:, None]
        w = (2.0 * math.pi * np.arange(L, dtype=np.float32)[:, None] / L).astype(np.float32)
        f = np.linspace(1e-4, 15, 16, dtype=np.float32)[None, :]
        z = np.concatenate([t, np.cos(f * w), -np.sin(f * w)], axis=-1).astype(np.float32)
        c[f'posT{L}'] = np.ascontiguousarray(z.T)
        min_decay = math.log(1e-2) / 1.5
        max_decay = math.log(1e-2) / 0.3
        deltas = np.abs(np.linspace(min_decay, max_decay, 256, dtype=np.float32))[None, :]
        dec = np.exp(-t * deltas).astype(np.float32)
        nT = L // 128
        c[f'decay{L}'] = np.ascontiguousarray(dec.reshape(nT, 128, 256).transpose(1, 0, 2))
        tt = np.arange(L, dtype=np.float64)[:, None]
        ff = np.arange(L, dtype=np.float64)[None, :] + 0.5
        ang = 2.0 * math.pi * tt * ff / (2 * L)
        C = np.cos(ang)
        Sn = np.sin(ang)
        def fwd(M):
            return np.ascontiguousarray(M.reshape(nT, 128, nT, 128).transpose(2, 1, 0, 3)).astype(ml_dtypes.bfloat16)
        def inv(M):
            return np.ascontiguousarray(M.reshape(nT, 128, nT, 128).transpose(0, 3, 2, 1)).astype(ml_dtypes.bfloat16)
        c[f'Cf{L}'] = fwd(C)
        c[f'Sf{L}'] = fwd(Sn)
        c[f'Ci{L}'] = inv(C)
        c[f'Si{L}'] = inv(Sn)
    _CONST_CACHE.update(c)
    return c


def routing_batched(mk, lg, T, cmb, sfx=""):
    I = mk.I
    def t(nm, shape):
        return mk.sb(f"rt_{nm}{sfx}", shape, F32).v()
    lg4 = lg[:, :, 0:4]
    gmax, se, gp = t("gmax", [128, T]), t("se", [128, T]), t("gp", [128, T])
    oh, sh = t("oh", [128, T, 4]), t("sh", [128, T, 4])
    selg, es, es2 = t("selg", [128, T, 32]), t("es", [128, T, 8]), t("es2", [128, T, 8])
    k1, k2 = t("k1", [128, T, 8]), t("k2", [128, T, 8])
    m1, m2, dd, ee, p1, p2 = (t(n, [128, T]) for n in ("m1", "m2", "dd", "ee", "p1", "p2"))
    ew = t("ew", [128, T, 8])
    I('dve', 'reduce_max', out=gmax, in_=lg4, axis=AX.X)
    I('dve', 'tensor_tensor', out=oh, in0=lg4, in1=gmax.unsq(2).bc([128, T, 4]), op=ALU.is_equal)
    I('dve', 'tensor_tensor', out=sh, in0=lg4, in1=gmax.unsq(2).bc([128, T, 4]), op=ALU.subtract)
    I('act', 'activation', out=sh, in_=sh, func=AF.Exp)
    I('dve', 'reduce_sum', out=se, in_=sh, axis=AX.X)
    I('dve', 'reciprocal', out=gp, in_=se)
    el = lg[:, :, 4:36].rearrange("p t (g e) -> p t g e", e=8)
    I('dve', 'tensor_tensor', out=selg.rearrange("p t (g e) -> p t g e", e=8), in0=el, in1=oh.unsq(3).bc([128, T, 4, 8]), op=ALU.mult)
    I('dve', 'reduce_sum', out=es, in_=selg.rearrange("p t (g e) -> p t e g", e=8), axis=AX.X)
    I('dve', 'reduce_max', out=m1, in_=es, axis=AX.X)
    I('dve', 'tensor_tensor', out=k1, in0=es, in1=m1.unsq(2).bc([128, T, 8]), op=ALU.is_equal)
    I('dve', 'scalar_tensor_tensor', out=es2, in0=k1, scalar=-1e30, in1=es, op0=ALU.mult, op1=ALU.add)
    I('dve', 'reduce_max', out=m2, in_=es2, axis=AX.X)
    I('dve', 'tensor_tensor', out=k2, in0=es2, in1=m2.unsq(2).bc([128, T, 8]), op=ALU.is_equal)
    I('dve', 'tensor_tensor', out=dd, in0=m2, in1=m1, op=ALU.subtract)
    I('act', 'activation', out=ee, in_=dd, func=AF.Exp)
    I('dve', 'tensor_scalar', out=p1, in0=ee, scalar1=1.0, scalar2=None, op0=ALU.add)
    I('dve', 'reciprocal', out=p1, in_=p1)
    I('dve', 'tensor_tensor', out=p2, in0=ee, in1=p1, op=ALU.mult)
    I('dve', 'tensor_tensor', out=p1, in0=p1, in1=gp, op=ALU.mult)
    I('dve', 'tensor_tensor', out=p2, in0=p2, in1=gp, op=ALU.mult)
    I('dve', 'tensor_tensor', out=k1, in0=k1, in1=p1.unsq(2).bc([128, T, 8]), op=ALU.mult)
    I('dve', 'tensor_tensor', out=k2, in0=k2, in1=p2.unsq(2).bc([128, T, 8]), op=ALU.mult)
    I('dve', 'tensor_tensor', out=ew, in0=k1, in1=k2, op=ALU.add)
    I('dve', 'tensor_tensor', out=cmb.rearrange("p t (g e) -> p t g e", e=8), in0=oh.unsq(3).bc([128, T, 4, 8]),
      in1=ew.unsq(2).bc([128, T, 4, 8]), op=ALU.mult)


def build_program(n_layers=DEPTH, stop=None, debug=False):
    nc = bass.Bass("TRN2", target_bir_lowering=False)
    mk = MK(nc)
    I = mk.I

    def ext(name, shape, dt=F32):
        return mk.dram(name, shape, dt, kind="ExternalInput")

    xin = ext("xin", [NTOK, D])
    cT = ext("cT", [128, 16])
    w_ada = ext("w_ada", [DEPTH, D, 6 * D])
    b_ada = ext("b_ada", [DEPTH, 6 * D])
    g_mix = ext("g_mix", [DEPTH, D])
    g_ffn = ext("g_ffn", [DEPTH, D])
    w_in = ext("w_in", [DEPTH, D, D_IN])
    w_out = ext("w_out", [DEPTH, D, D])
    gm_vg = ext("gmlp_v_gain", [DEPTH, 256])
    gm_ws = ext("gmlp_ws", [DEPTH, 4, 128, 128])
    gm_bsT = ext("gmlp_bsT", [DEPTH, 128, 4])
    na_qg = ext("na_q_gain", [DEPTH, 64])
    na_kg = ext("na_k_gain", [DEPTH, 64])
    na_bias = ext("na_bias", [DEPTH, 16, 128, NH * 4 * 128])
    hy_sw = ext("hy_short_w", [DEPTH, 3, 768])
    hy_sb = ext("hy_short_b", [DEPTH, 768])
    hy_w1 = ext("hy_w1", [DEPTH, 33, 64])
    hy_w2 = ext("hy_w2", [DEPTH, 64, 64])
    hy_w3 = ext("hy_w3", [DEPTH, 64, 1024])
    hy_fb = ext("hy_fb", [DEPTH, 64, 4])
    hy_bias = ext("hy_bias", [DEPTH, 512])
    moe_wr = ext("moe_wr", [DEPTH, D, 36])
    moe_br = ext("moe_br", [DEPTH, 36])
    moe_wg = ext("moe_w_gate", [DEPTH, NEXP, D, FH])
    moe_wu = ext("moe_w_up", [DEPTH, NEXP, D, FH])
    moe_wd = ext("moe_w_down", [DEPTH, NEXP, FH, D])
    identd = ext("ident", [128, 128])
    cst = {}
    for L in (S, CTX):
        nT = L // 128
        cst[f'posT{L}'] = ext(f"posT{L}", [33, L])
        cst[f'decay{L}'] = ext(f"decay{L}", [128, nT, 256])
        for nm in ('Cf', 'Sf', 'Ci', 'Si'):
            cst[f'{nm}{L}'] = ext(f"{nm}{L}", [nT, 128, nT * 128], BF16)

    out = mk.dram("out", [S, D], F32, kind="ExternalOutput")
    dk = "ExternalOutput" if debug else "Internal"
    xs = mk.dram("xs", [NTOK, D], F32, kind=dk)
    pl = mk.dram("pl", [NTOK, D_IN], F32, kind=dk)
    mix = mk.dram("mix", [NTOK, D], BF16, kind=dk)
    vd = mk.dram("vd", [NTOK, NH * 65], BF16, kind=dk)
    md = mk.dram("md", [2, 6 * D], F32, kind=dk)
    cbd = mk.dram("cbd", [NEXP, NTOK], BF16, kind=dk)
    if debug:
        dbgc = mk.dram("dbgc", [32, NTOK], BF16, kind=dk)
        dbgh = mk.dram("dbgh", [128, 8 * 256], BF16, kind=dk)

    identf = mk.sb("identf", [128, 128], F32)
    identb = mk.sb("identb", [128, 128], BF16)
    onesf = mk.sb("onesf", [128, 128], F32)
    lc = mk.sb("lc", [128, 16, 128], BF16)
    pmask = mk.sb("pmask", [128, 1], F32)
    negpi = mk.sb("negpi", [128, 1], F32)

    def rstd_from_ss(ss_v, scale, eps, n):
        I('dve', 'tensor_scalar', out=ss_v, in0=ss_v, scalar1=scale, scalar2=eps, op0=ALU.mult, op1=ALU.add)
        I('act', 'activation', out=ss_v, in_=ss_v, func=AF.Sqrt)
        I('dve', 'reciprocal', out=ss_v, in_=ss_v)

    with mk.phase():
        ct = mk.sb("ct", [128, 16], F32)
        sct = mk.sb("sct", [128, 16], F32)
        mk.dma('sp', identf.v(), identd.v())
        mk.dma('pool', identb.v(), identd.v())
        mk.dma('sp', ct.v(), cT.v())
        I('act', 'activation', out=sct.v(), in_=ct.v(), func=AF.Silu)
        I('dve', 'tensor_copy', out=lc.v(), in_=sct.v().unsq(2).bc([128, 16, 128]))
        I('dve', 'memset', ap=onesf.v(), constant=1.0)
        I('dve', 'memset', ap=negpi.v(), constant=-math.pi)
        I('dve', 'tensor_scalar', out=pmask.v(), in0=identf[:, 0:1], scalar1=-1.0, scalar2=1.0, op0=ALU.mult, op1=ALU.add)

    def done():
        mk.wait_bufs('sp', [out, xs, pl, mix, vd, md, cbd])
        mk.build()
        return nc, mk

    for l in range(n_layers):
        last = (l == DEPTH - 1)
        xsrc = xin if l == 0 else xs
        n_tiles = 16 if last else 18

        with mk.phase():
            wa = [mk.sb(f"wa{i}", [128, 8, 512], BF16) for i in range(2)]
            mrow = mk.sb("mrow", [1, 2, 6 * D], F32)
            ba = mk.sb("ba", [1, 6 * D], F32)
            gg = mk.sb("gg", [1, 2, D], F32)
            pm = [mk.ps(f"pm{i}", [128, 512], F32) for i in range(4)]
            mk.dma('sp', ba.v(), b_ada[l:l + 1, :])
            mk.dma('sp', gg[:, 0, :], g_mix[l:l + 1, :])
            mk.dma('sp', gg[:, 1, :], g_ffn[l:l + 1, :])
            for n in range(12):
                w = wa[n % 2]
                mk.dma('pool', w.v(), w_ada[l, :, n * 512:(n + 1) * 512].rearrange("(c p) n -> p c n", p=128))
                for s in range(2):
                    p = pm[(2 * n + s) % 4]
                    for c in range(8):
                        I('pe', 'matmul', out=p.v(), lhsT=lc[:, s * 8 + c, :], rhs=w[:, c, :], start=(c == 0), stop=(c == 7))
                    I('dve', 'tensor_tensor', out=mrow[:, s, n * 512:(n + 1) * 512], in0=p[0:1, :],
                      in1=ba[:, n * 512:(n + 1) * 512], op=ALU.add)
            for s in range(2):
                for (slot, g) in ((1, 0), (4, 1)):
                    I('dve', 'scalar_tensor_tensor', out=mrow[:, s, slot * D:(slot + 1) * D],
                      in0=mrow[:, s, slot * D:(slot + 1) * D], scalar=1.0, in1=gg[:, g, :], op0=ALU.add, op1=ALU.mult)
                mk.dma('sp', md[s:s + 1, :], mrow[:, s, :])
        if stop == f"p1_{l}":
            return done()

        def load_mod(tile_v, s, slot):
            mk.dma('sp', tile_v, md[s, slot * D:(slot + 1) * D].pbc(128))

        with mk.phase():
            win = mk.sb("win", [128, 8, D_IN], BF16)
            wst = [mk.sb(f"wst{i}", [128, D_IN], F32) for i in range(2)]
            for c in range(8):
                mk.dma('sp', wst[c % 2].v(), w_in[l, c * 128:(c + 1) * 128, :])
                if c % 4 == 3:
                    I('pool', 'tensor_copy', out=win[:, c, :], in_=wst[c % 2].v())
                elif c % 2 == 0:
                    I('act', 'activation', out=win[:, c, :], in_=wst[c % 2].v(), func=AF.Copy)
                else:
                    I('dve', 'tensor_copy', out=win[:, c, :], in_=wst[c % 2].v())
            gsb = [mk.sb(f"gsb{s}", [128, D], F32) for s in range(2)]
            shb = [mk.sb(f"shb{s}", [128, D], F32) for s in range(2)]
            for s in range(2):
                load_mod(gsb[s].v(), s, 1)
                load_mod(shb[s].v(), s, 0)
            xb = [mk.sb(f"xb{i}", [128, D], F32) for i in range(2)]
            junk = mk.sb("junk", [128, D], BF16)
            ssb = [mk.sb(f"ss{i}", [128, 1], F32) for i in range(2)]
            hf = mk.sb("hf", [128, D], F32)
            hb = [mk.sb(f"hb{i}", [128, D], BF16) for i in range(2)]
            hT = [mk.sb(f"hT{i}", [128, 8, 128], BF16) for i in range(2)]
            plt = [mk.sb(f"plt{i}", [128, D_IN], F32) for i in range(3)]
            pt = [mk.ps(f"pt{i}", [128, 1024], BF16) for i in range(2)]
            po = [mk.ps(f"po{i}", [128, 512], F32) for i in range(4)]
            kctr = [0]

            def p2_front(tt):
                s = 0 if tt < 16 else 1
                x_, ss_, hb_, hT_, pt_ = xb[tt % 2], ssb[tt % 2], hb[tt % 2], hT[tt % 2], pt[tt % 2]
                mk.dma('sp', x_.v(), xsrc[tt * 128:(tt + 1) * 128, :])
                I('act', 'activation', out=junk.v(), in_=x_.v(), func=AF.Square, accum_out=ss_.v())
                rstd_from_ss(ss_.v(), 1.0 / D, RMS_EPS, 1)
                I('dve', 'scalar_tensor_tensor', out=hf.v(), in0=x_.v(), scalar=ss_.v(), in1=gsb[s].v(), op0=ALU.mult, op1=ALU.mult)
                I('dve', 'tensor_tensor', out=hb_.v(), in0=hf.v(), in1=shb[s].v(), op=ALU.add)
                for c in range(8):
                    I('pe', 'transpose', out=pt_[:, c * 128:(c + 1) * 128], in_=hb_[:, c * 128:(c + 1) * 128], identity=identb.v())
                I('act', 'activation', out=hT_.v().rearrange("p c t -> p (c t)"), in_=pt_.v(), func=AF.Copy)

            def p2_back(tt):
                hT_, plt_ = hT[tt % 2], plt[tt % 3]
                for n in range(6):
                    n0 = n * 512
                    wdt = min(512, D_IN - n0)
                    p = po[kctr[0] % 4]
                    kctr[0] += 1
                    for c in range(8):
                        I('pe', 'matmul', out=p[:, 0:wdt], lhsT=hT_[:, c, :], rhs=win[:, c, n0:n0 + wdt], start=(c == 0), stop=(c == 7))
                    if n == 0:
                        I('act', 'activation', out=plt_[:, n0:n0 + wdt], in_=p[:, 0:wdt], func=AF.Gelu_apprx_tanh)
                    elif n % 2 == 0:
                        I('dve', 'tensor_copy', out=plt_[:, n0:n0 + wdt], in_=p[:, 0:wdt])
                    else:
                        I('act', 'activation', out=plt_[:, n0:n0 + wdt], in_=p[:, 0:wdt], func=AF.Copy)
                mk.dma('sp', pl[tt * 128:(tt + 1) * 128, :], plt_.v())

            for tt in range(19):
                if tt < 18:
                    p2_front(tt)
                if tt >= 1:
                    p2_back(tt - 1)
        if stop == f"p2_{l}":
            return done()

        with mk.phase():
            wsf = mk.sb("wsf", [128, 4, 128], F32)
            wsT = mk.sb("wsT", [128, 4, 128], BF16)
            bsT = mk.sb("bsT", [128, 4], F32)
            vg = mk.sb("vg", [128, 256], F32)
            pw = mk.ps("pw", [128, 512], F32)
            mk.dma('sp', wsf.v(), gm_ws[l].rearrange("g i j -> i g j"))
            mk.dma('sp', bsT.v(), gm_bsT[l])
            mk.dma('sp', vg.v(), gm_vg[l, :].pbc(128))
            for g in range(4):
                I('pe', 'transpose', out=pw[:, g * 128:(g + 1) * 128], in_=wsf[:, g, :], identity=identf.v())
            I('dve', 'tensor_copy', out=wsT.v().rearrange("p g i -> p (g i)"), in_=pw.v())
            guall = mk.sb("guall", [128, n_tiles, 512], F32)
            for t0_ in range(0, n_tiles, 6):
                t1_ = min(n_tiles, t0_ + 6)
                mk.dma('sp', guall[:, t0_:t1_, :], pl[t0_ * 128:t1_ * 128, 0:512].rearrange("(t p) n -> p t n", p=128))
            st4 = [mk.sb(f"st4{i}", [128, 4], F32) for i in range(2)]
            vr4 = [mk.sb(f"vr4{i}", [128, 4], F32) for i in range(2)]
            vc = mk.sb("vc", [128, 4, 64], F32)
            sq = mk.sb("sq", [128, 4, 64], F32)
            vnb = [mk.sb(f"vnb{i}", [128, 256], BF16) for i in range(2)]
            mo = [mk.sb(f"mo{i}", [128, 256], BF16) for i in range(3)]
            pg = [mk.ps(f"pg{i}", [128, 256], F32) for i in range(2)]
            for tt in range(n_tiles):
                m4, v4, vn_, mo_, pg_ = st4[tt % 2], vr4[tt % 2], vnb[tt % 2], mo[tt % 3], pg[tt % 2]
                gu = guall[:, tt, :]
                v3 = gu[:, 256:512].rearrange("p (g d) -> p g d", d=64)
                I('dve', 'reduce_sum', out=m4.v(), in_=v3, axis=AX.X)
                I('dve', 'tensor_scalar', out=m4.v(), in0=m4.v(), scalar1=-1.0 / 64, scalar2=None, op0=ALU.mult)
                I('dve', 'tensor_tensor', out=vc.v(), in0=v3, in1=m4.v().unsq(2).bc([128, 4, 64]), op=ALU.add)
                I('dve', 'tensor_tensor', out=sq.v(), in0=vc.v(), in1=vc.v(), op=ALU.mult)
                I('dve', 'reduce_sum', out=v4.v(), in_=sq.v(), axis=AX.X)
                rstd_from_ss(v4.v(), 1.0 / 64, LN_EPS, 4)
                I('dve', 'tensor_tensor', out=vc.v(), in0=vc.v(), in1=v4.v().unsq(2).bc([128, 4, 64]), op=ALU.mult)
                I('dve', 'tensor_tensor', out=vn_.v(), in0=vc.v().rearrange("p g d -> p (g d)"), in1=vg.v(), op=ALU.mult)
                for g in range(4):
                    I('pe', 'matmul', out=pg_[:, g * 64:(g + 1) * 64], lhsT=wsT[:, g, :], rhs=vn_[:, g * 64:(g + 1) * 64], start=True, stop=True)
                for g in range(4):
                    I('dve', 'scalar_tensor_tensor', out=mo_[:, g * 64:(g + 1) * 64], in0=pg_[:, g * 64:(g + 1) * 64],
                      scalar=bsT[:, g:g + 1], in1=gu[:, g * 64:(g + 1) * 64], op0=ALU.add, op1=ALU.mult)
                mk.dma('sp', mix[tt * 128:(tt + 1) * 128, 0:256], mo_.v())
        if stop == f"p3_{l}":
            return done()

        with mk.phase():
            qT = mk.sb("qT", [128, 4, NTOK], BF16)
            kT = mk.sb("kT", [128, 4, NTOK], BF16)
            with mk.phase():
                gqk = mk.sb("gqk", [128, 16, 64], F32)
                mk.dma('sp', gqk[:, 0:8, :], na_qg.custom(l * 64, [[0, 128], [0, 8], [1, 64]]))
                mk.dma('sp', gqk[:, 8:16, :], na_kg.custom(l * 64, [[0, 128], [0, 8], [1, 64]]))
                I('dve', 'tensor_scalar', out=gqk[:, 0:8, :], in0=gqk[:, 0:8, :], scalar1=DH ** -0.5, scalar2=None, op0=ALU.mult)
                qkv = [mk.sb(f"qkv{i}", [128, 1536], F32) for i in range(2)]
                sq16 = mk.sb("sq16", [128, 16, 64], F32)
                r16 = [mk.sb(f"r16{i}", [128, 16], F32) for i in range(2)]
                nb = [mk.sb(f"nb{i}", [128, 1024], BF16) for i in range(2)]
                vb = [mk.sb(f"vb{i}", [128, 8, 65], BF16) for i in range(3)]
                ptq = [mk.ps(f"ptq{i}", [128, 1024], BF16) for i in range(2)]
                for i in range(3):
                    I('dve', 'memset', ap=vb[i][:, :, 64:65], constant=1.0)

                def p4_front(tt):
                    t_, r_, nb_ = qkv[tt % 2], r16[tt % 2], nb[tt % 2]
                    mk.dma('sp', t_.v(), pl[tt * 128:(tt + 1) * 128, 512:2048])
                    t3 = t_[:, 0:1024].rearrange("p (h d) -> p h d", d=64)
                    I('pool', 'tensor_tensor', out=sq16.v(), in0=t3, in1=t3, op=ALU.mult)
                    I('dve', 'reduce_sum', out=r_.v(), in_=sq16.v(), axis=AX.X)
                    rstd_from_ss(r_.v(), 1.0 / DH, RMS_EPS, 16)
                    I('dve', 'tensor_tensor', out=sq16.v(), in0=t3, in1=r_.v().unsq(2).bc([128, 16, 64]), op=ALU.mult)
                    I('dve', 'tensor_tensor', out=nb_.v().rearrange("p (h d) -> p h d", d=64), in0=sq16.v(), in1=gqk.v(), op=ALU.mult)

                def p4_back(tt):
                    t_, nb_, p_, vb_ = qkv[tt % 2], nb[tt % 2], ptq[tt % 2], vb[tt % 3]
                    for c in range(8):
                        I('pe', 'transpose', out=p_[:, c * 128:(c + 1) * 128], in_=nb_[:, c * 128:(c + 1) * 128], identity=identb.v())
                    I('act', 'activation', out=qT[:, :, tt * 128:(tt + 1) * 128], in_=p_[:, 0:512].rearrange("p (c t) -> p c t", t=128), func=AF.Copy)
                    I('act', 'activation', out=kT[:, :, tt * 128:(tt + 1) * 128], in_=p_[:, 512:1024].rearrange("p (c t) -> p c t", t=128), func=AF.Copy)
                    I('act', 'activation', out=vb_[:, :, 0:64], in_=t_[:, 1024:1536].rearrange("p (h d) -> p h d", d=64), func=AF.Copy)
                    mk.dma('sp', vd[tt * 128:(tt + 1) * 128, :], vb_.v().rearrange("p h d -> p (h d)"))

                for tt in range(19):
                    if tt < 18:
                        p4_front(tt)
                    if tt >= 1:
                        p4_back(tt - 1)
            vctx = mk.sb("vctx", [128, 2, NH, 65], BF16)
            mk.dma('sp', vctx.v().rearrange("p c h d -> p c (h d)"), vd[S:NTOK, :].rearrange("(c p) n -> p c n", p=128))
            btb = [mk.sb(f"bt{i}", [128, NH, 4, 128], F32) for i in range(2)]
            vwb = [mk.sb(f"vw{i}", [128, 4, NH, 65], BF16) for i in range(2)]
            obb = [mk.sb(f"ob{i}", [128, 512], BF16) for i in range(2)]
            kbb = [mk.sb(f"kb{i}", [128, 4, 512], BF16) for i in range(2)]
            qbb = [mk.sb(f"qb{i}", [128, 4, 128], BF16) for i in range(2)]
            pTb = [mk.sb(f"pT{i}", [128, 768], BF16) for i in range(3)]
            sTb = [mk.sb(f"sT{i}", [128, 512], F32) for i in range(3)]
            rcp = [mk.sb(f"rcp{i}", [128, 1], F32) for i in range(2)]
            psw = [mk.ps(f"psw{i}", [128, 512], F32) for i in range(3)]
            psc = [mk.ps(f"psc{i}", [128, 256], F32) for i in range(3)]
            pov = [mk.ps(f"pov{i}", [128, 65], F32) for i in range(2)]
            info = _na_tables()
            nblk = 16 + (0 if last else 2)
            vmap = {0: 0, 1: 1, 2: 1, 3: 2}
            border = sorted(range(16), key=lambda b: (vmap[b // 4], vmap[b % 4], b)) + list(range(16, nblk))
            units = [(k, h) for k in range(nblk) for h in range(NH)]
            bias_buf = {}
            nbias = [0]

            def variant(blk):
                return (vmap[blk // 4], vmap[blk % 4])

            def load_block(k):
                blk = border[k]
                r0, c0, qr0, qc0 = info[blk][:4]
                vw = vwb[k % 2]
                if k == 0 or variant(blk) != variant(border[k - 1]):
                    nbias[0] += 1
                    bt = btb[nbias[0] % 2]
                    mk.dma('sp', bt.v().rearrange("p h c q -> p (h c q)"), na_bias[l, blk])
                    bias_buf[k] = bt
                else:
                    bias_buf[k] = bias_buf[k - 1]
                vd3 = vd.v().rearrange("(r c) n -> r c n", c=GRID_W)[r0:r0 + 16, c0:c0 + 32, :]
                vd4 = vd3.rearrange("(ch rr) x n -> rr x ch n", rr=4)
                for rr in range(4):
                    mk.dma('sp', vw[rr * 32:(rr + 1) * 32, :, :, :].rearrange("p c h d -> p c (h d)"), vd4[rr], nowaw=(rr > 0))
                kb, qb = kbb[k % 2], qbb[k % 2]
                for hp_ in range(4):
                    k3 = kT[:, hp_, 0:S].rearrange("p (r c) -> p r c", c=GRID_W)[:, r0:r0 + 16, c0:c0 + 32]
                    if hp_ < 2:
                        I('pool', 'tensor_copy', out=kb[:, hp_, :].rearrange("p (r c) -> p r c", c=32), in_=k3)
                    else:
                        I('act', 'activation', out=kb[:, hp_, :].rearrange("p (r c) -> p r c", c=32), in_=k3, func=AF.Copy)
                q4 = qT[:, :, 0:S].rearrange("p h (r c) -> p h r c", c=GRID_W)[:, :, qr0:qr0 + 8, qc0:qc0 + 16]
                I('pool', 'tensor_copy', out=qb.v().rearrange("p h (r c) -> p h r c", c=16), in_=q4)

            def front(i):
                k, h = units[i]
                blk = border[k]
                hp, base = h // 2, (h % 2) * 64
                pw_, pc_, pT_, sT_ = psw[i % 3], psc[i % 3], pTb[i % 3], sTb[i % 3]
                if blk < 16:
                    kb, qb, bt = kbb[k % 2], qbb[k % 2], bias_buf[k]
                    q3 = qb[base:base + 64, hp, :]
                    for c in range(4):
                        I('pe', 'matmul', out=pw_[:, c * 128:(c + 1) * 128], lhsT=kb[base:base + 64, hp, c * 128:(c + 1) * 128], rhs=q3, start=True, stop=True)
                    for c in range(2):
                        I('pe', 'matmul', out=pc_[:, c * 128:(c + 1) * 128], lhsT=kT[base:base + 64, hp, S + c * 128:S + (c + 1) * 128], rhs=q3, start=True, stop=True)
                    I('dve', 'tensor_tensor', out=sT_.v(), in0=pw_.v(), in1=bt[:, h, :, :].rearrange("p c q -> p (c q)"), op=ALU.add)
                    I('act', 'activation', out=pT_[:, 0:512], in_=sT_.v(), func=AF.Exp)
                    I('act', 'activation', out=pT_[:, 512:768], in_=pc_.v(), func=AF.Exp)
                else:
                    qt = blk - 16
                    q2 = qT[base:base + 64, hp, S + qt * 128:S + (qt + 1) * 128]
                    for c in range(2):
                        I('pe', 'matmul', out=pc_[:, c * 128:(c + 1) * 128], lhsT=kT[base:base + 64, hp, S + c * 128:S + (c + 1) * 128],
                          rhs=q2, start=True, stop=True)
                    I('act', 'activation', out=pT_[:, 512:768], in_=pc_.v(), func=AF.Exp)

            def back(i):
                k, h = units[i]
                blk = border[k]
                po_, pT_, rc_, ob = pov[i % 2], pTb[i % 3], rcp[i % 2], obb[k % 2]
                if blk < 16:
                    vw = vwb[k % 2]
                    for c in range(6):
                        rhs = vw[:, c, h, :] if c < 4 else vctx[:, c - 4, h, :]
                        I('pe', 'matmul', out=po_.v(), lhsT=pT_[:, c * 128:(c + 1) * 128], rhs=rhs, start=(c == 0), stop=(c == 5))
                else:
                    for c in range(2):
                        I('pe', 'matmul', out=po_.v(), lhsT=pT_[:, 512 + c * 128:512 + (c + 1) * 128], rhs=vctx[:, c, h, :], start=(c == 0), stop=(c == 1))
                I('dve', 'reciprocal', out=rc_.v(), in_=po_[:, 64:65])
                I('dve', 'tensor_scalar', out=ob[:, h * 64:(h + 1) * 64], in0=po_[:, 0:64], scalar1=rc_.v(), scalar2=None, op0=ALU.mult)
                if h == NH - 1:
                    if blk < 16:
                        qr0, qc0 = info[blk][2:4]
                        for a in range(8):
                            t0 = (qr0 + a) * GRID_W + qc0
                            mk.dma('sp', mix[t0:t0 + 16, 256:768], ob[a * 16:(a + 1) * 16, :], force=True, nowaw=(a > 0))
                    else:
                        t0 = S + (blk - 16) * 128
                        mk.dma('sp', mix[t0:t0 + 128, 256:768], ob.v(), force=True)

            load_block(0)
            for i in range(len(units) + 2):
                if i < len(units):
                    front(i)
                if i >= 2:
                    back(i - 2)
                if i < len(units):
                    k, h = units[i]
                    if h == 2 and k + 1 < 16:
                        load_block(k + 1)
        if stop == f"p5_{l}":
            return done()

        streams = [dict(L=S, row0=0, tag='l')]
        if not last:
            streams.append(dict(L=CTX, row0=S, tag='c'))
        for st in streams:
            st['nT'] = st['L'] // 128
        with mk.phase():
            for st in streams:
                st['Kr'] = mk.sb("Kr" + st['tag'], [128, st['nT'], 512], BF16)
                st['Ks'] = mk.sb("Ks" + st['tag'], [128, st['nT'], 512], BF16)
            with mk.phase():
                w1 = mk.sb("w1", [33, 64], F32)
                w2 = mk.sb("w2", [64, 64], F32)
                w3 = mk.sb("w3", [64, 1024], F32)
                fb = mk.sb("fb", [64, 4], F32)
                sc = mk.sb("sc", [64, 2], F32)
                of = mk.sb("of", [64, 2], F32)
                mk.dma('sp', w1.v(), hy_w1[l])
                mk.dma('sp', w2.v(), hy_w2[l])
                mk.dma('sp', w3.v(), hy_w3[l])
                mk.dma('sp', fb.v(), hy_fb[l])
                for st in streams:
                    L, nT, tg = st['L'], st['nT'], st['tag']
                    st['posT'] = mk.sb("posT" + tg, [33, L], F32)
                    st['dec'] = mk.sb("dec" + tg, [128, nT, 256], F32)
                    st['h2T'] = mk.sb("h2T" + tg, [64, L], F32)
                    st['Ab'] = mk.sb("Ab" + tg, [128, nT, 512], BF16)
                    st['Bb'] = mk.sb("Bb" + tg, [128, nT, 512], BF16)
                    st['rn'] = mk.sb("rn" + tg, [128, 2, 256], F32)
                    st['pS'] = [mk.ps(f"pS{tg}{i}", [128, 512], F32) for i in range(2)]
                    mk.dma('sp', st['posT'].v(), cst[f'posT{L}'].v())
                    mk.dma('sp', st['dec'].v(), cst[f'decay{L}'].v())
                I('dve', 'tensor_scalar', out=sc.v(), in0=fb[:, 0:2], scalar1=1.0 / 3.0, scalar2=None, op0=ALU.mult)
                I('dve', 'tensor_tensor', out=of.v(), in0=fb[:, 2:4], in1=sc.v(), op=ALU.mult)
                u1b = [mk.sb(f"u1{i}", [64, 512], F32) for i in range(2)]
                u2b = [mk.sb(f"u2{i}", [64, 512], F32) for i in range(2)]
                hrot = [mk.sb(f"hrot{i}", [128, 1024], F32) for i in range(2)]
                habs = [mk.sb(f"habs{i}", [128, 1024], F32) for i in range(2)]
                ph = [mk.ps(f"ph{i}", [128, 512], F32) for i in range(2)]
                p3 = [mk.ps(f"p3{i}", [128, 512], F32) for i in range(2)]
                chunks = []
                for st in streams:
                    CW = min(512, st['L'])
                    for pc in range(st['L'] // CW):
                        chunks.append((st, slice(pc * CW, (pc + 1) * CW), CW))
                h1c = [mk.sb(f"h1c{i}", [64, 512], F32) for i in range(len(chunks))]
                for k_ in range(2):
                    wk = (w1, w2)[k_]
                    for ci, (st, cs, CW) in enumerate(chunks):
                        u1, u2, p_ = u1b[ci % 2], u2b[ci % 2], ph[ci % 2]
                        src = st['posT'][:, cs] if k_ == 0 else h1c[ci][:, 0:CW]
                        dst = h1c[ci][:, 0:CW] if k_ == 0 else st['h2T'][:, cs]
                        I('pe', 'matmul', out=p_[0:64, 0:CW], lhsT=wk.v(), rhs=src, start=True, stop=True)
                        I('act', 'activation', out=u1[:, 0:CW], in_=p_[0:64, 0:CW], func=AF.Sin, bias=of[:, k_:k_ + 1], scale=sc[:, k_:k_ + 1])
                        I('dve', 'tensor_tensor', out=u2[:, 0:CW], in0=u1[:, 0:CW], in1=u1[:, 0:CW], op=ALU.mult)
                        I('dve', 'tensor_scalar', out=u2[:, 0:CW], in0=u2[:, 0:CW], scalar1=-4.0, scalar2=3.0, op0=ALU.mult, op1=ALU.add)
                        I('dve', 'tensor_tensor', out=dst, in0=u1[:, 0:CW], in1=u2[:, 0:CW], op=ALU.mult)
                kq = 0
                for st in streams:
                    nT = st['nT']
                    for tc in range(nT):
                        hr, ha = hrot[kq % 2], habs[kq % 2]
                        kq += 1
                        for hf_ in range(2):
                            p = p3[hf_]
                            I('pe', 'matmul', out=p.v(), lhsT=st['h2T'][:, tc * 128:(tc + 1) * 128], rhs=w3[:, hf_ * 512:(hf_ + 1) * 512], start=True, stop=True)
                            I('dve', 'tensor_tensor', out=hr[:, hf_ * 512:(hf_ + 1) * 512].rearrange("p (a c) -> p a c", c=256),
                              in0=p.v().rearrange("p (a c) -> p a c", c=256), in1=st['dec'][:, tc, :].unsq(1).bc([128, 2, 256]), op=ALU.mult)
                        I('act', 'activation', out=ha.v(), in_=hr.v(), func=AF.Abs)
                        for hf_ in range(2):
                            I('pe', 'matmul', out=st['pS'][hf_].v(), lhsT=onesf.v(), rhs=ha[:, hf_ * 512:(hf_ + 1) * 512], start=(tc == 0), stop=(tc == nT - 1))
                        h4 = hr.v().rearrange("p (o d c) -> p o d c", o=2, d=2)
                        if tc == 0:
                            I('dve', 'tensor_scalar', out=h4[:, :, 1, :], in0=h4[:, :, 1, :], scalar1=pmask.v(), scalar2=None, op0=ALU.mult)
                        I('dve', 'tensor_tensor', out=st['Ab'][:, tc, :].rearrange("p (o c) -> p o c", o=2), in0=h4[:, :, 0, :], in1=h4[:, :, 1, :], op=ALU.add)
                        I('pool', 'tensor_tensor', out=st['Bb'][:, tc, :].rearrange("p (o c) -> p o c", o=2), in0=h4[:, :, 0, :], in1=h4[:, :, 1, :], op=ALU.subtract)
                for st in streams:
                    rn = st['rn']
                    for o in range(2):
                        I('dve', 'tensor_copy', out=rn[:, o, :], in_=st['pS'][o][:, 0:256])
                        I('dve', 'tensor_tensor', out=rn[:, o, :], in0=rn[:, o, :], in1=st['pS'][o][:, 256:512], op=ALU.add)
                    I('dve', 'reciprocal', out=rn.v(), in_=rn.v())
                cfb = [mk.sb(f"cfk{i}", [128, 16, 128], BF16) for i in range(2)]
                sfb = [mk.sb(f"sfk{i}", [128, 16, 128], BF16) for i in range(2)]
                kq = 0
                for st in streams:
                    L, nT = st['L'], st['nT']
                    rnf = st['rn'].v().rearrange("p o c -> p (o c)")
                    for fc in range(nT):
                        cf, sf = cfb[kq % 2], sfb[kq % 2]
                        kq += 1
                        mk.dma('sp', cf[:, 0:nT, :].rearrange("p t f -> p (t f)"), cst[f'Cf{L}'][fc])
                        mk.dma('sp', sf[:, 0:nT, :].rearrange("p t f -> p (t f)"), cst[f'Sf{L}'][fc])
                        for tc in range(nT):
                            I('pe', 'matmul', out=ph[0].v(), lhsT=cf[:, tc, :], rhs=st['Ab'][:, tc, :], start=(tc == 0), stop=(tc == nT - 1))
                        for tc in range(nT):
                            I('pe', 'matmul', out=ph[1].v(), lhsT=sf[:, tc, :], rhs=st['Bb'][:, tc, :], start=(tc == 0), stop=(tc == nT - 1))
                        I('dve', 'tensor_tensor', out=st['Kr'][:, fc, :], in0=ph[0].v(), in1=rnf, op=ALU.mult)
                        I('dve', 'tensor_tensor', out=st['Ks'][:, fc, :], in0=ph[1].v(), in1=rnf, op=ALU.mult)
            swb = mk.sb("swb", [128, 3, 768], F32)
            sbb = mk.sb("sbb", [128, 768], F32)
            dbb = mk.sb("dbb", [128, 512], F32)
            mk.dma('sp', swb.v().rearrange("p k n -> p (k n)"), hy_sw[l].rearrange("k n -> (k n)").pbc(128))
            mk.dma('sp', sbb.v(), hy_sb[l, :].pbc(128))
            mk.dma('sp', dbb.v(), hy_bias[l, :].pbc(128))
            for st in streams:
                nT, tg = st['nT'], st['tag']
                st['zf'] = mk.sb("zf" + tg, [128, nT, 256], F32)
                st['zb'] = mk.sb("zb" + tg, [128, nT, 256], BF16)
                st['gts'] = mk.sb("gts" + tg, [128, nT, 512], F32)
                st['Yr'] = mk.sb("Yr" + tg, [128, nT, 256], BF16)
                st['Ys'] = mk.sb("Ys" + tg, [128, nT, 256], BF16)
            a3 = [mk.sb(f"a3{i}", [128, 3, 768], F32) for i in range(3)]
            accb = [mk.sb(f"acc{i}", [128, 768], F32) for i in range(2)]
            kq = 0
            for st in streams:
                nT, row0 = st['nT'], st['row0']
                for tc in range(nT):
                    a_, acc = a3[kq % 3], accb[kq % 2]
                    kq += 1
                    g0 = row0 + tc * 128
                    mk.dma('sp', a_[:, 1, :], pl[g0:g0 + 128, 2048:2816])
                    if tc == 0:
                        I('dve', 'memset', ap=a_[0:1, 0, :], constant=0.0)
                        mk.dma('sp', a_[1:128, 0, :], pl[g0:g0 + 127, 2048:2816], nowaw=True)
                    else:
                        mk.dma('sp', a_[:, 0, :], pl[g0 - 1:g0 + 127, 2048:2816], nowaw=True)
                    if tc == nT - 1:
                        I('dve', 'memset', ap=a_[:, 2, :], constant=0.0)
                        mk.dma('sp', a_[0:127, 2, :], pl[g0 + 1:g0 + 128, 2048:2816])
                    else:
                        mk.dma('sp', a_[:, 2, :], pl[g0 + 1:g0 + 129, 2048:2816], nowaw=True)
                    I('dve', 'tensor_tensor', out=acc.v(), in0=a_[:, 1, :], in1=swb[:, 1, :], op=ALU.mult)
                    I('dve', 'tensor_tensor', out=acc.v(), in0=acc.v(), in1=sbb.v(), op=ALU.add)
                    I('pool', 'tensor_tensor', out=a_[:, 0, :], in0=a_[:, 0, :], in1=swb[:, 0, :], op=ALU.mult)
                    I('pool', 'tensor_tensor', out=a_[:, 2, :], in0=a_[:, 2, :], in1=swb[:, 2, :], op=ALU.mult)
                    I('dve', 'tensor_tensor', out=acc.v(), in0=acc.v(), in1=a_[:, 0, :], op=ALU.add)
                    I('dve', 'tensor_tensor', out=acc.v(), in0=acc.v(), in1=a_[:, 2, :], op=ALU.add)
                    I('act', 'activation', out=st['zf'][:, tc, :], in_=acc[:, 0:256], func=AF.Copy)
                    I('act', 'activation', out=st['zb'][:, tc, :], in_=acc[:, 0:256], func=AF.Copy)
                    I('act', 'activation', out=st['gts'][:, tc, :], in_=acc[:, 256:768], func=AF.Copy)
            cfb = [mk.sb(f"cf{i}", [128, 16, 128], BF16) for i in range(2)]
            sfb = [mk.sb(f"sf{i}", [128, 16, 128], BF16) for i in range(2)]
            zr = [mk.sb(f"zr{i}", [128, 256], F32) for i in range(2)]
            zs = [mk.sb(f"zs{i}", [128, 256], F32) for i in range(2)]
            t1 = mk.sb("t1", [128, 256], F32)
            t2 = mk.sb("t2", [128, 256], F32)
            t3 = mk.sb("t3", [128, 256], F32)
            t4 = mk.sb("t4", [128, 256], F32)
            dzb = [mk.sb(f"dz{i}", [128, 256], F32) for i in range(2)]
            pz = [mk.ps(f"pz{i}", [128, 256], F32) for i in range(4)]
            py = [mk.ps(f"py{i}", [128, 256], F32) for i in range(2)]
            kq = 0
            for n in range(2):
                ksl = slice(n * 256, (n + 1) * 256)
                for st in streams:
                    L, nT = st['L'], st['nT']
                    Kr, Ks, zb, Yr, Ys = st['Kr'], st['Ks'], st['zb'], st['Yr'], st['Ys']
                    for fc in range(nT):
                        cf, sf = cfb[kq % 2], sfb[kq % 2]
                        pzr, pzs = pz[(kq % 2) * 2], pz[(kq % 2) * 2 + 1]
                        zr_, zs_ = zr[kq % 2], zs[kq % 2]
                        kq += 1
                        mk.dma('sp', cf[:, 0:nT, :].rearrange("p t f -> p (t f)"), cst[f'Cf{L}'][fc])
                        mk.dma('sp', sf[:, 0:nT, :].rearrange("p t f -> p (t f)"), cst[f'Sf{L}'][fc])
                        for tc in range(nT):
                            I('pe', 'matmul', out=pzr.v(), lhsT=cf[:, tc, :], rhs=zb[:, tc, :], start=(tc == 0), stop=(tc == nT - 1))
                        for tc in range(nT):
                            I('pe', 'matmul', out=pzs.v(), lhsT=sf[:, tc, :], rhs=zb[:, tc, :], start=(tc == 0), stop=(tc == nT - 1))
                        I('act', 'activation', out=zr_.v(), in_=pzr.v(), func=AF.Copy)
                        I('act', 'activation', out=zs_.v(), in_=pzs.v(), func=AF.Copy)
                        I('pool', 'tensor_tensor', out=t1.v(), in0=zr_.v(), in1=Kr[:, fc, ksl], op=ALU.mult)
                        I('pool', 'tensor_tensor', out=t2.v(), in0=zs_.v(), in1=Ks[:, fc, ksl], op=ALU.mult)
                        I('pool', 'tensor_tensor', out=Yr[:, fc, :], in0=t1.v(), in1=t2.v(), op=ALU.subtract)
                        I('dve', 'tensor_tensor', out=t3.v(), in0=zr_.v(), in1=Ks[:, fc, ksl], op=ALU.mult)
                        I('dve', 'tensor_tensor', out=t4.v(), in0=zs_.v(), in1=Kr[:, fc, ksl], op=ALU.mult)
                        I('dve', 'tensor_tensor', out=Ys[:, fc, :], in0=t3.v(), in1=t4.v(), op=ALU.add)
                for st in streams:
                    L, nT = st['L'], st['nT']
                    zf, zb, gts, Yr, Ys = st['zf'], st['zb'], st['gts'], st['Yr'], st['Ys']
                    for tc in range(nT):
                        ci, si = cfb[kq % 2], sfb[kq % 2]
                        p, dz = py[kq % 2], dzb[kq % 2]
                        kq += 1
                        mk.dma('sp', ci[:, 0:nT, :].rearrange("p t f -> p (t f)"), cst[f'Ci{L}'][tc])
                        mk.dma('sp', si[:, 0:nT, :].rearrange("p t f -> p (t f)"), cst[f'Si{L}'][tc])
                        for fc in range(nT):
                            I('pe', 'matmul', out=p.v(), lhsT=ci[:, fc, :], rhs=Yr[:, fc, :], start=(fc == 0), stop=False)
                        for fc in range(nT):
                            I('pe', 'matmul', out=p.v(), lhsT=si[:, fc, :], rhs=Ys[:, fc, :], start=False, stop=(fc == nT - 1))
                        I('pool', 'tensor_tensor', out=dz.v(), in0=zf[:, tc, :], in1=dbb[:, ksl], op=ALU.mult)
                        I('dve', 'scalar_tensor_tensor', out=zf[:, tc, :], in0=p.v(), scalar=1.0 / L, in1=dz.v(), op0=ALU.mult, op1=ALU.add)
                        I('dve', 'tensor_tensor', out=zf[:, tc, :], in0=zf[:, tc, :], in1=gts[:, tc, ksl], op=ALU.mult)
                        I('act', 'activation', out=zb[:, tc, :], in_=zf[:, tc, :], func=AF.Copy)
            for st in streams:
                L, row0 = st['L'], st['row0']
                mk.dma('sp', mix[row0:row0 + L, 768:1024].rearrange("(t p) c -> p t c", p=128), st['zb'].v())
        if stop == f"p6_{l}":
            return done()

        with mk.phase():
            wo = mk.sb("wo", [128, 8, D], BF16)
            wst = [mk.sb(f"wst{i}", [128, 2, D], F32) for i in range(2)]
            for c2 in range(4):
                mk.dma('sp', wst[c2 % 2].v(), w_out[l, c2 * 256:(c2 + 1) * 256, :].rearrange("(c p) n -> p c n", p=128))
                if c2 % 2 == 0:
                    I('act', 'activation', out=wo[:, c2 * 2:(c2 + 1) * 2, :], in_=wst[c2 % 2].v(), func=AF.Copy)
                else:
                    I('dve', 'tensor_copy', out=wo[:, c2 * 2:(c2 + 1) * 2, :], in_=wst[c2 % 2].v())
            g2 = [mk.sb(f"g2{s}", [128, D], F32) for s in range(2)]
            for s in range(2):
                load_mod(g2[s].v(), s, 2)
            xb = [mk.sb(f"xb{i}", [128, D], F32) for i in range(4)]
            mb = [mk.sb(f"mb{i}", [128, D], BF16) for i in range(4)]
            mT = [mk.sb(f"mT{i}", [128, 8, 128], BF16) for i in range(2)]
            tmp = mk.sb("tmp", [128, D], F32)
            pt = [mk.ps(f"pt{i}", [128, 1024], BF16) for i in range(2)]
            po = [mk.ps(f"po{i}", [128, 512], F32) for i in range(4)]
            def p7_front(tt):
                x_, m_, mT_, pt_ = xb[tt % 4], mb[tt % 4], mT[tt % 2], pt[tt % 2]
                mk.dma('sp', x_.v(), xsrc[tt * 128:(tt + 1) * 128, :])
                mk.dma('sp', m_.v(), mix[tt * 128:(tt + 1) * 128, :])
                for c in range(8):
                    I('pe', 'transpose', out=pt_[:, c * 128:(c + 1) * 128], in_=m_[:, c * 128:(c + 1) * 128], identity=identb.v())
                I('act', 'activation', out=mT_.v().rearrange("p c t -> p (c t)"), in_=pt_.v(), func=AF.Copy)

            def p7_back(tt):
                s = 0 if tt < 16 else 1
                x_, mT_ = xb[tt % 4], mT[tt % 2]
                for hf_ in range(2):
                    p = po[(tt % 2) * 2 + hf_]
                    for c in range(8):
                        I('pe', 'matmul', out=p.v(), lhsT=mT_[:, c, :], rhs=wo[:, c, hf_ * 512:(hf_ + 1) * 512], start=(c == 0), stop=(c == 7))
                    hs = slice(hf_ * 512, (hf_ + 1) * 512)
                    I('dve', 'tensor_tensor', out=tmp[:, hs], in0=p.v(), in1=g2[s][:, hs], op=ALU.mult)
                    I('dve', 'tensor_tensor', out=x_[:, hs], in0=x_[:, hs], in1=tmp[:, hs], op=ALU.add)
                mk.dma('sp', xs[tt * 128:(tt + 1) * 128, :], x_.v())

            for tt in range(n_tiles + 1):
                if tt < n_tiles:
                    p7_front(tt)
                if tt >= 1:
                    p7_back(tt - 1)
        if stop == f"p7_{l}":
            return done()

        ntok = n_tiles * 128
        with mk.phase():
            hTa = mk.sb("hTa", [128, 8, ntok], BF16)
            oacc = mk.sb("oacc", [128, n_tiles, D], F32)
            with mk.phase():
                gsb = [mk.sb(f"gsb{s}", [128, D], F32) for s in range(2)]
                shb = [mk.sb(f"shb{s}", [128, D], F32) for s in range(2)]
                for s in range(2):
                    load_mod(gsb[s].v(), s, 4)
                    load_mod(shb[s].v(), s, 3)
                wr = mk.sb("wr", [128, 8, 36], F32)
                brb = mk.sb("brb", [128, 36], F32)
                mk.dma('sp', wr.v(), moe_wr[l].rearrange("(c p) n -> p c n", p=128))
                mk.dma('sp', brb.v(), moe_br[l, :].pbc(128))
                xb = [mk.sb(f"xb{i}", [128, D], F32) for i in range(2)]
                junk = mk.sb("junk", [128, D], BF16)
                ssb = [mk.sb(f"ss{i}", [128, 1], F32) for i in range(2)]
                hf = [mk.sb(f"hf{i}", [128, D], F32) for i in range(2)]
                hTf = [mk.sb(f"hTf{i}", [128, 8, 128], F32) for i in range(2)]
                ptf = [mk.ps(f"ptf{i}", [128, 512], F32) for i in range(4)]
                pr = [mk.ps(f"pr{i}", [128, 36], F32) for i in range(2)]
                pct = [mk.ps(f"pct{i}", [32, 128], F32) for i in range(2)]
                combT = mk.sb("combT", [32, ntok], BF16)
                lgall = mk.sb("lgall", [128, n_tiles, 36], F32)
                cmball = mk.sb("cmball", [128, n_tiles, 32], F32)
                def p8_front(tt):
                    s = 0 if tt < 16 else 1
                    i2 = tt % 2
                    x_, ss_, hf_, hTf_ = xb[i2], ssb[i2], hf[i2], hTf[i2]
                    mk.dma('sp', x_.v(), xs[tt * 128:(tt + 1) * 128, :])
                    I('act', 'activation', out=junk.v(), in_=x_.v(), func=AF.Square, accum_out=ss_.v())
                    rstd_from_ss(ss_.v(), 1.0 / D, RMS_EPS, 1)
                    I('dve', 'scalar_tensor_tensor', out=hf_.v(), in0=x_.v(), scalar=ss_.v(), in1=gsb[s].v(), op0=ALU.mult, op1=ALU.mult)
                    I('pool', 'tensor_tensor', out=hf_.v(), in0=hf_.v(), in1=shb[s].v(), op=ALU.add)
                    for hh in range(2):
                        p = ptf[i2 * 2 + hh]
                        for c in range(4):
                            cc = hh * 4 + c
                            I('pe', 'transpose', out=p[:, c * 128:(c + 1) * 128], in_=hf_[:, cc * 128:(cc + 1) * 128], identity=identf.v())
                        I('act', 'activation', out=hTf_[:, hh * 4:(hh + 1) * 4, :].rearrange("p c t -> p (c t)"), in_=p.v(), func=AF.Copy)
                        I('dve', 'tensor_copy', out=hTa[:, hh * 4:(hh + 1) * 4, tt * 128:(tt + 1) * 128],
                          in_=hTf_[:, hh * 4:(hh + 1) * 4, :])
                def p8_back(tt):
                    i2 = tt % 2
                    hTf_ = hTf[i2]
                    pr_ = pr[i2]
                    for c in range(8):
                        I('pe', 'matmul', out=pr_.v(), lhsT=hTf_[:, c, :], rhs=wr[:, c, :], start=(c == 0), stop=(c == 7))
                    I('dve', 'tensor_tensor', out=lgall[:, tt, :], in0=pr_.v(), in1=brb.v(), op=ALU.add)
                for tt in range(n_tiles + 1):
                    if tt < n_tiles:
                        p8_front(tt)
                    if tt >= 1:
                        p8_back(tt - 1)
                routing_batched(mk, lgall.v(), n_tiles, cmball.v())
                for tt in range(n_tiles):
                    pc_ = pct[tt % 2]
                    I('pe', 'transpose', out=pc_.v(), in_=cmball[:, tt, :], identity=identf.v())
                    I('act', 'activation', out=combT[:, tt * 128:(tt + 1) * 128], in_=pc_.v(), func=AF.Copy)
                mk.dma('sp', cbd[:, 0:ntok], combT.v())
            with mk.phase():
                wgb = [mk.sb(f"wg{i}", [128, 2, 8, FH], BF16) for i in range(2)]
                wub = [mk.sb(f"wu{i}", [128, 2, 8, FH], BF16) for i in range(2)]
                wdb = [mk.sb(f"wd{i}", [128, 2, 2, D], BF16) for i in range(2)]
                actb = [mk.sb(f"act{i}", [128, 2, 2, 512], BF16) for i in range(2)]
                sgb = [mk.sb(f"sg{i}", [128, 512], F32) for i in range(2)]
                tb = [mk.sb(f"tb{i}", [128, 512], F32) for i in range(2)]
                pgp = [mk.ps(f"pgp{i}", [128, 512], F32) for i in range(2)]
                pup = [mk.ps(f"pup{i}", [128, 512], F32) for i in range(2)]
                cbb = [mk.sb(f"cbb{i}", [128, ntok], BF16) for i in range(3)]
                pdp = [mk.ps(f"pdp{i}", [128, 512], F32) for i in range(4)]
                groups = [(g0, min(512, ntok - g0)) for g0 in range(0, ntok, 512)]
                g5 = [mk.sb(f"g5{s}", [128, D], F32) for s in range(2)]
                for s in range(2):
                    load_mod(g5[s].v(), s, 5)
                xrb = [mk.sb(f"xr{i}", [128, D], F32) for i in range(2)]
                res_dst = out if (l == n_layers - 1 and not debug) or last else xs

                def res_load(tt):
                    if tt < n_tiles:
                        mk.dma('sp', xrb[tt % 2].v(), xs[tt * 128:(tt + 1) * 128, :])

                def res_finish(tt):
                    s = 0 if tt < 16 else 1
                    x_ = xrb[tt % 2]
                    I('dve', 'tensor_tensor', out=oacc[:, tt, :], in0=oacc[:, tt, :], in1=g5[s].v(), op=ALU.mult)
                    I('dve', 'tensor_tensor', out=x_.v(), in0=x_.v(), in1=oacc[:, tt, :], op=ALU.add)
                    if not (res_dst is out and tt >= 16):
                        mk.dma('sp', res_dst[tt * 128:(tt + 1) * 128, :], x_.v())
                kk = 0
                kd = 0
                gi = 0
                for pair in range(0 if stop == f"p8a_{l}" else NEXP // 2):
                    wg, wu, wd = wgb[pair % 2], wub[pair % 2], wdb[pair % 2]
                    for e in range(2):
                        ge = pair * 2 + e
                        mk.dma('sp', cbb[ge % 3].v(), cbd[ge, 0:ntok].pbc(128))
                        mk.dma('pool', wg[:, e, :, :], moe_wg[l, ge].rearrange("(c p) f -> p c f", p=128))
                        mk.dma('pool', wu[:, e, :, :], moe_wu[l, ge].rearrange("(c p) f -> p c f", p=128))
                        mk.dma('pool', wd[:, e, :, :], moe_wd[l, ge].rearrange("(c p) n -> p c n", p=128))
                    last_pair = (pair == NEXP // 2 - 1)
                    if last_pair:
                        res_load(0)
                    for (g0, gw) in groups:
                        at = actb[gi % 2]
                        gi += 1
                        for e in range(2):
                            ge = pair * 2 + e
                            for fc in range(2):
                                pg_, pu_, sg_, tb_ = pgp[kk % 2], pup[kk % 2], sgb[kk % 2], tb[kk % 2]
                                cb_ = cbb[ge % 3]
                                kk += 1
                                for c in range(8):
                                    I('pe', 'matmul', out=pg_[:, 0:gw], lhsT=wg[:, e, c, fc * 128:(fc + 1) * 128], rhs=hTa[:, c, g0:g0 + gw], start=(c == 0), stop=(c == 7))
                                for c in range(8):
                                    I('pe', 'matmul', out=pu_[:, 0:gw], lhsT=wu[:, e, c, fc * 128:(fc + 1) * 128], rhs=hTa[:, c, g0:g0 + gw], start=(c == 0), stop=(c == 7))
                                I('act', 'activation', out=sg_[:, 0:gw], in_=pg_[:, 0:gw], func=AF.Silu)
                                I('dve', 'tensor_tensor', out=tb_[:, 0:gw], in0=sg_[:, 0:gw], in1=pu_[:, 0:gw], op=ALU.mult)
                                I('dve', 'tensor_tensor', out=at[:, e, fc, 0:gw], in0=tb_[:, 0:gw], in1=cb_[:, g0:g0 + gw], op=ALU.mult)
                        for ti in range(gw // 128):
                            tt = g0 // 128 + ti
                            if last_pair:
                                res_load(tt + 1)
                            for hf_ in range(2):
                                pd_ = pdp[kd % 4]
                                kd += 1
                                hs = slice(hf_ * 512, (hf_ + 1) * 512)
                                n_ = 0
                                for e in range(2):
                                    for fc in range(2):
                                        I('pe', 'matmul', out=pd_.v(), lhsT=at[:, e, fc, ti * 128:(ti + 1) * 128], rhs=wd[:, e, fc, hs], start=(n_ == 0), stop=(n_ == 3))
                                        n_ += 1
                                if pair == 0:
                                    I('dve', 'tensor_copy', out=oacc[:, tt, hs], in_=pd_.v())
                                else:
                                    I('dve', 'tensor_tensor', out=oacc[:, tt, hs], in0=oacc[:, tt, hs], in1=pd_.v(), op=ALU.add)
                            if last_pair:
                                res_finish(tt)
        if stop in (f"p8_{l}", f"p8a_{l}"):
            return done()
    return done()


def make_in_maps(inputs):
    c = _consts()
    f32 = np.float32
    x = np.asarray(inputs['x'], f32)
    B = x.shape[0]
    ctx = np.asarray(inputs['ctx'], f32)
    cc = np.asarray(inputs['c'], f32)
    c_ctx = np.asarray(inputs['c_ctx'], f32)
    info = _na_tables()
    rpb = np.asarray(inputs['na_rpb'], f32)
    nab = np.empty((DEPTH, 16, 128, NH, 4, 128), f32)
    for b_, (r0, c0, qr0, qc0, mask, dr, dc) in enumerate(info):
        g = rpb[:, :, dr, dc]
        g = np.where(mask[None, None], g, f32(NEG_MASK))
        nab[:, b_] = g.transpose(0, 3, 1, 2, 4)
    nab = nab.reshape(DEPTH, 16, 128, NH * 4 * 128)
    fb = np.stack([np.asarray(inputs['hy_freq'], f32)[:, 0], np.asarray(inputs['hy_freq'], f32)[:, 1],
                   np.asarray(inputs['hy_b1'], f32), np.asarray(inputs['hy_b2'], f32)], axis=-1)
    shared = {
        'w_ada': np.asarray(inputs['w_ada'], f32), 'b_ada': np.asarray(inputs['b_ada'], f32),
        'g_mix': np.asarray(inputs['g_mix'], f32), 'g_ffn': np.asarray(inputs['g_ffn'], f32),
        'w_in': np.asarray(inputs['w_in'], f32), 'w_out': np.asarray(inputs['w_out'], f32),
        'gmlp_v_gain': np.asarray(inputs['gmlp_v_gain'], f32), 'gmlp_ws': np.asarray(inputs['gmlp_ws'], f32),
        'gmlp_bsT': np.ascontiguousarray(np.asarray(inputs['gmlp_bs'], f32).transpose(0, 2, 1)),
        'na_q_gain': np.asarray(inputs['na_q_gain'], f32), 'na_k_gain': np.asarray(inputs['na_k_gain'], f32),
        'na_bias': nab,
        'hy_short_w': np.asarray(inputs['hy_short_w'], f32), 'hy_short_b': np.asarray(inputs['hy_short_b'], f32),
        'hy_w1': np.asarray(inputs['hy_w1'], f32), 'hy_w2': np.asarray(inputs['hy_w2'], f32), 'hy_w3': np.asarray(inputs['hy_w3'], f32),
        'hy_fb': np.ascontiguousarray(fb), 'hy_bias': np.asarray(inputs['hy_bias'], f32).reshape(DEPTH, 512),
        'moe_wr': np.ascontiguousarray(np.concatenate([np.asarray(inputs['moe_w_rg'], f32), np.asarray(inputs['moe_w_re'], f32)], axis=-1)),
        'moe_br': np.ascontiguousarray(np.concatenate([np.asarray(inputs['moe_b_rg'], f32), np.asarray(inputs['moe_b_re'], f32)], axis=-1)),
        'moe_w_gate': np.asarray(inputs['moe_w_gate'], f32).reshape(DEPTH, NEXP, D, FH),
        'moe_w_up': np.asarray(inputs['moe_w_up'], f32).reshape(DEPTH, NEXP, D, FH),
        'moe_w_down': np.asarray(inputs['moe_w_down'], f32).reshape(DEPTH, NEXP, FH, D),
        'ident': c['ident'],
    }
    for L in (S, CTX):
        nT = L // 128
        shared[f'posT{L}'] = c[f'posT{L}']
        shared[f'decay{L}'] = c[f'decay{L}']
        for nm in ('Cf', 'Sf', 'Ci', 'Si'):
            shared[f'{nm}{L}'] = c[f'{nm}{L}'].reshape(nT, 128, nT * 128)
    maps = []
    for b in range(B):
        m = dict(shared)
        m['xin'] = np.ascontiguousarray(np.concatenate([x[b], ctx[b]], axis=0))
        cT = np.concatenate([cc[b].reshape(8, 128).T, c_ctx.reshape(8, 128).T], axis=1)
        m['cT'] = np.ascontiguousarray(cT)
        maps.append(m)
    return maps


_PROG = {}


def kernel(**inputs):
    if 'nc' not in _PROG:
        _PROG['nc'] = build_program()[0]
    maps = make_in_maps(inputs)
    res = run_bass_kernel_spmd(_PROG['nc'], maps, core_ids=list(range(len(maps))))
    return np.stack([np.asarray(r['out'], np.float32) for r in res.results], axis=0)
```

```python
import math
import os
import numpy as np
import ml_dtypes
import concourse.bass as bass
import concourse.mybir as mybir
from concourse.bass_utils import run_bass_kernel_spmd
from contextlib import ExitStack, contextmanager

F32 = mybir.dt.float32
BF16 = mybir.dt.bfloat16
ALU = mybir.AluOpType
AF = mybir.ActivationFunctionType
AX = mybir.AxisListType

ENGS = ('pe', 'act', 'dve', 'pool', 'sp')
WRITE_KW = ('out', 'accum_out', 'ap')


class Buf:
    def __init__(self, mk, t, name, kind):
        self.mk, self.t, self.name, self.kind = mk, t, name, kind
        self.last_w = None
        self.readers = []
        self.sems = {}
        self.lastw_kind = {}

    view = None

    def __getitem__(self, idx):
        return self.v()[idx]

    def v(self):
        if self.view is not None:
            return V(self, self.t[0:self.view[0], 0:self.view[1]])
        return V(self, self.t[:])

    def custom(self, offset, ap):
        return V(self, bass.AP(self.t, offset, ap))


class V:
    def __init__(self, buf, ap):
        self.buf, self.ap = buf, ap

    def __getitem__(self, idx):
        return V(self.buf, self.ap[idx])

    def rearrange(self, pat, **kw):
        return V(self.buf, self.ap.rearrange(pat, **kw))

    def bc(self, shape):
        return V(self.buf, self.ap.to_broadcast(list(shape)))

    def unsq(self, axis):
        return V(self.buf, self.ap.unsqueeze(axis))

    def pbc(self, n):
        return V(self.buf, self.ap.partition_broadcast(n))


class Ins:
    __slots__ = ('eng', 'fn', 'deps', 'is_dma', 'sem', 'semval', 'signals', 'sigidx', 'nowaw')

    def __init__(self, eng, fn, is_dma=False):
        self.eng, self.fn, self.is_dma = eng, fn, is_dma
        self.deps = []
        self.sem = None
        self.semval = 0
        self.signals = False
        self.sigidx = None
        self.nowaw = False


class SemSlot:
    def __init__(self, sem, kind):
        self.sem = sem
        self.kind = kind
        self.cnt = 0
        self.last = None


class MK:
    def __init__(self, nc, n_pool_sems=70):
        self.nc = nc
        self.es = ExitStack()
        self.lists = {e: [] for e in ENGS}
        self.esem = {}
        n_hw = (n_pool_sems * 3) // 5
        self.free_slots = {
            'hw': [SemSlot(self.es.enter_context(nc.semaphore(f"dqh{i}")), 'hw') for i in range(n_hw)],
            'sw': [SemSlot(self.es.enter_context(nc.semaphore(f"dqs{i}")), 'sw') for i in range(n_pool_sems - n_hw)],
        }
        self.all_slots = self.free_slots['hw'] + self.free_slots['sw']
        self.stack = [self.es]
        self.phase_slots = [[]]
        self.uid = 0

    def sb(self, name, shape, dtype=F32):
        self.uid += 1
        t = self.stack[-1].enter_context(self.nc.sbuf_tensor(f"{name}_{self.uid}", list(shape), dtype))
        return Buf(self, t, name, 'sb')

    def ps(self, name, shape, dtype=F32):
        self.uid += 1
        full = 512 if dtype == F32 else 1024
        t = self.stack[-1].enter_context(self.nc.psum_tensor(f"{name}_{self.uid}", [128, full], dtype))
        b = Buf(self, t, name, 'ps')
        b.view = (shape[0], shape[1])
        return b

    def dram(self, name, shape, dtype=F32, kind="Internal"):
        t = self.nc.dram_tensor(name, list(shape), dtype, kind=kind)
        return Buf(self, t, name, 'dram')

    def _slot_for(self, buf, q):
        kind = 'sw' if q == 'pool' else 'hw'
        if kind not in buf.sems:
            slot = self.free_slots[kind].pop()
            buf.sems[kind] = slot
            if buf.kind != 'dram':
                self.phase_slots[-1].append(slot)
        return buf.sems[kind]

    @contextmanager
    def phase(self):
        st = ExitStack()
        self.stack.append(st)
        self.phase_slots.append([])
        try:
            yield
        finally:
            self.barrier()
            self.stack.pop()
            for s in self.phase_slots.pop():
                self.free_slots[s.kind].append(s)
            st.close()

    def _track(self, ins, reads, writes):
        deps = []
        for b in reads:
            if b.last_w is not None:
                deps.append(('raw', b.last_w))
            for d in b.lastw_kind.values():
                deps.append(('raw', d))
        for b in writes:
            dram_store = ins.is_dma and ins.nowaw and b.last_w is not None and b.last_w.is_dma
            if b.last_w is not None and not dram_store:
                deps.append(('waw', b.last_w))
                for d in b.lastw_kind.values():
                    deps.append(('waw', d))
            for r in b.readers:
                deps.append(('war', r))
        seen = set()
        for kind, d in deps:
            if d is ins or id(d) in seen:
                continue
            if not d.is_dma and not ins.is_dma and d.eng == ins.eng:
                if ins.eng == 'pe':
                    continue
            seen.add(id(d))
            ins.deps.append(d)
            if not d.is_dma:
                d.signals = True
        for b in reads:
            b.readers.append(ins)
        for b in writes:
            b.last_w = ins
            b.readers = []

    def I(self, eng, meth, **kw):
        rb, wb, kk = [], [], {}
        for k, v in kw.items():
            if isinstance(v, V):
                (wb if k in WRITE_KW else rb).append(v.buf)
                kk[k] = v.ap
            else:
                kk[k] = v
        ins = Ins(eng, lambda e: getattr(e, meth)(**kk))
        self._track(ins, rb, wb)
        self.lists[eng].append(ins)
        return ins

    def dma(self, q, out, in_, force=False, nowaw=False, **kw):
        if q == 'sp' and out.buf.kind == 'dram' and not force:
            q = 'pool'
        ins = Ins(q, lambda e: e.dma_start(out=out.ap, in_=in_.ap, **kw), is_dma=True)
        ins.nowaw = nowaw
        slot = self._slot_for(out.buf, q)
        slot.cnt += 16
        ins.sem, ins.semval = slot.sem, slot.cnt
        slot.last = ins
        out.buf.lastw_kind[slot.kind] = ins
        self._track(ins, [in_.buf], [out.buf])
        self.lists[q].append(ins)
        return ins

    def barrier(self):
        last = {}
        for e in ENGS:
            for ins in reversed(self.lists[e]):
                if not ins.is_dma and ins.fn is not None:
                    last[e] = ins
                    ins.signals = True
                    break
        dmas = [s.last for s in self.all_slots if s.last is not None]
        for e in ENGS:
            w = Ins(e, None)
            w.deps = [d for ee, d in last.items() if ee != e] + dmas
            self.lists[e].append(w)

    def wait_bufs(self, eng, bufs):
        w = Ins(eng, None)
        self._track(w, list(bufs), [])
        self.lists[eng].append(w)

    def build(self):
        nc = self.nc
        for e in ENGS:
            self.esem[e] = self.es.enter_context(nc.semaphore("eng_" + e))
            n = 0
            for ins in self.lists[e]:
                if ins.signals and not ins.is_dma and ins.fn is not None:
                    n += 1
                    ins.sigidx = n
        lists, esem = self.lists, self.esem
        stats = {}

        def replay(ename, e):
            waited = {}
            nw = 0
            for ins in lists[ename]:
                need = {}
                for d in ins.deps:
                    if d.is_dma:
                        key, val, sem = ('d', id(d.sem)), d.semval, d.sem
                    else:
                        key, val, sem = ('e', d.eng), d.sigidx, esem[d.eng]
                    if key not in need or need[key][0] < val:
                        need[key] = (val, sem)
                for key, (val, sem) in need.items():
                    if waited.get(key, 0) >= val:
                        continue
                    waited[key] = val
                    e.wait_ge(sem, val)
                    nw += 1
                if ins.fn is None:
                    continue
                r = ins.fn(e)
                if ins.is_dma:
                    r.then_inc(ins.sem, 16)
                elif ins.signals:
                    r.then_inc(esem[ename], 1)
            stats[ename] = (len(lists[ename]), nw)

        with nc.Block() as block:
            @block.tensor
            def _(e):
                replay('pe', e)

            @block.scalar
            def _(e):
                replay('act', e)

            @block.vector
            def _(e):
                replay('dve', e)

            @block.gpsimd
            def _(e):
                replay('pool', e)

            @block.sync
            def _(e):
                replay('sp', e)
        self.stats = stats
        self.es.close()
        return nc


D = 1024
S = 2048
CTX = 256
NTOK = S + CTX
DEPTH = 4
D_IN = 2816
GRID_W = 64
NH = 8
DH = 64
NEXP = 32
FH = 256
RMS_EPS = 1e-6
LN_EPS = 1e-5
TWO_PI = 2.0 * math.pi
NEG_MASK = -30000.0


def _na_tables():
    rows = S // GRID_W
    kr, kc, qr, qc, br, bc = 8, 16, 8, 16, 16, 32
    n_rb, n_cb = rows // qr, GRID_W // qc
    r0 = np.clip(np.arange(n_rb) * qr - kr // 2, 0, rows - br)
    c0 = np.clip(np.arange(n_cb) * qc - kc // 2, 0, GRID_W - bc)
    info = []
    cch, rr, cc = np.meshgrid(np.arange(4), np.arange(4), np.arange(32), indexing='ij')
    krow_l = (4 * cch + rr).reshape(4, 128)
    kcol_l = cc.reshape(4, 128)
    qa, qb = np.meshgrid(np.arange(8), np.arange(16), indexing='ij')
    qa, qb = qa.reshape(128), qb.reshape(128)
    for i in range(n_rb):
        for j in range(n_cb):
            krow = r0[i] + krow_l[:, :, None]
            kcol = c0[j] + kcol_l[:, :, None]
            qrow = (i * qr + qa)[None, None, :]
            qcol = (j * qc + qb)[None, None, :]
            wr = np.clip(qrow - kr // 2, 0, rows - kr)
            wc = np.clip(qcol - kc // 2, 0, GRID_W - kc)
            mask = (krow >= wr) & (krow < wr + kr) & (kcol >= wc) & (kcol < wc + kc)
            dr = np.clip(krow - qrow + 7, 0, 14) + 0 * kcol
            dc = np.clip(kcol - qcol + 15, 0, 30) + 0 * krow
            info.append((int(r0[i]), int(c0[j]), i * qr, j * qc, np.broadcast_to(mask, dr.shape), dr, dc))
    return info


_CONST_CACHE = {}


def _consts():
    if _CONST_CACHE:
        return _CONST_CACHE
    c = {}
    c['ident'] = np.eye(128, dtype=np.float32)
    for L in (S, CTX):
        t = np.linspace(0.0, 1.0, L, dtype=np.float32)[:, None]
        w = (2.0 * math.pi * np.arange(L, dtype=np.float32)[:, None] / L).astype(np.float32)
        f = np.linspace(1e-4, 15, 16, dtype=np.float32)[None, :]
        z = np.concatenate([t, np.cos(f * w), -np.sin(f * w)], axis=-1).astype(np.float32)
        c[f'posT{L}'] = np.ascontiguousarray(z.T)
        min_decay = math.log(1e-2) / 1.5
        max_decay = math.log(1e-2) / 0.3
        deltas = np.abs(np.linspace(min_decay, max_decay, 256, dtype=np.float32))[None, :]
        dec = np.exp(-t * deltas).astype(np.float32)
        nT = L // 128
        c[f'decay{L}'] = np.ascontiguousarray(dec.reshape(nT, 128, 256).transpose(1, 0, 2))
        tt = np.arange(L, dtype=np.float64)[:, None]
        ff = np.arange(L, dtype=np.float64)[None, :] + 0.5
        ang = 2.0 * math.pi * tt * ff / (2 * L)
        C = np.cos(ang)
        Sn = np.sin(ang)
        def fwd(M):
            return np.ascontiguousarray(M.reshape(nT, 128, nT, 128).transpose(2, 1, 0, 3)).astype(ml_dtypes.bfloat16)
        def inv(M):
            return np.ascontiguousarray(M.reshape(nT, 128, nT, 128).transpose(0, 3, 2, 1)).astype(ml_dtypes.bfloat16)
        c[f'Cf{L}'] = fwd(C)
        c[f'Sf{L}'] = fwd(Sn)
        c[f'Ci{L}'] = inv(C)
        c[f'Si{L}'] = inv(Sn)
    _CONST_CACHE.update(c)
    return c


def routing_batched(mk, lg, T, cmb, sfx=""):
    I = mk.I
    def t(nm, shape):
        return mk.sb(f"rt_{nm}{sfx}", shape, F32).v()
    lg4 = lg[:, :, 0:4]
    gmax, se, gp = t("gmax", [128, T]), t("se", [128, T]), t("gp", [128, T])
    oh, sh = t("oh", [128, T, 4]), t("sh", [128, T, 4])
    selg, es, es2 = t("selg", [128, T, 32]), t("es", [128, T, 8]), t("es2", [128, T, 8])
    k1, k2 = t("k1", [128, T, 8]), t("k2", [128, T, 8])
    m1, m2, dd, ee, p1, p2 = (t(n, [128, T]) for n in ("m1", "m2", "dd", "ee", "p1", "p2"))
    ew = t("ew", [128, T, 8])
    I('dve', 'reduce_max', out=gmax, in_=lg4, axis=AX.X)
    I('dve', 'tensor_tensor', out=oh, in0=lg4, in1=gmax.unsq(2).bc([128, T, 4]), op=ALU.is_equal)
    I('dve', 'tensor_tensor', out=sh, in0=lg4, in1=gmax.unsq(2).bc([128, T, 4]), op=ALU.subtract)
    I('act', 'activation', out=sh, in_=sh, func=AF.Exp)
    I('dve', 'reduce_sum', out=se, in_=sh, axis=AX.X)
    I('dve', 'reciprocal', out=gp, in_=se)
    el = lg[:, :, 4:36].rearrange("p t (g e) -> p t g e", e=8)
    I('dve', 'tensor_tensor', out=selg.rearrange("p t (g e) -> p t g e", e=8), in0=el, in1=oh.unsq(3).bc([128, T, 4, 8]), op=ALU.mult)
    I('dve', 'reduce_sum', out=es, in_=selg.rearrange("p t (g e) -> p t e g", e=8), axis=AX.X)
    I('dve', 'reduce_max', out=m1, in_=es, axis=AX.X)
    I('dve', 'tensor_tensor', out=k1, in0=es, in1=m1.unsq(2).bc([128, T, 8]), op=ALU.is_equal)
    I('dve', 'scalar_tensor_tensor', out=es2, in0=k1, scalar=-1e30, in1=es, op0=ALU.mult, op1=ALU.add)
    I('dve', 'reduce_max', out=m2, in_=es2, axis=AX.X)
    I('dve', 'tensor_tensor', out=k2, in0=es2, in1=m2.unsq(2).bc([128, T, 8]), op=ALU.is_equal)
    I('dve', 'tensor_tensor', out=dd, in0=m2, in1=m1, op=ALU.subtract)
    I('act', 'activation', out=ee, in_=dd, func=AF.Exp)
    I('dve', 'tensor_scalar', out=p1, in0=ee, scalar1=1.0, scalar2=None, op0=ALU.add)
    I('dve', 'reciprocal', out=p1, in_=p1)
    I('dve', 'tensor_tensor', out=p2, in0=ee, in1=p1, op=ALU.mult)
    I('dve', 'tensor_tensor', out=p1, in0=p1, in1=gp, op=ALU.mult)
    I('dve', 'tensor_tensor', out=p2, in0=p2, in1=gp, op=ALU.mult)
    I('dve', 'tensor_tensor', out=k1, in0=k1, in1=p1.unsq(2).bc([128, T, 8]), op=ALU.mult)
    I('dve', 'tensor_tensor', out=k2, in0=k2, in1=p2.unsq(2).bc([128, T, 8]), op=ALU.mult)
    I('dve', 'tensor_tensor', out=ew, in0=k1, in1=k2, op=ALU.add)
    I('dve', 'tensor_tensor', out=cmb.rearrange("p t (g e) -> p t g e", e=8), in0=oh.unsq(3).bc([128, T, 4, 8]),
      in1=ew.unsq(2).bc([128, T, 4, 8]), op=ALU.mult)


def build_program(n_layers=DEPTH, stop=None, debug=False):
    nc = bass.Bass("TRN2", target_bir_lowering=False)
    mk = MK(nc)
    I = mk.I

    def ext(name, shape, dt=F32):
        return mk.dram(name, shape, dt, kind="ExternalInput")

    xin = ext("xin", [NTOK, D])
    cT = ext("cT", [128, 16])
    w_ada = ext("w_ada", [DEPTH, D, 6 * D])
    b_ada = ext("b_ada", [DEPTH, 6 * D])
    g_mix = ext("g_mix", [DEPTH, D])
    g_ffn = ext("g_ffn", [DEPTH, D])
    w_in = ext("w_in", [DEPTH, D, D_IN])
    w_out = ext("w_out", [DEPTH, D, D])
    gm_vg = ext("gmlp_v_gain", [DEPTH, 256])
    gm_ws = ext("gmlp_ws", [DEPTH, 4, 128, 128])
    gm_bsT = ext("gmlp_bsT", [DEPTH, 128, 4])
    na_qg = ext("na_q_gain", [DEPTH, 64])
    na_kg = ext("na_k_gain", [DEPTH, 64])
    na_bias = ext("na_bias", [DEPTH, 16, 128, NH * 4 * 128])
    hy_sw = ext("hy_short_w", [DEPTH, 3, 768])
    hy_sb = ext("hy_short_b", [DEPTH, 768])
    hy_w1 = ext("hy_w1", [DEPTH, 33, 64])
    hy_w2 = ext("hy_w2", [DEPTH, 64, 64])
    hy_w3 = ext("hy_w3", [DEPTH, 64, 1024])
    hy_fb = ext("hy_fb", [DEPTH, 64, 4])
    hy_bias = ext("hy_bias", [DEPTH, 512])
    moe_wr = ext("moe_wr", [DEPTH, D, 36])
    moe_br = ext("moe_br", [DEPTH, 36])
    moe_wg = ext("moe_w_gate", [DEPTH, NEXP, D, FH])
    moe_wu = ext("moe_w_up", [DEPTH, NEXP, D, FH])
    moe_wd = ext("moe_w_down", [DEPTH, NEXP, FH, D])
    identd = ext("ident", [128, 128])
    cst = {}
    for L in (S, CTX):
        nT = L // 128
        cst[f'posT{L}'] = ext(f"posT{L}", [33, L])
        cst[f'decay{L}'] = ext(f"decay{L}", [128, nT, 256])
        for nm in ('Cf', 'Sf', 'Ci', 'Si'):
            cst[f'{nm}{L}'] = ext(f"{nm}{L}", [nT, 128, nT * 128], BF16)

    out = mk.dram("out", [S, D], F32, kind="ExternalOutput")
    dk = "ExternalOutput" if debug else "Internal"
    xs = mk.dram("xs", [NTOK, D], F32, kind=dk)
    pl = mk.dram("pl", [NTOK, D_IN], F32, kind=dk)
    mix = mk.dram("mix", [NTOK, D], BF16, kind=dk)
    vd = mk.dram("vd", [NTOK, NH * 65], BF16, kind=dk)
    md = mk.dram("md", [2, 6 * D], F32, kind=dk)
    cbd = mk.dram("cbd", [NEXP, NTOK], BF16, kind=dk)
    if debug:
        dbgc = mk.dram("dbgc", [32, NTOK], BF16, kind=dk)
        dbgh = mk.dram("dbgh", [128, 8 * 256], BF16, kind=dk)

    identf = mk.sb("identf", [128, 128], F32)
    identb = mk.sb("identb", [128, 128], BF16)
    onesf = mk.sb("onesf", [128, 128], F32)
    lc = mk.sb("lc", [128, 16, 128], BF16)
    pmask = mk.sb("pmask", [128, 1], F32)
    negpi = mk.sb("negpi", [128, 1], F32)

    def rstd_from_ss(ss_v, scale, eps, n):
        I('dve', 'tensor_scalar', out=ss_v, in0=ss_v, scalar1=scale, scalar2=eps, op0=ALU.mult, op1=ALU.add)
        I('act', 'activation', out=ss_v, in_=ss_v, func=AF.Sqrt)
        I('dve', 'reciprocal', out=ss_v, in_=ss_v)

    with mk.phase():
        ct = mk.sb("ct", [128, 16], F32)
        sct = mk.sb("sct", [128, 16], F32)
        mk.dma('sp', identf.v(), identd.v())
        mk.dma('pool', identb.v(), identd.v())
        mk.dma('sp', ct.v(), cT.v())
        I('act', 'activation', out=sct.v(), in_=ct.v(), func=AF.Silu)
        I('dve', 'tensor_copy', out=lc.v(), in_=sct.v().unsq(2).bc([128, 16, 128]))
        I('dve', 'memset', ap=onesf.v(), constant=1.0)
        I('dve', 'memset', ap=negpi.v(), constant=-math.pi)
        I('dve', 'tensor_scalar', out=pmask.v(), in0=identf[:, 0:1], scalar1=-1.0, scalar2=1.0, op0=ALU.mult, op1=ALU.add)

    def done():
        mk.wait_bufs('sp', [out, xs, pl, mix, vd, md, cbd])
        mk.build()
        return nc, mk

    for l in range(n_layers):
        last = (l == DEPTH - 1)
        xsrc = xin if l == 0 else xs
        n_tiles = 16 if last else 18

        with mk.phase():
            wa = [mk.sb(f"wa{i}", [128, 8, 512], BF16) for i in range(2)]
            mrow = mk.sb("mrow", [1, 2, 6 * D], F32)
            ba = mk.sb("ba", [1, 6 * D], F32)
            gg = mk.sb("gg", [1, 2, D], F32)
            pm = [mk.ps(f"pm{i}", [128, 512], F32) for i in range(4)]
            mk.dma('sp', ba.v(), b_ada[l:l + 1, :])
            mk.dma('sp', gg[:, 0, :], g_mix[l:l + 1, :])
            mk.dma('sp', gg[:, 1, :], g_ffn[l:l + 1, :])
            for n in range(12):
                w = wa[n % 2]
                mk.dma('pool', w.v(), w_ada[l, :, n * 512:(n + 1) * 512].rearrange("(c p) n -> p c n", p=128))
                for s in range(2):
                    p = pm[(2 * n + s) % 4]
                    for c in range(8):
                        I('pe', 'matmul', out=p.v(), lhsT=lc[:, s * 8 + c, :], rhs=w[:, c, :], start=(c == 0), stop=(c == 7))
                    I('dve', 'tensor_tensor', out=mrow[:, s, n * 512:(n + 1) * 512], in0=p[0:1, :],
                      in1=ba[:, n * 512:(n + 1) * 512], op=ALU.add)
            for s in range(2):
                for (slot, g) in ((1, 0), (4, 1)):
                    I('dve', 'scalar_tensor_tensor', out=mrow[:, s, slot * D:(slot + 1) * D],
                      in0=mrow[:, s, slot * D:(slot + 1) * D], scalar=1.0, in1=gg[:, g, :], op0=ALU.add, op1=ALU.mult)
                mk.dma('sp', md[s:s + 1, :], mrow[:, s, :])
        if stop == f"p1_{l}":
            return done()

        def load_mod(tile_v, s, slot):
            mk.dma('sp', tile_v, md[s, slot * D:(slot + 1) * D].pbc(128))

        with mk.phase():
            win = mk.sb("win", [128, 8, D_IN], BF16)
            wst = [mk.sb(f"wst{i}", [128, D_IN], F32) for i in range(2)]
            for c in range(8):
                mk.dma('sp', wst[c % 2].v(), w_in[l, c * 128:(c + 1) * 128, :])
                if c % 4 == 3:
                    I('pool', 'tensor_copy', out=win[:, c, :], in_=wst[c % 2].v())
                elif c % 2 == 0:
                    I('act', 'activation', out=win[:, c, :], in_=wst[c % 2].v(), func=AF.Copy)
                else:
                    I('dve', 'tensor_copy', out=win[:, c, :], in_=wst[c % 2].v())
            gsb = [mk.sb(f"gsb{s}", [128, D], F32) for s in range(2)]
            shb = [mk.sb(f"shb{s}", [128, D], F32) for s in range(2)]
            for s in range(2):
                load_mod(gsb[s].v(), s, 1)
                load_mod(shb[s].v(), s, 0)
            xb = [mk.sb(f"xb{i}", [128, D], F32) for i in range(2)]
            junk = mk.sb("junk", [128, D], BF16)
            ssb = [mk.sb(f"ss{i}", [128, 1], F32) for i in range(2)]
            hf = mk.sb("hf", [128, D], F32)
            hb = [mk.sb(f"hb{i}", [128, D], BF16) for i in range(2)]
            hT = [mk.sb(f"hT{i}", [128, 8, 128], BF16) for i in range(2)]
            plt = [mk.sb(f"plt{i}", [128, D_IN], F32) for i in range(3)]
            pt = [mk.ps(f"pt{i}", [128, 1024], BF16) for i in range(2)]
            po = [mk.ps(f"po{i}", [128, 512], F32) for i in range(4)]
            kctr = [0]

            def p2_front(tt):
                s = 0 if tt < 16 else 1
                x_, ss_, hb_, hT_, pt_ = xb[tt % 2], ssb[tt % 2], hb[tt % 2], hT[tt % 2], pt[tt % 2]
                mk.dma('sp', x_.v(), xsrc[tt * 128:(tt + 1) * 128, :])
                I('act', 'activation', out=junk.v(), in_=x_.v(), func=AF.Square, accum_out=ss_.v())
                rstd_from_ss(ss_.v(), 1.0 / D, RMS_EPS, 1)
                I('dve', 'scalar_tensor_tensor', out=hf.v(), in0=x_.v(), scalar=ss_.v(), in1=gsb[s].v(), op0=ALU.mult, op1=ALU.mult)
                I('dve', 'tensor_tensor', out=hb_.v(), in0=hf.v(), in1=shb[s].v(), op=ALU.add)
                for c in range(8):
                    I('pe', 'transpose', out=pt_[:, c * 128:(c + 1) * 128], in_=hb_[:, c * 128:(c + 1) * 128], identity=identb.v())
                I('act', 'activation', out=hT_.v().rearrange("p c t -> p (c t)"), in_=pt_.v(), func=AF.Copy)

            def p2_back(tt):
                hT_, plt_ = hT[tt % 2], plt[tt % 3]
                for n in range(6):
                    n0 = n * 512
                    wdt = min(512, D_IN - n0)
                    p = po[kctr[0] % 4]
                    kctr[0] += 1
                    for c in range(8):
                        I('pe', 'matmul', out=p[:, 0:wdt], lhsT=hT_[:, c, :], rhs=win[:, c, n0:n0 + wdt], start=(c == 0), stop=(c == 7))
                    if n == 0:
                        I('act', 'activation', out=plt_[:, n0:n0 + wdt], in_=p[:, 0:wdt], func=AF.Gelu_apprx_tanh)
                    elif n % 2 == 0:
                        I('dve', 'tensor_copy', out=plt_[:, n0:n0 + wdt], in_=p[:, 0:wdt])
                    else:
                        I('act', 'activation', out=plt_[:, n0:n0 + wdt], in_=p[:, 0:wdt], func=AF.Copy)
                mk.dma('sp', pl[tt * 128:(tt + 1) * 128, :], plt_.v())

            for tt in range(19):
                if tt < 18:
                    p2_front(tt)
                if tt >= 1:
                    p2_back(tt - 1)
        if stop == f"p2_{l}":
            return done()

        with mk.phase():
            wsf = mk.sb("wsf", [128, 4, 128], F32)
            wsT = mk.sb("wsT", [128, 4, 128], BF16)
            bsT = mk.sb("bsT", [128, 4], F32)
            vg = mk.sb("vg", [128, 256], F32)
            pw = mk.ps("pw", [128, 512], F32)
            mk.dma('sp', wsf.v(), gm_ws[l].rearrange("g i j -> i g j"))
            mk.dma('sp', bsT.v(), gm_bsT[l])
            mk.dma('sp', vg.v(), gm_vg[l, :].pbc(128))
            for g in range(4):
                I('pe', 'transpose', out=pw[:, g * 128:(g + 1) * 128], in_=wsf[:, g, :], identity=identf.v())
            I('dve', 'tensor_copy', out=wsT.v().rearrange("p g i -> p (g i)"), in_=pw.v())
            guall = mk.sb("guall", [128, n_tiles, 512], F32)
            for t0_ in range(0, n_tiles, 6):
                t1_ = min(n_tiles, t0_ + 6)
                mk.dma('sp', guall[:, t0_:t1_, :], pl[t0_ * 128:t1_ * 128, 0:512].rearrange("(t p) n -> p t n", p=128))
            st4 = [mk.sb(f"st4{i}", [128, 4], F32) for i in range(2)]
            vr4 = [mk.sb(f"vr4{i}", [128, 4], F32) for i in range(2)]
            vc = mk.sb("vc", [128, 4, 64], F32)
            sq = mk.sb("sq", [128, 4, 64], F32)
            vnb = [mk.sb(f"vnb{i}", [128, 256], BF16) for i in range(2)]
            mo = [mk.sb(f"mo{i}", [128, 256], BF16) for i in range(3)]
            pg = [mk.ps(f"pg{i}", [128, 256], F32) for i in range(2)]
            for tt in range(n_tiles):
                m4, v4, vn_, mo_, pg_ = st4[tt % 2], vr4[tt % 2], vnb[tt % 2], mo[tt % 3], pg[tt % 2]
                gu = guall[:, tt, :]
                v3 = gu[:, 256:512].rearrange("p (g d) -> p g d", d=64)
                I('dve', 'reduce_sum', out=m4.v(), in_=v3, axis=AX.X)
                I('dve', 'tensor_scalar', out=m4.v(), in0=m4.v(), scalar1=-1.0 / 64, scalar2=None, op0=ALU.mult)
                I('dve', 'tensor_tensor', out=vc.v(), in0=v3, in1=m4.v().unsq(2).bc([128, 4, 64]), op=ALU.add)
                I('dve', 'tensor_tensor', out=sq.v(), in0=vc.v(), in1=vc.v(), op=ALU.mult)
                I('dve', 'reduce_sum', out=v4.v(), in_=sq.v(), axis=AX.X)
                rstd_from_ss(v4.v(), 1.0 / 64, LN_EPS, 4)
                I('dve', 'tensor_tensor', out=vc.v(), in0=vc.v(), in1=v4.v().unsq(2).bc([128, 4, 64]), op=ALU.mult)
                I('dve', 'tensor_tensor', out=vn_.v(), in0=vc.v().rearrange("p g d -> p (g d)"), in1=vg.v(), op=ALU.mult)
                for g in range(4):
                    I('pe', 'matmul', out=pg_[:, g * 64:(g + 1) * 64], lhsT=wsT[:, g, :], rhs=vn_[:, g * 64:(g + 1) * 64], start=True, stop=True)
                for g in range(4):
                    I('dve', 'scalar_tensor_tensor', out=mo_[:, g * 64:(g + 1) * 64], in0=pg_[:, g * 64:(g + 1) * 64],
                      scalar=bsT[:, g:g + 1], in1=gu[:, g * 64:(g + 1) * 64], op0=ALU.add, op1=ALU.mult)
                mk.dma('sp', mix[tt * 128:(tt + 1) * 128, 0:256], mo_.v())
        if stop == f"p3_{l}":
            return done()

        with mk.phase():
            qT = mk.sb("qT", [128, 4, NTOK], BF16)
            kT = mk.sb("kT", [128, 4, NTOK], BF16)
            with mk.phase():
                gqk = mk.sb("gqk", [128, 16, 64], F32)
                mk.dma('sp', gqk[:, 0:8, :], na_qg.custom(l * 64, [[0, 128], [0, 8], [1, 64]]))
                mk.dma('sp', gqk[:, 8:16, :], na_kg.custom(l * 64, [[0, 128], [0, 8], [1, 64]]))
                I('dve', 'tensor_scalar', out=gqk[:, 0:8, :], in0=gqk[:, 0:8, :], scalar1=DH ** -0.5, scalar2=None, op0=ALU.mult)
                qkv = [mk.sb(f"qkv{i}", [128, 1536], F32) for i in range(2)]
                sq16 = mk.sb("sq16", [128, 16, 64], F32)
                r16 = [mk.sb(f"r16{i}", [128, 16], F32) for i in range(2)]
                nb = [mk.sb(f"nb{i}", [128, 1024], BF16) for i in range(2)]
                vb = [mk.sb(f"vb{i}", [128, 8, 65], BF16) for i in range(3)]
                ptq = [mk.ps(f"ptq{i}", [128, 1024], BF16) for i in range(2)]
                for i in range(3):
                    I('dve', 'memset', ap=vb[i][:, :, 64:65], constant=1.0)

                def p4_front(tt):
                    t_, r_, nb_ = qkv[tt % 2], r16[tt % 2], nb[tt % 2]
                    mk.dma('sp', t_.v(), pl[tt * 128:(tt + 1) * 128, 512:2048])
                    t3 = t_[:, 0:1024].rearrange("p (h d) -> p h d", d=64)
                    I('pool', 'tensor_tensor', out=sq16.v(), in0=t3, in1=t3, op=ALU.mult)
                    I('dve', 'reduce_sum', out=r_.v(), in_=sq16.v(), axis=AX.X)
                    rstd_from_ss(r_.v(), 1.0 / DH, RMS_EPS, 16)
                    I('dve', 'tensor_tensor', out=sq16.v(), in0=t3, in1=r_.v().unsq(2).bc([128, 16, 64]), op=ALU.mult)
                    I('dve', 'tensor_tensor', out=nb_.v().rearrange("p (h d) -> p h d", d=64), in0=sq16.v(), in1=gqk.v(), op=ALU.mult)

                def p4_back(tt):
                    t_, nb_, p_, vb_ = qkv[tt % 2], nb[tt % 2], ptq[tt % 2], vb[tt % 3]
                    for c in range(8):
                        I('pe', 'transpose', out=p_[:, c * 128:(c + 1) * 128], in_=nb_[:, c * 128:(c + 1) * 128], identity=identb.v())
                    I('act', 'activation', out=qT[:, :, tt * 128:(tt + 1) * 128], in_=p_[:, 0:512].rearrange("p (c t) -> p c t", t=128), func=AF.Copy)
                    I('act', 'activation', out=kT[:, :, tt * 128:(tt + 1) * 128], in_=p_[:, 512:1024].rearrange("p (c t) -> p c t", t=128), func=AF.Copy)
                    I('act', 'activation', out=vb_[:, :, 0:64], in_=t_[:, 1024:1536].rearrange("p (h d) -> p h d", d=64), func=AF.Copy)
                    mk.dma('sp', vd[tt * 128:(tt + 1) * 128, :], vb_.v().rearrange("p h d -> p (h d)"))

                for tt in range(19):
                    if tt < 18:
                        p4_front(tt)
                    if tt >= 1:
                        p4_back(tt - 1)
            vctx = mk.sb("vctx", [128, 2, NH, 65], BF16)
            mk.dma('sp', vctx.v().rearrange("p c h d -> p c (h d)"), vd[S:NTOK, :].rearrange("(c p) n -> p c n", p=128))
            btb = [mk.sb(f"bt{i}", [128, NH, 4, 128], F32) for i in range(2)]
            vwb = [mk.sb(f"vw{i}", [128, 4, NH, 65], BF16) for i in range(2)]
            obb = [mk.sb(f"ob{i}", [128, 512], BF16) for i in range(2)]
            kbb = [mk.sb(f"kb{i}", [128, 4, 512], BF16) for i in range(2)]
            qbb = [mk.sb(f"qb{i}", [128, 4, 128], BF16) for i in range(2)]
            pTb = [mk.sb(f"pT{i}", [128, 768], BF16) for i in range(3)]
            sTb = [mk.sb(f"sT{i}", [128, 512], F32) for i in range(3)]
            rcp = [mk.sb(f"rcp{i}", [128, 1], F32) for i in range(2)]
            psw = [mk.ps(f"psw{i}", [128, 512], F32) for i in range(3)]
            psc = [mk.ps(f"psc{i}", [128, 256], F32) for i in range(3)]
            pov = [mk.ps(f"pov{i}", [128, 65], F32) for i in range(2)]
            info = _na_tables()
            nblk = 16 + (0 if last else 2)
            vmap = {0: 0, 1: 1, 2: 1, 3: 2}
            border = sorted(range(16), key=lambda b: (vmap[b // 4], vmap[b % 4], b)) + list(range(16, nblk))
            units = [(k, h) for k in range(nblk) for h in range(NH)]
            bias_buf = {}
            nbias = [0]

            def variant(blk):
                return (vmap[blk // 4], vmap[blk % 4])

            def load_block(k):
                blk = border[k]
                r0, c0, qr0, qc0 = info[blk][:4]
                vw = vwb[k % 2]
                if k == 0 or variant(blk) != variant(border[k - 1]):
                    nbias[0] += 1
                    bt = btb[nbias[0] % 2]
                    mk.dma('sp', bt.v().rearrange("p h c q -> p (h c q)"), na_bias[l, blk])
                    bias_buf[k] = bt
                else:
                    bias_buf[k] = bias_buf[k - 1]
                vd3 = vd.v().rearrange("(r c) n -> r c n", c=GRID_W)[r0:r0 + 16, c0:c0 + 32, :]
                vd4 = vd3.rearrange("(ch rr) x n -> rr x ch n", rr=4)
                for rr in range(4):
                    mk.dma('sp', vw[rr * 32:(rr + 1) * 32, :, :, :].rearrange("p c h d -> p c (h d)"), vd4[rr], nowaw=(rr > 0))
                kb, qb = kbb[k % 2], qbb[k % 2]
                for hp_ in range(4):
                    k3 = kT[:, hp_, 0:S].rearrange("p (r c) -> p r c", c=GRID_W)[:, r0:r0 + 16, c0:c0 + 32]
                    if hp_ < 2:
                        I('pool', 'tensor_copy', out=kb[:, hp_, :].rearrange("p (r c) -> p r c", c=32), in_=k3)
                    else:
                        I('act', 'activation', out=kb[:, hp_, :].rearrange("p (r c) -> p r c", c=32), in_=k3, func=AF.Copy)
                q4 = qT[:, :, 0:S].rearrange("p h (r c) -> p h r c", c=GRID_W)[:, :, qr0:qr0 + 8, qc0:qc0 + 16]
                I('pool', 'tensor_copy', out=qb.v().rearrange("p h (r c) -> p h r c", c=16), in_=q4)

            def front(i):
                k, h = units[i]
                blk = border[k]
                hp, base = h // 2, (h % 2) * 64
                pw_, pc_, pT_, sT_ = psw[i % 3], psc[i % 3], pTb[i % 3], sTb[i % 3]
                if blk < 16:
                    kb, qb, bt = kbb[k % 2], qbb[k % 2], bias_buf[k]
                    q3 = qb[base:base + 64, hp, :]
                    for c in range(4):
                        I('pe', 'matmul', out=pw_[:, c * 128:(c + 1) * 128], lhsT=kb[base:base + 64, hp, c * 128:(c + 1) * 128], rhs=q3, start=True, stop=True)
                    for c in range(2):
                        I('pe', 'matmul', out=pc_[:, c * 128:(c + 1) * 128], lhsT=kT[base:base + 64, hp, S + c * 128:S + (c + 1) * 128], rhs=q3, start=True, stop=True)
                    I('dve', 'tensor_tensor', out=sT_.v(), in0=pw_.v(), in1=bt[:, h, :, :].rearrange("p c q -> p (c q)"), op=ALU.add)
                    I('act', 'activation', out=pT_[:, 0:512], in_=sT_.v(), func=AF.Exp)
                    I('act', 'activation', out=pT_[:, 512:768], in_=pc_.v(), func=AF.Exp)
                else:
                    qt = blk - 16
                    q2 = qT[base:base + 64, hp, S + qt * 128:S + (qt + 1) * 128]
                    for c in range(2):
                        I('pe', 'matmul', out=pc_[:, c * 128:(c + 1) * 128], lhsT=kT[base:base + 64, hp, S + c * 128:S + (c + 1) * 128],
                          rhs=q2, start=True, stop=True)
                    I('act', 'activation', out=pT_[:, 512:768], in_=pc_.v(), func=AF.Exp)

            def back(i):
                k, h = units[i]
                blk = border[k]
                po_, pT_, rc_, ob = pov[i % 2], pTb[i % 3], rcp[i % 2], obb[k % 2]
                if blk < 16:
                    vw = vwb[k % 2]
                    for c in range(6):
                        rhs = vw[:, c, h, :] if c < 4 else vctx[:, c - 4, h, :]
                        I('pe', 'matmul', out=po_.v(), lhsT=pT_[:, c * 128:(c + 1) * 128], rhs=rhs, start=(c == 0), stop=(c == 5))
                else:
                    for c in range(2):
                        I('pe', 'matmul', out=po_.v(), lhsT=pT_[:, 512 + c * 128:512 + (c + 1) * 128], rhs=vctx[:, c, h, :], start=(c == 0), stop=(c == 1))
                I('dve', 'reciprocal', out=rc_.v(), in_=po_[:, 64:65])
                I('dve', 'tensor_scalar', out=ob[:, h * 64:(h + 1) * 64], in0=po_[:, 0:64], scalar1=rc_.v(), scalar2=None, op0=ALU.mult)
                if h == NH - 1:
                    if blk < 16:
                        qr0, qc0 = info[blk][2:4]
                        for a in range(8):
                            t0 = (qr0 + a) * GRID_W + qc0
                            mk.dma('sp', mix[t0:t0 + 16, 256:768], ob[a * 16:(a + 1) * 16, :], force=True, nowaw=(a > 0))
                    else:
                        t0 = S + (blk - 16) * 128
                        mk.dma('sp', mix[t0:t0 + 128, 256:768], ob.v(), force=True)

            load_block(0)
            for i in range(len(units) + 2):
                if i < len(units):
                    front(i)
                if i >= 2:
                    back(i - 2)
                if i < len(units):
                    k, h = units[i]
                    if h == 2 and k + 1 < 16:
                        load_block(k + 1)
        if stop == f"p5_{l}":
            return done()

        streams = [dict(L=S, row0=0, tag='l')]
        if not last:
            streams.append(dict(L=CTX, row0=S, tag='c'))
        for st in streams:
            st['nT'] = st['L'] // 128
        with mk.phase():
            for st in streams:
                st['Kr'] = mk.sb("Kr" + st['tag'], [128, st['nT'], 512], BF16)
                st['Ks'] = mk.sb("Ks" + st['tag'], [128, st['nT'], 512], BF16)
            with mk.phase():
                w1 = mk.sb("w1", [33, 64], F32)
                w2 = mk.sb("w2", [64, 64], F32)
                w3 = mk.sb("w3", [64, 1024], F32)
                fb = mk.sb("fb", [64, 4], F32)
                sc = mk.sb("sc", [64, 2], F32)
                of = mk.sb("of", [64, 2], F32)
                mk.dma('sp', w1.v(), hy_w1[l])
                mk.dma('sp', w2.v(), hy_w2[l])
                mk.dma('sp', w3.v(), hy_w3[l])
                mk.dma('sp', fb.v(), hy_fb[l])
                for st in streams:
                    L, nT, tg = st['L'], st['nT'], st['tag']
                    st['posT'] = mk.sb("posT" + tg, [33, L], F32)
                    st['dec'] = mk.sb("dec" + tg, [128, nT, 256], F32)
                    st['h2T'] = mk.sb("h2T" + tg, [64, L], F32)
                    st['Ab'] = mk.sb("Ab" + tg, [128, nT, 512], BF16)
                    st['Bb'] = mk.sb("Bb" + tg, [128, nT, 512], BF16)
                    st['rn'] = mk.sb("rn" + tg, [128, 2, 256], F32)
                    st['pS'] = [mk.ps(f"pS{tg}{i}", [128, 512], F32) for i in range(2)]
                    mk.dma('sp', st['posT'].v(), cst[f'posT{L}'].v())
                    mk.dma('sp', st['dec'].v(), cst[f'decay{L}'].v())
                I('dve', 'tensor_scalar', out=sc.v(), in0=fb[:, 0:2], scalar1=1.0 / 3.0, scalar2=None, op0=ALU.mult)
                I('dve', 'tensor_tensor', out=of.v(), in0=fb[:, 2:4], in1=sc.v(), op=ALU.mult)
                u1b = [mk.sb(f"u1{i}", [64, 512], F32) for i in range(2)]
                u2b = [mk.sb(f"u2{i}", [64, 512], F32) for i in range(2)]
                hrot = [mk.sb(f"hrot{i}", [128, 1024], F32) for i in range(2)]
                habs = [mk.sb(f"habs{i}", [128, 1024], F32) for i in range(2)]
                ph = [mk.ps(f"ph{i}", [128, 512], F32) for i in range(2)]
                p3 = [mk.ps(f"p3{i}", [128, 512], F32) for i in range(2)]
                chunks = []
                for st in streams:
                    CW = min(512, st['L'])
                    for pc in range(st['L'] // CW):
                        chunks.append((st, slice(pc * CW, (pc + 1) * CW), CW))
                h1c = [mk.sb(f"h1c{i}", [64, 512], F32) for i in range(len(chunks))]
                for k_ in range(2):
                    wk = (w1, w2)[k_]
                    for ci, (st, cs, CW) in enumerate(chunks):
                        u1, u2, p_ = u1b[ci % 2], u2b[ci % 2], ph[ci % 2]
                        src = st['posT'][:, cs] if k_ == 0 else h1c[ci][:, 0:CW]
                        dst = h1c[ci][:, 0:CW] if k_ == 0 else st['h2T'][:, cs]
                        I('pe', 'matmul', out=p_[0:64, 0:CW], lhsT=wk.v(), rhs=src, start=True, stop=True)
                        I('act', 'activation', out=u1[:, 0:CW], in_=p_[0:64, 0:CW], func=AF.Sin, bias=of[:, k_:k_ + 1], scale=sc[:, k_:k_ + 1])
                        I('dve', 'tensor_tensor', out=u2[:, 0:CW], in0=u1[:, 0:CW], in1=u1[:, 0:CW], op=ALU.mult)
                        I('dve', 'tensor_scalar', out=u2[:, 0:CW], in0=u2[:, 0:CW], scalar1=-4.0, scalar2=3.0, op0=ALU.mult, op1=ALU.add)
                        I('dve', 'tensor_tensor', out=dst, in0=u1[:, 0:CW], in1=u2[:, 0:CW], op=ALU.mult)
                kq = 0
                for st in streams:
                    nT = st['nT']
                    for tc in range(nT):
                        hr, ha = hrot[kq % 2], habs[kq % 2]
                        kq += 1
                        for hf_ in range(2):
                            p = p3[hf_]
                            I('pe', 'matmul', out=p.v(), lhsT=st['h2T'][:, tc * 128:(tc + 1) * 128], rhs=w3[:, hf_ * 512:(hf_ + 1) * 512], start=True, stop=True)
                            I('dve', 'tensor_tensor', out=hr[:, hf_ * 512:(hf_ + 1) * 512].rearrange("p (a c) -> p a c", c=256),
                              in0=p.v().rearrange("p (a c) -> p a c", c=256), in1=st['dec'][:, tc, :].unsq(1).bc([128, 2, 256]), op=ALU.mult)
                        I('act', 'activation', out=ha.v(), in_=hr.v(), func=AF.Abs)
                        for hf_ in range(2):
                            I('pe', 'matmul', out=st['pS'][hf_].v(), lhsT=onesf.v(), rhs=ha[:, hf_ * 512:(hf_ + 1) * 512], start=(tc == 0), stop=(tc == nT - 1))
                        h4 = hr.v().rearrange("p (o d c) -> p o d c", o=2, d=2)
                        if tc == 0:
                            I('dve', 'tensor_scalar', out=h4[:, :, 1, :], in0=h4[:, :, 1, :], scalar1=pmask.v(), scalar2=None, op0=ALU.mult)
                        I('dve', 'tensor_tensor', out=st['Ab'][:, tc, :].rearrange("p (o c) -> p o c", o=2), in0=h4[:, :, 0, :], in1=h4[:, :, 1, :], op=ALU.add)
                        I('pool', 'tensor_tensor', out=st['Bb'][:, tc, :].rearrange("p (o c) -> p o c", o=2), in0=h4[:, :, 0, :], in1=h4[:, :, 1, :], op=ALU.subtract)
                for st in streams:
                    rn = st['rn']
                    for o in range(2):
                        I('dve', 'tensor_copy', out=rn[:, o, :], in_=st['pS'][o][:, 0:256])
                        I('dve', 'tensor_tensor', out=rn[:, o, :], in0=rn[:, o, :], in1=st['pS'][o][:, 256:512], op=ALU.add)
                    I('dve', 'reciprocal', out=rn.v(), in_=rn.v())
                cfb = [mk.sb(f"cfk{i}", [128, 16, 128], BF16) for i in range(2)]
                sfb = [mk.sb(f"sfk{i}", [128, 16, 128], BF16) for i in range(2)]
                kq = 0
                for st in streams:
                    L, nT = st['L'], st['nT']
                    rnf = st['rn'].v().rearrange("p o c -> p (o c)")
                    for fc in range(nT):
                        cf, sf = cfb[kq % 2], sfb[kq % 2]
                        kq += 1
                        mk.dma('sp', cf[:, 0:nT, :].rearrange("p t f -> p (t f)"), cst[f'Cf{L}'][fc])
                        mk.dma('sp', sf[:, 0:nT, :].rearrange("p t f -> p (t f)"), cst[f'Sf{L}'][fc])
                        for tc in range(nT):
                            I('pe', 'matmul', out=ph[0].v(), lhsT=cf[:, tc, :], rhs=st['Ab'][:, tc, :], start=(tc == 0), stop=(tc == nT - 1))
                        for tc in range(nT):
                            I('pe', 'matmul', out=ph[1].v(), lhsT=sf[:, tc, :], rhs=st['Bb'][:, tc, :], start=(tc == 0), stop=(tc == nT - 1))
                        I('dve', 'tensor_tensor', out=st['Kr'][:, fc, :], in0=ph[0].v(), in1=rnf, op=ALU.mult)
                        I('dve', 'tensor_tensor', out=st['Ks'][:, fc, :], in0=ph[1].v(), in1=rnf, op=ALU.mult)
            swb = mk.sb("swb", [128, 3, 768], F32)
            sbb = mk.sb("sbb", [128, 768], F32)
            dbb = mk.sb("dbb", [128, 512], F32)
            mk.dma('sp', swb.v().rearrange("p k n -> p (k n)"), hy_sw[l].rearrange("k n -> (k n)").pbc(128))
            mk.dma('sp', sbb.v(), hy_sb[l, :].pbc(128))
            mk.dma('sp', dbb.v(), hy_bias[l, :].pbc(128))
            for st in streams:
                nT, tg = st['nT'], st['tag']
                st['zf'] = mk.sb("zf" + tg, [128, nT, 256], F32)
                st['zb'] = mk.sb("zb" + tg, [128, nT, 256], BF16)
                st['gts'] = mk.sb("gts" + tg, [128, nT, 512], F32)
                st['Yr'] = mk.sb("Yr" + tg, [128, nT, 256], BF16)
                st['Ys'] = mk.sb("Ys" + tg, [128, nT, 256], BF16)
            a3 = [mk.sb(f"a3{i}", [128, 3, 768], F32) for i in range(3)]
            accb = [mk.sb(f"acc{i}", [128, 768], F32) for i in range(2)]
            kq = 0
            for st in streams:
                nT, row0 = st['nT'], st['row0']
                for tc in range(nT):
                    a_, acc = a3[kq % 3], accb[kq % 2]
                    kq += 1
                    g0 = row0 + tc * 128
                    if tc == 0:
                        I('dve', 'memset', ap=a_[0:1, 0, :], constant=0.0)
                    if tc == nT - 1:
                        I('dve', 'memset', ap=a_[:, 2, :], constant=0.0)
                    mk.dma('sp', a_[:, 1, :], pl[g0:g0 + 128, 2048:2816])
                    if tc == 0:
                        mk.dma('sp', a_[1:128, 0, :], pl[g0:g0 + 127, 2048:2816], nowaw=True)
                    else:
                        mk.dma('sp', a_[:, 0, :], pl[g0 - 1:g0 + 127, 2048:2816], nowaw=True)
                    if tc == nT - 1:
                        mk.dma('sp', a_[0:127, 2, :], pl[g0 + 1:g0 + 128, 2048:2816], nowaw=True)
                    else:
                        mk.dma('sp', a_[:, 2, :], pl[g0 + 1:g0 + 129, 2048:2816], nowaw=True)
                    I('dve', 'tensor_tensor', out=acc.v(), in0=a_[:, 1, :], in1=swb[:, 1, :], op=ALU.mult)
                    I('dve', 'tensor_tensor', out=acc.v(), in0=acc.v(), in1=sbb.v(), op=ALU.add)
                    I('pool', 'tensor_tensor', out=a_[:, 0, :], in0=a_[:, 0, :], in1=swb[:, 0, :], op=ALU.mult)
                    I('pool', 'tensor_tensor', out=a_[:, 2, :], in0=a_[:, 2, :], in1=swb[:, 2, :], op=ALU.mult)
                    I('dve', 'tensor_tensor', out=acc.v(), in0=acc.v(), in1=a_[:, 0, :], op=ALU.add)
                    I('dve', 'tensor_tensor', out=acc.v(), in0=acc.v(), in1=a_[:, 2, :], op=ALU.add)
                    I('act', 'activation', out=st['zf'][:, tc, :], in_=acc[:, 0:256], func=AF.Copy)
                    I('act', 'activation', out=st['zb'][:, tc, :], in_=acc[:, 0:256], func=AF.Copy)
                    I('act', 'activation', out=st['gts'][:, tc, :], in_=acc[:, 256:768], func=AF.Copy)
            cfb = [mk.sb(f"cf{i}", [128, 16, 128], BF16) for i in range(2)]
            sfb = [mk.sb(f"sf{i}", [128, 16, 128], BF16) for i in range(2)]
            zr = [mk.sb(f"zr{i}", [128, 256], F32) for i in range(2)]
            zs = [mk.sb(f"zs{i}", [128, 256], F32) for i in range(2)]
            t1 = mk.sb("t1", [128, 256], F32)
            t2 = mk.sb("t2", [128, 256], F32)
            t3 = mk.sb("t3", [128, 256], F32)
            t4 = mk.sb("t4", [128, 256], F32)
            dzb = [mk.sb(f"dz{i}", [128, 256], F32) for i in range(2)]
            pz = [mk.ps(f"pz{i}", [128, 256], F32) for i in range(4)]
            py = [mk.ps(f"py{i}", [128, 256], F32) for i in range(2)]
            kq = 0
            for n in range(2):
                ksl = slice(n * 256, (n + 1) * 256)
                for st in streams:
                    L, nT = st['L'], st['nT']
                    Kr, Ks, zb, Yr, Ys = st['Kr'], st['Ks'], st['zb'], st['Yr'], st['Ys']
                    for fc in range(nT):
                        cf, sf = cfb[kq % 2], sfb[kq % 2]
                        pzr, pzs = pz[(kq % 2) * 2], pz[(kq % 2) * 2 + 1]
                        zr_, zs_ = zr[kq % 2], zs[kq % 2]
                        kq += 1
                        mk.dma('sp', cf[:, 0:nT, :].rearrange("p t f -> p (t f)"), cst[f'Cf{L}'][fc])
                        mk.dma('sp', sf[:, 0:nT, :].rearrange("p t f -> p (t f)"), cst[f'Sf{L}'][fc])
                        for tc in range(nT):
                            I('pe', 'matmul', out=pzr.v(), lhsT=cf[:, tc, :], rhs=zb[:, tc, :], start=(tc == 0), stop=(tc == nT - 1))
                        for tc in range(nT):
                            I('pe', 'matmul', out=pzs.v(), lhsT=sf[:, tc, :], rhs=zb[:, tc, :], start=(tc == 0), stop=(tc == nT - 1))
                        I('act', 'activation', out=zr_.v(), in_=pzr.v(), func=AF.Copy)
                        I('act', 'activation', out=zs_.v(), in_=pzs.v(), func=AF.Copy)
                        I('pool', 'tensor_tensor', out=t1.v(), in0=zr_.v(), in1=Kr[:, fc, ksl], op=ALU.mult)
                        I('pool', 'tensor_tensor', out=t2.v(), in0=zs_.v(), in1=Ks[:, fc, ksl], op=ALU.mult)
                        I('pool', 'tensor_tensor', out=Yr[:, fc, :], in0=t1.v(), in1=t2.v(), op=ALU.subtract)
                        I('dve', 'tensor_tensor', out=t3.v(), in0=zr_.v(), in1=Ks[:, fc, ksl], op=ALU.mult)
                        I('dve', 'tensor_tensor', out=t4.v(), in0=zs_.v(), in1=Kr[:, fc, ksl], op=ALU.mult)
                        I('dve', 'tensor_tensor', out=Ys[:, fc, :], in0=t3.v(), in1=t4.v(), op=ALU.add)
                for st in streams:
                    L, nT = st['L'], st['nT']
                    zf, zb, gts, Yr, Ys = st['zf'], st['zb'], st['gts'], st['Yr'], st['Ys']
                    for tc in range(nT):
                        ci, si = cfb[kq % 2], sfb[kq % 2]
                        p, dz = py[kq % 2], dzb[kq % 2]
                        kq += 1
                        mk.dma('sp', ci[:, 0:nT, :].rearrange("p t f -> p (t f)"), cst[f'Ci{L}'][tc])
                        mk.dma('sp', si[:, 0:nT, :].rearrange("p t f -> p (t f)"), cst[f'Si{L}'][tc])
                        for fc in range(nT):
                            I('pe', 'matmul', out=p.v(), lhsT=ci[:, fc, :], rhs=Yr[:, fc, :], start=(fc == 0), stop=False)
                        for fc in range(nT):
                            I('pe', 'matmul', out=p.v(), lhsT=si[:, fc, :], rhs=Ys[:, fc, :], start=False, stop=(fc == nT - 1))
                        I('pool', 'tensor_tensor', out=dz.v(), in0=zf[:, tc, :], in1=dbb[:, ksl], op=ALU.mult)
                        I('dve', 'scalar_tensor_tensor', out=zf[:, tc, :], in0=p.v(), scalar=1.0 / L, in1=dz.v(), op0=ALU.mult, op1=ALU.add)
                        I('dve', 'tensor_tensor', out=zf[:, tc, :], in0=zf[:, tc, :], in1=gts[:, tc, ksl], op=ALU.mult)
                        I('act', 'activation', out=zb[:, tc, :], in_=zf[:, tc, :], func=AF.Copy)
            for st in streams:
                L, row0 = st['L'], st['row0']
                mk.dma('sp', mix[row0:row0 + L, 768:1024].rearrange("(t p) c -> p t c", p=128), st['zb'].v())
        if stop == f"p6_{l}":
            return done()

        with mk.phase():
            wo = mk.sb("wo", [128, 8, D], BF16)
            wst = [mk.sb(f"wst{i}", [128, 2, D], F32) for i in range(2)]
            for c2 in range(4):
                mk.dma('sp', wst[c2 % 2].v(), w_out[l, c2 * 256:(c2 + 1) * 256, :].rearrange("(c p) n -> p c n", p=128))
                if c2 % 2 == 0:
                    I('act', 'activation', out=wo[:, c2 * 2:(c2 + 1) * 2, :], in_=wst[c2 % 2].v(), func=AF.Copy)
                else:
                    I('dve', 'tensor_copy', out=wo[:, c2 * 2:(c2 + 1) * 2, :], in_=wst[c2 % 2].v())
            g2 = [mk.sb(f"g2{s}", [128, D], F32) for s in range(2)]
            for s in range(2):
                load_mod(g2[s].v(), s, 2)
            xb = [mk.sb(f"xb{i}", [128, D], F32) for i in range(4)]
            mb = [mk.sb(f"mb{i}", [128, D], BF16) for i in range(4)]
            mT = [mk.sb(f"mT{i}", [128, 8, 128], BF16) for i in range(2)]
            tmp = mk.sb("tmp", [128, D], F32)
            pt = [mk.ps(f"pt{i}", [128, 1024], BF16) for i in range(2)]
            po = [mk.ps(f"po{i}", [128, 512], F32) for i in range(4)]
            def p7_front(tt):
                x_, m_, mT_, pt_ = xb[tt % 4], mb[tt % 4], mT[tt % 2], pt[tt % 2]
                mk.dma('sp', x_.v(), xsrc[tt * 128:(tt + 1) * 128, :])
                mk.dma('sp', m_.v(), mix[tt * 128:(tt + 1) * 128, :])
                for c in range(8):
                    I('pe', 'transpose', out=pt_[:, c * 128:(c + 1) * 128], in_=m_[:, c * 128:(c + 1) * 128], identity=identb.v())
                I('act', 'activation', out=mT_.v().rearrange("p c t -> p (c t)"), in_=pt_.v(), func=AF.Copy)

            def p7_back(tt):
                s = 0 if tt < 16 else 1
                x_, mT_ = xb[tt % 4], mT[tt % 2]
                for hf_ in range(2):
                    p = po[(tt % 2) * 2 + hf_]
                    for c in range(8):
                        I('pe', 'matmul', out=p.v(), lhsT=mT_[:, c, :], rhs=wo[:, c, hf_ * 512:(hf_ + 1) * 512], start=(c == 0), stop=(c == 7))
                    hs = slice(hf_ * 512, (hf_ + 1) * 512)
                    I('dve', 'tensor_tensor', out=tmp[:, hs], in0=p.v(), in1=g2[s][:, hs], op=ALU.mult)
                    I('dve', 'tensor_tensor', out=x_[:, hs], in0=x_[:, hs], in1=tmp[:, hs], op=ALU.add)
                mk.dma('sp', xs[tt * 128:(tt + 1) * 128, :], x_.v())

            for tt in range(n_tiles + 1):
                if tt < n_tiles:
                    p7_front(tt)
                if tt >= 1:
                    p7_back(tt - 1)
        if stop == f"p7_{l}":
            return done()

        ntok = n_tiles * 128
        with mk.phase():
            hTa = mk.sb("hTa", [128, 8, ntok], BF16)
            oacc = mk.sb("oacc", [128, n_tiles, D], F32)
            with mk.phase():
                gsb = [mk.sb(f"gsb{s}", [128, D], F32) for s in range(2)]
                shb = [mk.sb(f"shb{s}", [128, D], F32) for s in range(2)]
                for s in range(2):
                    load_mod(gsb[s].v(), s, 4)
                    load_mod(shb[s].v(), s, 3)
                wr = mk.sb("wr", [128, 8, 36], F32)
                brb = mk.sb("brb", [128, 36], F32)
                mk.dma('sp', wr.v(), moe_wr[l].rearrange("(c p) n -> p c n", p=128))
                mk.dma('sp', brb.v(), moe_br[l, :].pbc(128))
                xb = [mk.sb(f"xb{i}", [128, D], F32) for i in range(2)]
                junk = mk.sb("junk", [128, D], BF16)
                ssb = [mk.sb(f"ss{i}", [128, 1], F32) for i in range(2)]
                hf = [mk.sb(f"hf{i}", [128, D], F32) for i in range(2)]
                hTf = [mk.sb(f"hTf{i}", [128, 8, 128], F32) for i in range(2)]
                ptf = [mk.ps(f"ptf{i}", [128, 512], F32) for i in range(4)]
                pr = [mk.ps(f"pr{i}", [128, 36], F32) for i in range(2)]
                pct = [mk.ps(f"pct{i}", [32, 128], F32) for i in range(2)]
                combT = mk.sb("combT", [32, ntok], BF16)
                lgall = mk.sb("lgall", [128, n_tiles, 36], F32)
                cmball = mk.sb("cmball", [128, n_tiles, 32], F32)
                def p8_front(tt):
                    s = 0 if tt < 16 else 1
                    i2 = tt % 2
                    x_, ss_, hf_, hTf_ = xb[i2], ssb[i2], hf[i2], hTf[i2]
                    mk.dma('sp', x_.v(), xs[tt * 128:(tt + 1) * 128, :])
                    I('act', 'activation', out=junk.v(), in_=x_.v(), func=AF.Square, accum_out=ss_.v())
                    rstd_from_ss(ss_.v(), 1.0 / D, RMS_EPS, 1)
                    I('dve', 'scalar_tensor_tensor', out=hf_.v(), in0=x_.v(), scalar=ss_.v(), in1=gsb[s].v(), op0=ALU.mult, op1=ALU.mult)
                    I('pool', 'tensor_tensor', out=hf_.v(), in0=hf_.v(), in1=shb[s].v(), op=ALU.add)
                    for hh in range(2):
                        p = ptf[i2 * 2 + hh]
                        for c in range(4):
                            cc = hh * 4 + c
                            I('pe', 'transpose', out=p[:, c * 128:(c + 1) * 128], in_=hf_[:, cc * 128:(cc + 1) * 128], identity=identf.v())
                        I('act', 'activation', out=hTf_[:, hh * 4:(hh + 1) * 4, :].rearrange("p c t -> p (c t)"), in_=p.v(), func=AF.Copy)
                        I('dve', 'tensor_copy', out=hTa[:, hh * 4:(hh + 1) * 4, tt * 128:(tt + 1) * 128],
                          in_=hTf_[:, hh * 4:(hh + 1) * 4, :])
                def p8_back(tt):
                    i2 = tt % 2
                    hTf_ = hTf[i2]
                    pr_ = pr[i2]
                    for c in range(8):
                        I('pe', 'matmul', out=pr_.v(), lhsT=hTf_[:, c, :], rhs=wr[:, c, :], start=(c == 0), stop=(c == 7))
                    I('dve', 'tensor_tensor', out=lgall[:, tt, :], in0=pr_.v(), in1=brb.v(), op=ALU.add)
                for tt in range(n_tiles + 1):
                    if tt < n_tiles:
                        p8_front(tt)
                    if tt >= 1:
                        p8_back(tt - 1)
                routing_batched(mk, lgall.v(), n_tiles, cmball.v())
                for tt in range(n_tiles):
                    pc_ = pct[tt % 2]
                    I('pe', 'transpose', out=pc_.v(), in_=cmball[:, tt, :], identity=identf.v())
                    I('act', 'activation', out=combT[:, tt * 128:(tt + 1) * 128], in_=pc_.v(), func=AF.Copy)
                mk.dma('sp', cbd[:, 0:ntok], combT.v())
            with mk.phase():
                wgb = [mk.sb(f"wg{i}", [128, 2, 8, FH], BF16) for i in range(2)]
                wub = [mk.sb(f"wu{i}", [128, 2, 8, FH], BF16) for i in range(2)]
                wdb = [mk.sb(f"wd{i}", [128, 2, 2, D], BF16) for i in range(2)]
                actb = [mk.sb(f"act{i}", [128, 2, 2, 512], BF16) for i in range(2)]
                sgb = [mk.sb(f"sg{i}", [128, 512], F32) for i in range(2)]
                tb = [mk.sb(f"tb{i}", [128, 512], F32) for i in range(2)]
                pgp = [mk.ps(f"pgp{i}", [128, 512], F32) for i in range(2)]
                pup = [mk.ps(f"pup{i}", [128, 512], F32) for i in range(2)]
                cbb = [mk.sb(f"cbb{i}", [128, ntok], BF16) for i in range(3)]
                pdp = [mk.ps(f"pdp{i}", [128, 512], F32) for i in range(4)]
                groups = [(g0, min(512, ntok - g0)) for g0 in range(0, ntok, 512)]
                g5 = [mk.sb(f"g5{s}", [128, D], F32) for s in range(2)]
                for s in range(2):
                    load_mod(g5[s].v(), s, 5)
                xrb = [mk.sb(f"xr{i}", [128, D], F32) for i in range(2)]
                res_dst = out if (l == n_layers - 1 and not debug) or last else xs

                def res_load(tt):
                    if tt < n_tiles:
                        mk.dma('sp', xrb[tt % 2].v(), xs[tt * 128:(tt + 1) * 128, :])

                def res_finish(tt):
                    s = 0 if tt < 16 else 1
                    x_ = xrb[tt % 2]
                    I('dve', 'tensor_tensor', out=oacc[:, tt, :], in0=oacc[:, tt, :], in1=g5[s].v(), op=ALU.mult)
                    I('dve', 'tensor_tensor', out=x_.v(), in0=x_.v(), in1=oacc[:, tt, :], op=ALU.add)
                    if not (res_dst is out and tt >= 16):
                        mk.dma('sp', res_dst[tt * 128:(tt + 1) * 128, :], x_.v())
                kk = 0
                kd = 0
                gi = 0
                for pair in range(0 if stop == f"p8a_{l}" else NEXP // 2):
                    wg, wu, wd = wgb[pair % 2], wub[pair % 2], wdb[pair % 2]
                    for e in range(2):
                        ge = pair * 2 + e
                        mk.dma('sp', cbb[ge % 3].v(), cbd[ge, 0:ntok].pbc(128))
                        mk.dma('pool', wg[:, e, :, :], moe_wg[l, ge].rearrange("(c p) f -> p c f", p=128))
                        mk.dma('pool', wu[:, e, :, :], moe_wu[l, ge].rearrange("(c p) f -> p c f", p=128))
                        mk.dma('pool', wd[:, e, :, :], moe_wd[l, ge].rearrange("(c p) n -> p c n", p=128))
                    last_pair = (pair == NEXP // 2 - 1)
                    if last_pair:
                        res_load(0)
                    for (g0, gw) in groups:
                        at = actb[gi % 2]
                        gi += 1
                        for e in range(2):
                            ge = pair * 2 + e
                            for fc in range(2):
                                pg_, pu_, sg_, tb_ = pgp[kk % 2], pup[kk % 2], sgb[kk % 2], tb[kk % 2]
                                cb_ = cbb[ge % 3]
                                kk += 1
                                for c in range(8):
                                    I('pe', 'matmul', out=pg_[:, 0:gw], lhsT=wg[:, e, c, fc * 128:(fc + 1) * 128], rhs=hTa[:, c, g0:g0 + gw], start=(c == 0), stop=(c == 7))
                                for c in range(8):
                                    I('pe', 'matmul', out=pu_[:, 0:gw], lhsT=wu[:, e, c, fc * 128:(fc + 1) * 128], rhs=hTa[:, c, g0:g0 + gw], start=(c == 0), stop=(c == 7))
                                I('act', 'activation', out=sg_[:, 0:gw], in_=pg_[:, 0:gw], func=AF.Silu)
                                I('dve', 'tensor_tensor', out=tb_[:, 0:gw], in0=sg_[:, 0:gw], in1=pu_[:, 0:gw], op=ALU.mult)
                                I('dve', 'tensor_tensor', out=at[:, e, fc, 0:gw], in0=tb_[:, 0:gw], in1=cb_[:, g0:g0 + gw], op=ALU.mult)
                        for ti in range(gw // 128):
                            tt = g0 // 128 + ti
                            if last_pair:
                                res_load(tt + 1)
                            for hf_ in range(2):
                                pd_ = pdp[kd % 4]
                                kd += 1
                                hs = slice(hf_ * 512, (hf_ + 1) * 512)
                                n_ = 0
                                for e in range(2):
                                    for fc in range(2):
                                        I('pe', 'matmul', out=pd_.v(), lhsT=at[:, e, fc, ti * 128:(ti + 1) * 128], rhs=wd[:, e, fc, hs], start=(n_ == 0), stop=(n_ == 3))
                                        n_ += 1
                                if pair == 0:
                                    I('dve', 'tensor_copy', out=oacc[:, tt, hs], in_=pd_.v())
                                else:
                                    I('dve', 'tensor_tensor', out=oacc[:, tt, hs], in0=oacc[:, tt, hs], in1=pd_.v(), op=ALU.add)
                            if last_pair:
                                res_finish(tt)
        if stop in (f"p8_{l}", f"p8a_{l}"):
            return done()
    return done()


def make_in_maps(inputs):
    c = _consts()
    f32 = np.float32
    x = np.asarray(inputs['x'], f32)
    B = x.shape[0]
    ctx = np.asarray(inputs['ctx'], f32)
    cc = np.asarray(inputs['c'], f32)
    c_ctx = np.asarray(inputs['c_ctx'], f32)
    info = _na_tables()
    rpb = np.asarray(inputs['na_rpb'], f32)
    nab = np.empty((DEPTH, 16, 128, NH, 4, 128), f32)
    for b_, (r0, c0, qr0, qc0, mask, dr, dc) in enumerate(info):
        g = rpb[:, :, dr, dc]
        g = np.where(mask[None, None], g, f32(NEG_MASK))
        nab[:, b_] = g.transpose(0, 3, 1, 2, 4)
    nab = nab.reshape(DEPTH, 16, 128, NH * 4 * 128)
    fb = np.stack([np.asarray(inputs['hy_freq'], f32)[:, 0], np.asarray(inputs['hy_freq'], f32)[:, 1],
                   np.asarray(inputs['hy_b1'], f32), np.asarray(inputs['hy_b2'], f32)], axis=-1)
    shared = {
        'w_ada': np.asarray(inputs['w_ada'], f32), 'b_ada': np.asarray(inputs['b_ada'], f32),
        'g_mix': np.asarray(inputs['g_mix'], f32), 'g_ffn': np.asarray(inputs['g_ffn'], f32),
        'w_in': np.asarray(inputs['w_in'], f32), 'w_out': np.asarray(inputs['w_out'], f32),
        'gmlp_v_gain': np.asarray(inputs['gmlp_v_gain'], f32), 'gmlp_ws': np.asarray(inputs['gmlp_ws'], f32),
        'gmlp_bsT': np.ascontiguousarray(np.asarray(inputs['gmlp_bs'], f32).transpose(0, 2, 1)),
        'na_q_gain': np.asarray(inputs['na_q_gain'], f32), 'na_k_gain': np.asarray(inputs['na_k_gain'], f32),
        'na_bias': nab,
        'hy_short_w': np.asarray(inputs['hy_short_w'], f32), 'hy_short_b': np.asarray(inputs['hy_short_b'], f32),
        'hy_w1': np.asarray(inputs['hy_w1'], f32), 'hy_w2': np.asarray(inputs['hy_w2'], f32), 'hy_w3': np.asarray(inputs['hy_w3'], f32),
        'hy_fb': np.ascontiguousarray(fb), 'hy_bias': np.asarray(inputs['hy_bias'], f32).reshape(DEPTH, 512),
        'moe_wr': np.ascontiguousarray(np.concatenate([np.asarray(inputs['moe_w_rg'], f32), np.asarray(inputs['moe_w_re'], f32)], axis=-1)),
        'moe_br': np.ascontiguousarray(np.concatenate([np.asarray(inputs['moe_b_rg'], f32), np.asarray(inputs['moe_b_re'], f32)], axis=-1)),
        'moe_w_gate': np.asarray(inputs['moe_w_gate'], f32).reshape(DEPTH, NEXP, D, FH),
        'moe_w_up': np.asarray(inputs['moe_w_up'], f32).reshape(DEPTH, NEXP, D, FH),
        'moe_w_down': np.asarray(inputs['moe_w_down'], f32).reshape(DEPTH, NEXP, FH, D),
        'ident': c['ident'],
    }
    for L in (S, CTX):
        nT = L // 128
        shared[f'posT{L}'] = c[f'posT{L}']
        shared[f'decay{L}'] = c[f'decay{L}']
        for nm in ('Cf', 'Sf', 'Ci', 'Si'):
            shared[f'{nm}{L}'] = c[f'{nm}{L}'].reshape(nT, 128, nT * 128)
    maps = []
    for b in range(B):
        m = dict(shared)
        m['xin'] = np.ascontiguousarray(np.concatenate([x[b], ctx[b]], axis=0))
        cT = np.concatenate([cc[b].reshape(8, 128).T, c_ctx.reshape(8, 128).T], axis=1)
        m['cT'] = np.ascontiguousarray(cT)
        maps.append(m)
    return maps


_PROG = {}


def kernel(**inputs):
    if 'nc' not in _PROG:
        _PROG['nc'] = build_program()[0]
    maps = make_in_maps(inputs)
    res = run_bass_kernel_spmd(_PROG['nc'], maps, core_ids=list(range(len(maps))))
    return np.stack([np.asarray(r['out'], np.float32) for r in res.results], axis=0)
```

```python
import math
import os
import numpy as np
import ml_dtypes
import concourse.bass as bass
import concourse.mybir as mybir
from concourse.bass_utils import run_bass_kernel_spmd
from contextlib import ExitStack, contextmanager

F32 = mybir.dt.float32
BF16 = mybir.dt.bfloat16
ALU = mybir.AluOpType
AF = mybir.ActivationFunctionType
AX = mybir.AxisListType

ENGS = ('pe', 'act', 'dve', 'pool', 'sp')
WRITE_KW = ('out', 'accum_out', 'ap')


class Buf:
    def __init__(self, mk, t, name, kind):
        self.mk, self.t, self.name, self.kind = mk, t, name, kind
        self.last_w = None
        self.readers = []
        self.sems = {}
        self.lastw_kind = {}

    view = None

    def __getitem__(self, idx):
        return self.v()[idx]

    def v(self):
        if self.view is not None:
            return V(self, self.t[0:self.view[0], 0:self.view[1]])
        return V(self, self.t[:])

    def custom(self, offset, ap):
        return V(self, bass.AP(self.t, offset, ap))


class V:
    def __init__(self, buf, ap):
        self.buf, self.ap = buf, ap

    def __getitem__(self, idx):
        return V(self.buf, self.ap[idx])

    def rearrange(self, pat, **kw):
        return V(self.buf, self.ap.rearrange(pat, **kw))

    def bc(self, shape):
        return V(self.buf, self.ap.to_broadcast(list(shape)))

    def unsq(self, axis):
        return V(self.buf, self.ap.unsqueeze(axis))

    def pbc(self, n):
        return V(self.buf, self.ap.partition_broadcast(n))


class Ins:
    __slots__ = ('eng', 'fn', 'deps', 'is_dma', 'sem', 'semval', 'signals', 'sigidx', 'nowaw', 'waits')

    def __init__(self, eng, fn, is_dma=False):
        self.eng, self.fn, self.is_dma = eng, fn, is_dma
        self.deps = []
        self.sem = None
        self.semval = 0
        self.signals = False
        self.sigidx = None
        self.nowaw = False
        self.waits = []


class SemSlot:
    def __init__(self, sem, kind):
        self.sem = sem
        self.kind = kind
        self.cnt = 0
        self.last = None


class MK:
    def __init__(self, nc, n_pool_sems=70):
        self.nc = nc
        self.es = ExitStack()
        self.lists = {e: [] for e in ENGS}
        self.esem = {}
        n_hw = (n_pool_sems * 3) // 5
        self.free_slots = {
            'hw': [SemSlot(self.es.enter_context(nc.semaphore(f"dqh{i}")), 'hw') for i in range(n_hw)],
            'sw': [SemSlot(self.es.enter_context(nc.semaphore(f"dqs{i}")), 'sw') for i in range(n_pool_sems - n_hw)],
        }
        self.all_slots = self.free_slots['hw'] + self.free_slots['sw']
        self.stack = [self.es]
        self.phase_slots = [[]]
        self.uid = 0
        self.order = []

    def sb(self, name, shape, dtype=F32):
        self.uid += 1
        t = self.stack[-1].enter_context(self.nc.sbuf_tensor(f"{name}_{self.uid}", list(shape), dtype))
        return Buf(self, t, name, 'sb')

    def ps(self, name, shape, dtype=F32):
        self.uid += 1
        full = 512 if dtype == F32 else 1024
        t = self.stack[-1].enter_context(self.nc.psum_tensor(f"{name}_{self.uid}", [128, full], dtype))
        b = Buf(self, t, name, 'ps')
        b.view = (shape[0], shape[1])
        return b

    def dram(self, name, shape, dtype=F32, kind="Internal"):
        t = self.nc.dram_tensor(name, list(shape), dtype, kind=kind)
        return Buf(self, t, name, 'dram')

    def _slot_for(self, buf, q):
        kind = 'sw' if q == 'pool' else 'hw'
        if kind not in buf.sems:
            slot = self.free_slots[kind].pop()
            buf.sems[kind] = slot
            if buf.kind != 'dram':
                self.phase_slots[-1].append(slot)
        return buf.sems[kind]

    @contextmanager
    def phase(self):
        st = ExitStack()
        self.stack.append(st)
        self.phase_slots.append([])
        try:
            yield
        finally:
            self.barrier()
            self.stack.pop()
            for s in self.phase_slots.pop():
                self.free_slots[s.kind].append(s)
            st.close()

    def _track(self, ins, reads, writes):
        deps = []
        for b in reads:
            if b.last_w is not None:
                deps.append(('raw', b.last_w))
            for d in b.lastw_kind.values():
                deps.append(('raw', d))
        for b in writes:
            dram_store = ins.is_dma and ins.nowaw and b.last_w is not None and b.last_w.is_dma
            if b.last_w is not None and not dram_store:
                deps.append(('waw', b.last_w))
                for d in b.lastw_kind.values():
                    deps.append(('waw', d))
            for r in b.readers:
                deps.append(('war', r))
        seen = set()
        for kind, d in deps:
            if d is ins or id(d) in seen:
                continue
            if not d.is_dma and not ins.is_dma and d.eng == ins.eng:
                if ins.eng == 'pe':
                    continue
            seen.add(id(d))
            ins.deps.append(d)
            if not d.is_dma:
                d.signals = True
        for b in reads:
            b.readers.append(ins)
        for b in writes:
            b.last_w = ins
            b.readers = []

    def I(self, eng, meth, **kw):
        rb, wb, kk = [], [], {}
        for k, v in kw.items():
            if isinstance(v, V):
                (wb if k in WRITE_KW else rb).append(v.buf)
                kk[k] = v.ap
            else:
                kk[k] = v
        ins = Ins(eng, lambda e: getattr(e, meth)(**kk))
        self._track(ins, rb, wb)
        self.lists[eng].append(ins)
        self.order.append(ins)
        return ins

    def dma(self, q, out, in_, force=False, nowaw=False, **kw):
        if q == 'sp' and out.buf.kind == 'dram' and not force:
            q = 'pool'
        ins = Ins(q, lambda e: e.dma_start(out=out.ap, in_=in_.ap, **kw), is_dma=True)
        ins.nowaw = nowaw
        slot = self._slot_for(out.buf, q)
        slot.cnt += 16
        ins.sem, ins.semval = slot.sem, slot.cnt
        slot.last = ins
        out.buf.lastw_kind[slot.kind] = ins
        self._track(ins, [in_.buf], [out.buf])
        self.lists[q].append(ins)
        self.order.append(ins)
        return ins

    def barrier(self):
        last = {}
        for e in ENGS:
            for ins in reversed(self.lists[e]):
                if not ins.is_dma and ins.fn is not None:
                    last[e] = ins
                    ins.signals = True
                    break
        dmas = [s.last for s in self.all_slots if s.last is not None]
        for e in ENGS:
            w = Ins(e, None)
            w.deps = [d for ee, d in last.items() if ee != e] + dmas
            self.lists[e].append(w)
            self.order.append(w)

    def wait_bufs(self, eng, bufs):
        w = Ins(eng, None)
        self._track(w, list(bufs), [])
        self.lists[eng].append(w)
        self.order.append(w)

    def build(self):
        nc = self.nc
        for e in ENGS:
            self.esem[e] = self.es.enter_context(nc.semaphore("eng_" + e))
            n = 0
            for ins in self.lists[e]:
                if ins.signals and not ins.is_dma and ins.fn is not None:
                    n += 1
                    ins.sigidx = n
        lists, esem = self.lists, self.esem
        stats = {}
        known = {e: {} for e in ENGS}
        snap = {}
        for ins in self.order:
            kn = known[ins.eng]
            need = {}
            for d in ins.deps:
                if d.is_dma:
                    key, val, sem = ('d', id(d.sem)), d.semval, d.sem
                else:
                    key, val, sem = ('e', d.eng), d.sigidx, esem[d.eng]
                if key not in need or need[key][0] < val:
                    need[key] = (val, sem, d)
            waits = []
            for key, (val, sem, d) in need.items():
                if kn.get(key, 0) >= val:
                    continue
                waits.append((sem, val))
                kn[key] = val
                ps = snap.get(id(d))
                if ps:
                    for k2, v2 in ps.items():
                        if kn.get(k2, 0) < v2:
                            kn[k2] = v2
            ins.waits = waits
            if ins.fn is not None and (ins.signals or ins.is_dma):
                snap[id(ins)] = {k: v for k, v in kn.items() if k[0] == 'e'}

        def replay(ename, e):
            nw = 0
            for ins in lists[ename]:
                for (sem, val) in ins.waits:
                    e.wait_ge(sem, val)
                    nw += 1
                if ins.fn is None:
                    continue
                r = ins.fn(e)
                if ins.is_dma:
                    r.then_inc(ins.sem, 16)
                elif ins.signals:
                    r.then_inc(esem[ename], 1)
            stats[ename] = (len(lists[ename]), nw)

        with nc.Block() as block:
            @block.tensor
            def _(e):
                replay('pe', e)

            @block.scalar
            def _(e):
                replay('act', e)

            @block.vector
            def _(e):
                replay('dve', e)

            @block.gpsimd
            def _(e):
                replay('pool', e)

            @block.sync
            def _(e):
                replay('sp', e)
        self.stats = stats
        self.es.close()
        return nc


D = 1024
S = 2048
CTX = 256
NTOK = S + CTX
DEPTH = 4
D_IN = 2816
GRID_W = 64
NH = 8
DH = 64
NEXP = 32
FH = 256
RMS_EPS = 1e-6
LN_EPS = 1e-5
TWO_PI = 2.0 * math.pi
NEG_MASK = -30000.0


def _na_tables():
    rows = S // GRID_W
    kr, kc, qr, qc, br, bc = 8, 16, 8, 16, 16, 32
    n_rb, n_cb = rows // qr, GRID_W // qc
    r0 = np.clip(np.arange(n_rb) * qr - kr // 2, 0, rows - br)
    c0 = np.clip(np.arange(n_cb) * qc - kc // 2, 0, GRID_W - bc)
    info = []
    cch, rr, cc = np.meshgrid(np.arange(4), np.arange(4), np.arange(32), indexing='ij')
    krow_l = (4 * cch + rr).reshape(4, 128)
    kcol_l = cc.reshape(4, 128)
    qa, qb = np.meshgrid(np.arange(8), np.arange(16), indexing='ij')
    qa, qb = qa.reshape(128), qb.reshape(128)
    for i in range(n_rb):
        for j in range(n_cb):
            krow = r0[i] + krow_l[:, :, None]
            kcol = c0[j] + kcol_l[:, :, None]
            qrow = (i * qr + qa)[None, None, :]
            qcol = (j * qc + qb)[None, None, :]
            wr = np.clip(qrow - kr // 2, 0, rows - kr)
            wc = np.clip(qcol - kc // 2, 0, GRID_W - kc)
            mask = (krow >= wr) & (krow < wr + kr) & (kcol >= wc) & (kcol < wc + kc)
            dr = np.clip(krow - qrow + 7, 0, 14) + 0 * kcol
            dc = np.clip(kcol - qcol + 15, 0, 30) + 0 * krow
            info.append((int(r0[i]), int(c0[j]), i * qr, j * qc, np.broadcast_to(mask, dr.shape), dr, dc))
    return info


_CONST_CACHE = {}


def _consts():
    if _CONST_CACHE:
        return _CONST_CACHE
    c = {}
    c['ident'] = np.eye(128, dtype=np.float32)
    for L in (S, CTX):
        t = np.linspace(0.0, 1.0, L, dtype=np.float32)[:, None]
        w = (2.0 * math.pi * np.arange(L, dtype=np.float32)[:, None] / L).astype(np.float32)
        f = np.linspace(1e-4, 15, 16, dtype=np.float32)[None, :]
        z = np.concatenate([t, np.cos(f * w), -np.sin(f * w)], axis=-1).astype(np.float32)
        c[f'posT{L}'] = np.ascontiguousarray(z.T)
        min_decay = math.log(1e-2) / 1.5
        max_decay = math.log(1e-2) / 0.3
        deltas = np.abs(np.linspace(min_decay, max_decay, 256, dtype=np.float32))[None, :]
        dec = np.exp(-t * deltas).astype(np.float32)
        nT = L // 128
        c[f'decay{L}'] = np.ascontiguousarray(dec.reshape(nT, 128, 256).transpose(1, 0, 2))
        tt = np.arange(L, dtype=np.float64)[:, None]
        ff = np.arange(L, dtype=np.float64)[None, :] + 0.5
        ang = 2.0 * math.pi * tt * ff / (2 * L)
        C = np.cos(ang)
        Sn = np.sin(ang)
        def fwd(M):
            return np.ascontiguousarray(M.reshape(nT, 128, nT, 128).transpose(2, 1, 0, 3)).astype(ml_dtypes.bfloat16)
        def inv(M):
            return np.ascontiguousarray(M.reshape(nT, 128, nT, 128).transpose(0, 3, 2, 1)).astype(ml_dtypes.bfloat16)
        c[f'Cf{L}'] = fwd(C)
        c[f'Sf{L}'] = fwd(Sn)
        c[f'Ci{L}'] = inv(C)
        c[f'Si{L}'] = inv(Sn)
    _CONST_CACHE.update(c)
    return c


def routing_batched(mk, lg, T, cmb, sfx=""):
    I = mk.I
    def t(nm, shape):
        return mk.sb(f"rt_{nm}{sfx}", shape, F32).v()
    lg4 = lg[:, :, 0:4]
    gmax, se, gp = t("gmax", [128, T]), t("se", [128, T]), t("gp", [128, T])
    oh, sh = t("oh", [128, T, 4]), t("sh", [128, T, 4])
    selg, es, es2 = t("selg", [128, T, 32]), t("es", [128, T, 8]), t("es2", [128, T, 8])
    k1, k2 = t("k1", [128, T, 8]), t("k2", [128, T, 8])
    m1, m2, dd, ee, p1, p2 = (t(n, [128, T]) for n in ("m1", "m2", "dd", "ee", "p1", "p2"))
    ew = t("ew", [128, T, 8])
    I('dve', 'reduce_max', out=gmax, in_=lg4, axis=AX.X)
    I('dve', 'tensor_tensor', out=oh, in0=lg4, in1=gmax.unsq(2).bc([128, T, 4]), op=ALU.is_equal)
    I('dve', 'tensor_tensor', out=sh, in0=lg4, in1=gmax.unsq(2).bc([128, T, 4]), op=ALU.subtract)
    I('act', 'activation', out=sh, in_=sh, func=AF.Exp)
    I('dve', 'reduce_sum', out=se, in_=sh, axis=AX.X)
    I('dve', 'reciprocal', out=gp, in_=se)
    el = lg[:, :, 4:36].rearrange("p t (g e) -> p t g e", e=8)
    I('dve', 'tensor_tensor', out=selg.rearrange("p t (g e) -> p t g e", e=8), in0=el, in1=oh.unsq(3).bc([128, T, 4, 8]), op=ALU.mult)
    I('dve', 'reduce_sum', out=es, in_=selg.rearrange("p t (g e) -> p t e g", e=8), axis=AX.X)
    I('dve', 'reduce_max', out=m1, in_=es, axis=AX.X)
    I('dve', 'tensor_tensor', out=k1, in0=es, in1=m1.unsq(2).bc([128, T, 8]), op=ALU.is_equal)
    I('dve', 'scalar_tensor_tensor', out=es2, in0=k1, scalar=-1e30, in1=es, op0=ALU.mult, op1=ALU.add)
    I('dve', 'reduce_max', out=m2, in_=es2, axis=AX.X)
    I('dve', 'tensor_tensor', out=k2, in0=es2, in1=m2.unsq(2).bc([128, T, 8]), op=ALU.is_equal)
    I('dve', 'tensor_tensor', out=dd, in0=m2, in1=m1, op=ALU.subtract)
    I('act', 'activation', out=ee, in_=dd, func=AF.Exp)
    I('dve', 'tensor_scalar', out=p1, in0=ee, scalar1=1.0, scalar2=None, op0=ALU.add)
    I('dve', 'reciprocal', out=p1, in_=p1)
    I('dve', 'tensor_tensor', out=p2, in0=ee, in1=p1, op=ALU.mult)
    I('dve', 'tensor_tensor', out=p1, in0=p1, in1=gp, op=ALU.mult)
    I('dve', 'tensor_tensor', out=p2, in0=p2, in1=gp, op=ALU.mult)
    I('dve', 'tensor_tensor', out=k1, in0=k1, in1=p1.unsq(2).bc([128, T, 8]), op=ALU.mult)
    I('dve', 'tensor_tensor', out=k2, in0=k2, in1=p2.unsq(2).bc([128, T, 8]), op=ALU.mult)
    I('dve', 'tensor_tensor', out=ew, in0=k1, in1=k2, op=ALU.add)
    I('dve', 'tensor_tensor', out=cmb.rearrange("p t (g e) -> p t g e", e=8), in0=oh.unsq(3).bc([128, T, 4, 8]),
      in1=ew.unsq(2).bc([128, T, 4, 8]), op=ALU.mult)


def build_program(n_layers=DEPTH, stop=None, debug=False):
    nc = bass.Bass("TRN2", target_bir_lowering=False)
    mk = MK(nc)
    I = mk.I

    def ext(name, shape, dt=F32):
        return mk.dram(name, shape, dt, kind="ExternalInput")

    xin = ext("xin", [NTOK, D])
    cT = ext("cT", [128, 16])
    w_ada = ext("w_ada", [DEPTH, D, 6 * D])
    b_ada = ext("b_ada", [DEPTH, 6 * D])
    g_mix = ext("g_mix", [DEPTH, D])
    g_ffn = ext("g_ffn", [DEPTH, D])
    w_in = ext("w_in", [DEPTH, D, D_IN])
    w_out = ext("w_out", [DEPTH, D, D])
    gm_vg = ext("gmlp_v_gain", [DEPTH, 256])
    gm_ws = ext("gmlp_ws", [DEPTH, 4, 128, 128])
    gm_bsT = ext("gmlp_bsT", [DEPTH, 128, 4])
    na_qg = ext("na_q_gain", [DEPTH, 64])
    na_kg = ext("na_k_gain", [DEPTH, 64])
    na_bias = ext("na_bias", [DEPTH, 16, 128, NH * 4 * 128])
    hy_sw = ext("hy_short_w", [DEPTH, 3, 768])
    hy_sb = ext("hy_short_b", [DEPTH, 768])
    hy_w1 = ext("hy_w1", [DEPTH, 33, 64])
    hy_w2 = ext("hy_w2", [DEPTH, 64, 64])
    hy_w3 = ext("hy_w3", [DEPTH, 64, 1024])
    hy_fb = ext("hy_fb", [DEPTH, 64, 4])
    hy_bias = ext("hy_bias", [DEPTH, 512])
    moe_wr = ext("moe_wr", [DEPTH, D, 36])
    moe_br = ext("moe_br", [DEPTH, 36])
    moe_wg = ext("moe_w_gate", [DEPTH, NEXP, D, FH])
    moe_wu = ext("moe_w_up", [DEPTH, NEXP, D, FH])
    moe_wd = ext("moe_w_down", [DEPTH, NEXP, FH, D])
    identd = ext("ident", [128, 128])
    cst = {}
    for L in (S, CTX):
        nT = L // 128
        cst[f'posT{L}'] = ext(f"posT{L}", [33, L])
        cst[f'decay{L}'] = ext(f"decay{L}", [128, nT, 256])
        for nm in ('Cf', 'Sf', 'Ci', 'Si'):
            cst[f'{nm}{L}'] = ext(f"{nm}{L}", [nT, 128, nT * 128], BF16)

    out = mk.dram("out", [S, D], F32, kind="ExternalOutput")
    dk = "ExternalOutput" if debug else "Internal"
    xs = mk.dram("xs", [NTOK, D], F32, kind=dk)
    pl = mk.dram("pl", [NTOK, D_IN], F32, kind=dk)
    mix = mk.dram("mix", [NTOK, D], BF16, kind=dk)
    vd = mk.dram("vd", [NTOK, NH * 65], BF16, kind=dk)
    md = mk.dram("md", [2, 6 * D], F32, kind=dk)
    cbd = mk.dram("cbd", [NEXP, NTOK], BF16, kind=dk)
    if debug:
        dbgc = mk.dram("dbgc", [32, NTOK], BF16, kind=dk)
        dbgh = mk.dram("dbgh", [128, 8 * 256], BF16, kind=dk)

    identf = mk.sb("identf", [128, 128], F32)
    identb = mk.sb("identb", [128, 128], BF16)
    onesf = mk.sb("onesf", [128, 128], F32)
    lc = mk.sb("lc", [128, 16, 128], BF16)
    pmask = mk.sb("pmask", [128, 1], F32)
    negpi = mk.sb("negpi", [128, 1], F32)

    def rstd_from_ss(ss_v, scale, eps, n):
        I('dve', 'tensor_scalar', out=ss_v, in0=ss_v, scalar1=scale, scalar2=eps, op0=ALU.mult, op1=ALU.add)
        I('act', 'activation', out=ss_v, in_=ss_v, func=AF.Sqrt)
        I('dve', 'reciprocal', out=ss_v, in_=ss_v)

    with mk.phase():
        ct = mk.sb("ct", [128, 16], F32)
        sct = mk.sb("sct", [128, 16], F32)
        mk.dma('sp', identf.v(), identd.v())
        mk.dma('pool', identb.v(), identd.v())
        mk.dma('sp', ct.v(), cT.v())
        I('act', 'activation', out=sct.v(), in_=ct.v(), func=AF.Silu)
        I('dve', 'tensor_copy', out=lc.v(), in_=sct.v().unsq(2).bc([128, 16, 128]))
        I('dve', 'memset', ap=onesf.v(), constant=1.0)
        I('dve', 'memset', ap=negpi.v(), constant=-math.pi)
        I('dve', 'tensor_scalar', out=pmask.v(), in0=identf[:, 0:1], scalar1=-1.0, scalar2=1.0, op0=ALU.mult, op1=ALU.add)

    def done():
        mk.wait_bufs('sp', [out, xs, pl, mix, vd, md, cbd])
        mk.build()
        return nc, mk

    for l in range(n_layers):
        last = (l == DEPTH - 1)
        xsrc = xin if l == 0 else xs
        n_tiles = 16 if last else 18

        with mk.phase():
            wa = [mk.sb(f"wa{i}", [128, 8, 512], BF16) for i in range(2)]
            mrow = mk.sb("mrow", [1, 2, 6 * D], F32)
            ba = mk.sb("ba", [1, 6 * D], F32)
            gg = mk.sb("gg", [1, 2, D], F32)
            pm = [mk.ps(f"pm{i}", [128, 512], F32) for i in range(4)]
            mk.dma('sp', ba.v(), b_ada[l:l + 1, :])
            mk.dma('sp', gg[:, 0, :], g_mix[l:l + 1, :])
            mk.dma('sp', gg[:, 1, :], g_ffn[l:l + 1, :])
            for n in range(12):
                w = wa[n % 2]
                mk.dma('pool', w.v(), w_ada[l, :, n * 512:(n + 1) * 512].rearrange("(c p) n -> p c n", p=128))
                for s in range(2):
                    p = pm[(2 * n + s) % 4]
                    for c in range(8):
                        I('pe', 'matmul', out=p.v(), lhsT=lc[:, s * 8 + c, :], rhs=w[:, c, :], start=(c == 0), stop=(c == 7))
                    I('dve', 'tensor_tensor', out=mrow[:, s, n * 512:(n + 1) * 512], in0=p[0:1, :],
                      in1=ba[:, n * 512:(n + 1) * 512], op=ALU.add)
            for s in range(2):
                for (slot, g) in ((1, 0), (4, 1)):
                    I('dve', 'scalar_tensor_tensor', out=mrow[:, s, slot * D:(slot + 1) * D],
                      in0=mrow[:, s, slot * D:(slot + 1) * D], scalar=1.0, in1=gg[:, g, :], op0=ALU.add, op1=ALU.mult)
                mk.dma('sp', md[s:s + 1, :], mrow[:, s, :])
        if stop == f"p1_{l}":
            return done()

        def load_mod(tile_v, s, slot):
            mk.dma('sp', tile_v, md[s, slot * D:(slot + 1) * D].pbc(128))

        with mk.phase():
            win = mk.sb("win", [128, 8, D_IN], BF16)
            wst = [mk.sb(f"wst{i}", [128, D_IN], F32) for i in range(2)]
            for c in range(8):
                mk.dma('sp', wst[c % 2].v(), w_in[l, c * 128:(c + 1) * 128, :])
                if c % 4 == 3:
                    I('pool', 'tensor_copy', out=win[:, c, :], in_=wst[c % 2].v())
                elif c % 2 == 0:
                    I('act', 'activation', out=win[:, c, :], in_=wst[c % 2].v(), func=AF.Copy)
                else:
                    I('dve', 'tensor_copy', out=win[:, c, :], in_=wst[c % 2].v())
            gsb = [mk.sb(f"gsb{s}", [128, D], F32) for s in range(2)]
            shb = [mk.sb(f"shb{s}", [128, D], F32) for s in range(2)]
            for s in range(2):
                load_mod(gsb[s].v(), s, 1)
                load_mod(shb[s].v(), s, 0)
            xb = [mk.sb(f"xb{i}", [128, D], F32) for i in range(2)]
            junk = mk.sb("junk", [128, D], BF16)
            ssb = [mk.sb(f"ss{i}", [128, 1], F32) for i in range(2)]
            hf = mk.sb("hf", [128, D], F32)
            hb = [mk.sb(f"hb{i}", [128, D], BF16) for i in range(2)]
            hT = [mk.sb(f"hT{i}", [128, 8, 128], BF16) for i in range(2)]
            plt = [mk.sb(f"plt{i}", [128, D_IN], F32) for i in range(3)]
            pt = [mk.ps(f"pt{i}", [128, 1024], BF16) for i in range(2)]
            po = [mk.ps(f"po{i}", [128, 512], F32) for i in range(4)]
            kctr = [0]

            def p2_front(tt):
                s = 0 if tt < 16 else 1
                x_, ss_, hb_, hT_, pt_ = xb[tt % 2], ssb[tt % 2], hb[tt % 2], hT[tt % 2], pt[tt % 2]
                mk.dma('sp', x_.v(), xsrc[tt * 128:(tt + 1) * 128, :])
                I('act', 'activation', out=junk.v(), in_=x_.v(), func=AF.Square, accum_out=ss_.v())
                rstd_from_ss(ss_.v(), 1.0 / D, RMS_EPS, 1)
                I('dve', 'scalar_tensor_tensor', out=hf.v(), in0=x_.v(), scalar=ss_.v(), in1=gsb[s].v(), op0=ALU.mult, op1=ALU.mult)
                I('dve', 'tensor_tensor', out=hb_.v(), in0=hf.v(), in1=shb[s].v(), op=ALU.add)
                for c in range(8):
                    I('pe', 'transpose', out=pt_[:, c * 128:(c + 1) * 128], in_=hb_[:, c * 128:(c + 1) * 128], identity=identb.v())
                I('act', 'activation', out=hT_.v().rearrange("p c t -> p (c t)"), in_=pt_.v(), func=AF.Copy)

            def p2_back(tt):
                hT_, plt_ = hT[tt % 2], plt[tt % 3]
                for n in range(6):
                    n0 = n * 512
                    wdt = min(512, D_IN - n0)
                    p = po[kctr[0] % 4]
                    kctr[0] += 1
                    for c in range(8):
                        I('pe', 'matmul', out=p[:, 0:wdt], lhsT=hT_[:, c, :], rhs=win[:, c, n0:n0 + wdt], start=(c == 0), stop=(c == 7))
                    if n == 0:
                        I('act', 'activation', out=plt_[:, n0:n0 + wdt], in_=p[:, 0:wdt], func=AF.Gelu_apprx_tanh)
                    elif n % 2 == 0:
                        I('dve', 'tensor_copy', out=plt_[:, n0:n0 + wdt], in_=p[:, 0:wdt])
                    else:
                        I('act', 'activation', out=plt_[:, n0:n0 + wdt], in_=p[:, 0:wdt], func=AF.Copy)
                mk.dma('sp', pl[tt * 128:(tt + 1) * 128, :], plt_.v())

            for tt in range(19):
                if tt < 18:
                    p2_front(tt)
                if tt >= 1:
                    p2_back(tt - 1)
        if stop == f"p2_{l}":
            return done()

        with mk.phase():
            wsf = mk.sb("wsf", [128, 4, 128], F32)
            wsT = mk.sb("wsT", [128, 4, 128], BF16)
            bsT = mk.sb("bsT", [128, 4], F32)
            vg = mk.sb("vg", [128, 256], F32)
            pw = mk.ps("pw", [128, 512], F32)
            mk.dma('sp', wsf.v(), gm_ws[l].rearrange("g i j -> i g j"))
            mk.dma('sp', bsT.v(), gm_bsT[l])
            mk.dma('sp', vg.v(), gm_vg[l, :].pbc(128))
            for g in range(4):
                I('pe', 'transpose', out=pw[:, g * 128:(g + 1) * 128], in_=wsf[:, g, :], identity=identf.v())
            I('dve', 'tensor_copy', out=wsT.v().rearrange("p g i -> p (g i)"), in_=pw.v())
            guall = mk.sb("guall", [128, n_tiles, 512], F32)
            for t0_ in range(0, n_tiles, 6):
                t1_ = min(n_tiles, t0_ + 6)
                mk.dma('sp', guall[:, t0_:t1_, :], pl[t0_ * 128:t1_ * 128, 0:512].rearrange("(t p) n -> p t n", p=128))
            st4 = [mk.sb(f"st4{i}", [128, 4], F32) for i in range(2)]
            vr4 = [mk.sb(f"vr4{i}", [128, 4], F32) for i in range(2)]
            vc = mk.sb("vc", [128, 4, 64], F32)
            sq = mk.sb("sq", [128, 4, 64], F32)
            vnb = [mk.sb(f"vnb{i}", [128, 256], BF16) for i in range(2)]
            mo = [mk.sb(f"mo{i}", [128, 256], BF16) for i in range(3)]
            pg = [mk.ps(f"pg{i}", [128, 256], F32) for i in range(2)]
            for tt in range(n_tiles):
                m4, v4, vn_, mo_, pg_ = st4[tt % 2], vr4[tt % 2], vnb[tt % 2], mo[tt % 3], pg[tt % 2]
                gu = guall[:, tt, :]
                v3 = gu[:, 256:512].rearrange("p (g d) -> p g d", d=64)
                I('dve', 'reduce_sum', out=m4.v(), in_=v3, axis=AX.X)
                I('dve', 'tensor_scalar', out=m4.v(), in0=m4.v(), scalar1=-1.0 / 64, scalar2=None, op0=ALU.mult)
                I('dve', 'tensor_tensor', out=vc.v(), in0=v3, in1=m4.v().unsq(2).bc([128, 4, 64]), op=ALU.add)
                I('dve', 'tensor_tensor', out=sq.v(), in0=vc.v(), in1=vc.v(), op=ALU.mult)
                I('dve', 'reduce_sum', out=v4.v(), in_=sq.v(), axis=AX.X)
                rstd_from_ss(v4.v(), 1.0 / 64, LN_EPS, 4)
                I('dve', 'tensor_tensor', out=vc.v(), in0=vc.v(), in1=v4.v().unsq(2).bc([128, 4, 64]), op=ALU.mult)
                I('dve', 'tensor_tensor', out=vn_.v(), in0=vc.v().rearrange("p g d -> p (g d)"), in1=vg.v(), op=ALU.mult)
                for g in range(4):
                    I('pe', 'matmul', out=pg_[:, g * 64:(g + 1) * 64], lhsT=wsT[:, g, :], rhs=vn_[:, g * 64:(g + 1) * 64], start=True, stop=True)
                for g in range(4):
                    I('dve', 'scalar_tensor_tensor', out=mo_[:, g * 64:(g + 1) * 64], in0=pg_[:, g * 64:(g + 1) * 64],
                      scalar=bsT[:, g:g + 1], in1=gu[:, g * 64:(g + 1) * 64], op0=ALU.add, op1=ALU.mult)
                mk.dma('sp', mix[tt * 128:(tt + 1) * 128, 0:256], mo_.v())
        if stop == f"p3_{l}":
            return done()

        with mk.phase():
            qT = mk.sb("qT", [128, 4, NTOK], BF16)
            kT = mk.sb("kT", [128, 4, NTOK], BF16)
            with mk.phase():
                gqk = mk.sb("gqk", [128, 16, 64], F32)
                mk.dma('sp', gqk[:, 0:8, :], na_qg.custom(l * 64, [[0, 128], [0, 8], [1, 64]]))
                mk.dma('sp', gqk[:, 8:16, :], na_kg.custom(l * 64, [[0, 128], [0, 8], [1, 64]]))
                I('dve', 'tensor_scalar', out=gqk[:, 0:8, :], in0=gqk[:, 0:8, :], scalar1=DH ** -0.5, scalar2=None, op0=ALU.mult)
                qkv = [mk.sb(f"qkv{i}", [128, 1536], F32) for i in range(2)]
                sq16 = mk.sb("sq16", [128, 16, 64], F32)
                r16 = [mk.sb(f"r16{i}", [128, 16], F32) for i in range(2)]
                nb = [mk.sb(f"nb{i}", [128, 1024], BF16) for i in range(2)]
                vb = [mk.sb(f"vb{i}", [128, 8, 65], BF16) for i in range(3)]
                ptq = [mk.ps(f"ptq{i}", [128, 1024], BF16) for i in range(2)]
                for i in range(3):
                    I('dve', 'memset', ap=vb[i][:, :, 64:65], constant=1.0)

                def p4_front(tt):
                    t_, r_, nb_ = qkv[tt % 2], r16[tt % 2], nb[tt % 2]
                    mk.dma('sp', t_.v(), pl[tt * 128:(tt + 1) * 128, 512:2048])
                    t3 = t_[:, 0:1024].rearrange("p (h d) -> p h d", d=64)
                    I('pool', 'tensor_tensor', out=sq16.v(), in0=t3, in1=t3, op=ALU.mult)
                    I('dve', 'reduce_sum', out=r_.v(), in_=sq16.v(), axis=AX.X)
                    rstd_from_ss(r_.v(), 1.0 / DH, RMS_EPS, 16)
                    I('dve', 'tensor_tensor', out=sq16.v(), in0=t3, in1=r_.v().unsq(2).bc([128, 16, 64]), op=ALU.mult)
                    I('dve', 'tensor_tensor', out=nb_.v().rearrange("p (h d) -> p h d", d=64), in0=sq16.v(), in1=gqk.v(), op=ALU.mult)

                def p4_back(tt):
                    t_, nb_, p_, vb_ = qkv[tt % 2], nb[tt % 2], ptq[tt % 2], vb[tt % 3]
                    for c in range(8):
                        I('pe', 'transpose', out=p_[:, c * 128:(c + 1) * 128], in_=nb_[:, c * 128:(c + 1) * 128], identity=identb.v())
                    I('act', 'activation', out=qT[:, :, tt * 128:(tt + 1) * 128], in_=p_[:, 0:512].rearrange("p (c t) -> p c t", t=128), func=AF.Copy)
                    I('act', 'activation', out=kT[:, :, tt * 128:(tt + 1) * 128], in_=p_[:, 512:1024].rearrange("p (c t) -> p c t", t=128), func=AF.Copy)
                    I('act', 'activation', out=vb_[:, :, 0:64], in_=t_[:, 1024:1536].rearrange("p (h d) -> p h d", d=64), func=AF.Copy)
                    mk.dma('sp', vd[tt * 128:(tt + 1) * 128, :], vb_.v().rearrange("p h d -> p (h d)"))

                for tt in range(19):
                    if tt < 18:
                        p4_front(tt)
                    if tt >= 1:
                        p4_back(tt - 1)
            vctx = mk.sb("vctx", [128, 2, NH, 65], BF16)
            mk.dma('sp', vctx.v().rearrange("p c h d -> p c (h d)"), vd[S:NTOK, :].rearrange("(c p) n -> p c n", p=128))
            btb = [mk.sb(f"bt{i}", [128, NH, 4, 128], F32) for i in range(2)]
            vwb = [mk.sb(f"vw{i}", [128, 4, NH, 65], BF16) for i in range(2)]
            obb = [mk.sb(f"ob{i}", [128, 512], BF16) for i in range(2)]
            kbb = [mk.sb(f"kb{i}", [128, 4, 512], BF16) for i in range(2)]
            qbb = [mk.sb(f"qb{i}", [128, 4, 128], BF16) for i in range(2)]
            pTb = [mk.sb(f"pT{i}", [128, 768], BF16) for i in range(3)]
            sTb = [mk.sb(f"sT{i}", [128, 512], F32) for i in range(3)]
            rcp = [mk.sb(f"rcp{i}", [128, 1], F32) for i in range(2)]
            psw = [mk.ps(f"psw{i}", [128, 512], F32) for i in range(3)]
            psc = [mk.ps(f"psc{i}", [128, 256], F32) for i in range(3)]
            pov = [mk.ps(f"pov{i}", [128, 65], F32) for i in range(2)]
            info = _na_tables()
            nblk = 16 + (0 if last else 2)
            vmap = {0: 0, 1: 1, 2: 1, 3: 2}
            border = sorted(range(16), key=lambda b: (vmap[b // 4], vmap[b % 4], b)) + list(range(16, nblk))
            units = [(k, h) for k in range(nblk) for h in range(NH)]
            bias_buf = {}
            nbias = [0]

            def variant(blk):
                return (vmap[blk // 4], vmap[blk % 4])

            def load_block(k):
                blk = border[k]
                r0, c0, qr0, qc0 = info[blk][:4]
                vw = vwb[k % 2]
                if k == 0 or variant(blk) != variant(border[k - 1]):
                    nbias[0] += 1
                    bt = btb[nbias[0] % 2]
                    mk.dma('sp', bt.v().rearrange("p h c q -> p (h c q)"), na_bias[l, blk])
                    bias_buf[k] = bt
                else:
                    bias_buf[k] = bias_buf[k - 1]
                vd3 = vd.v().rearrange("(r c) n -> r c n", c=GRID_W)[r0:r0 + 16, c0:c0 + 32, :]
                vd4 = vd3.rearrange("(ch rr) x n -> rr x ch n", rr=4)
                for rr in range(4):
                    mk.dma('sp', vw[rr * 32:(rr + 1) * 32, :, :, :].rearrange("p c h d -> p c (h d)"), vd4[rr], nowaw=(rr > 0))
                kb, qb = kbb[k % 2], qbb[k % 2]
                for hp_ in range(4):
                    k3 = kT[:, hp_, 0:S].rearrange("p (r c) -> p r c", c=GRID_W)[:, r0:r0 + 16, c0:c0 + 32]
                    if hp_ < 2:
                        I('pool', 'tensor_copy', out=kb[:, hp_, :].rearrange("p (r c) -> p r c", c=32), in_=k3)
                    else:
                        I('act', 'activation', out=kb[:, hp_, :].rearrange("p (r c) -> p r c", c=32), in_=k3, func=AF.Copy)
                q4 = qT[:, :, 0:S].rearrange("p h (r c) -> p h r c", c=GRID_W)[:, :, qr0:qr0 + 8, qc0:qc0 + 16]
                I('pool', 'tensor_copy', out=qb.v().rearrange("p h (r c) -> p h r c", c=16), in_=q4)

            def front(i):
                k, h = units[i]
                blk = border[k]
                hp, base = h // 2, (h % 2) * 64
                pw_, pc_, pT_, sT_ = psw[i % 3], psc[i % 3], pTb[i % 3], sTb[i % 3]
                if blk < 16:
                    kb, qb, bt = kbb[k % 2], qbb[k % 2], bias_buf[k]
                    q3 = qb[base:base + 64, hp, :]
                    for c in range(4):
                        I('pe', 'matmul', out=pw_[:, c * 128:(c + 1) * 128], lhsT=kb[base:base + 64, hp, c * 128:(c + 1) * 128], rhs=q3, start=True, stop=True)
                    for c in range(2):
                        I('pe', 'matmul', out=pc_[:, c * 128:(c + 1) * 128], lhsT=kT[base:base + 64, hp, S + c * 128:S + (c + 1) * 128], rhs=q3, start=True, stop=True)
                    I('dve', 'tensor_tensor', out=sT_.v(), in0=pw_.v(), in1=bt[:, h, :, :].rearrange("p c q -> p (c q)"), op=ALU.add)
                    I('act', 'activation', out=pT_[:, 0:512], in_=sT_.v(), func=AF.Exp)
                    I('act', 'activation', out=pT_[:, 512:768], in_=pc_.v(), func=AF.Exp)
                else:
                    qt = blk - 16
                    q2 = qT[base:base + 64, hp, S + qt * 128:S + (qt + 1) * 128]
                    for c in range(2):
                        I('pe', 'matmul', out=pc_[:, c * 128:(c + 1) * 128], lhsT=kT[base:base + 64, hp, S + c * 128:S + (c + 1) * 128],
                          rhs=q2, start=True, stop=True)
                    I('act', 'activation', out=pT_[:, 512:768], in_=pc_.v(), func=AF.Exp)

            def back(i):
                k, h = units[i]
                blk = border[k]
                po_, pT_, rc_, ob = pov[i % 2], pTb[i % 3], rcp[i % 2], obb[k % 2]
                if blk < 16:
                    vw = vwb[k % 2]
                    for c in range(6):
                        rhs = vw[:, c, h, :] if c < 4 else vctx[:, c - 4, h, :]
                        I('pe', 'matmul', out=po_.v(), lhsT=pT_[:, c * 128:(c + 1) * 128], rhs=rhs, start=(c == 0), stop=(c == 5))
                else:
                    for c in range(2):
                        I('pe', 'matmul', out=po_.v(), lhsT=pT_[:, 512 + c * 128:512 + (c + 1) * 128], rhs=vctx[:, c, h, :], start=(c == 0), stop=(c == 1))
                I('dve', 'reciprocal', out=rc_.v(), in_=po_[:, 64:65])
                I('dve', 'tensor_scalar', out=ob[:, h * 64:(h + 1) * 64], in0=po_[:, 0:64], scalar1=rc_.v(), scalar2=None, op0=ALU.mult)
                if h == NH - 1:
                    if blk < 16:
                        qr0, qc0 = info[blk][2:4]
                        for a in range(8):
                            t0 = (qr0 + a) * GRID_W + qc0
                            mk.dma('sp', mix[t0:t0 + 16, 256:768], ob[a * 16:(a + 1) * 16, :], force=True, nowaw=(a > 0))
                    else:
                        t0 = S + (blk - 16) * 128
                        mk.dma('sp', mix[t0:t0 + 128, 256:768], ob.v(), force=True)

            load_block(0)
            for i in range(len(units) + 2):
                if i < len(units):
                    front(i)
                if i >= 2:
                    back(i - 2)
                if i < len(units):
                    k, h = units[i]
                    if h == 2 and k + 1 < 16:
                        load_block(k + 1)
        if stop == f"p5_{l}":
            return done()

        streams = [dict(L=S, row0=0, tag='l')]
        if not last:
            streams.append(dict(L=CTX, row0=S, tag='c'))
        for st in streams:
            st['nT'] = st['L'] // 128
        with mk.phase():
            for st in streams:
                st['Kr'] = mk.sb("Kr" + st['tag'], [128, st['nT'], 512], BF16)
                st['Ks'] = mk.sb("Ks" + st['tag'], [128, st['nT'], 512], BF16)
            with mk.phase():
                w1 = mk.sb("w1", [33, 64], F32)
                w2 = mk.sb("w2", [64, 64], F32)
                w3 = mk.sb("w3", [64, 1024], F32)
                fb = mk.sb("fb", [64, 4], F32)
                sc = mk.sb("sc", [64, 2], F32)
                of = mk.sb("of", [64, 2], F32)
                mk.dma('sp', w1.v(), hy_w1[l])
                mk.dma('sp', w2.v(), hy_w2[l])
                mk.dma('sp', w3.v(), hy_w3[l])
                mk.dma('sp', fb.v(), hy_fb[l])
                for st in streams:
                    L, nT, tg = st['L'], st['nT'], st['tag']
                    st['posT'] = mk.sb("posT" + tg, [33, L], F32)
                    st['dec'] = mk.sb("dec" + tg, [128, nT, 256], F32)
                    st['h2T'] = mk.sb("h2T" + tg, [64, L], F32)
                    st['Ab'] = mk.sb("Ab" + tg, [128, nT, 512], BF16)
                    st['Bb'] = mk.sb("Bb" + tg, [128, nT, 512], BF16)
                    st['rn'] = mk.sb("rn" + tg, [128, 2, 256], F32)
                    st['pS'] = [mk.ps(f"pS{tg}{i}", [128, 512], F32) for i in range(2)]
                    mk.dma('sp', st['posT'].v(), cst[f'posT{L}'].v())
                    mk.dma('sp', st['dec'].v(), cst[f'decay{L}'].v())
                I('dve', 'tensor_scalar', out=sc.v(), in0=fb[:, 0:2], scalar1=1.0 / 3.0, scalar2=None, op0=ALU.mult)
                I('dve', 'tensor_tensor', out=of.v(), in0=fb[:, 2:4], in1=sc.v(), op=ALU.mult)
                u1b = [mk.sb(f"u1{i}", [64, 512], F32) for i in range(2)]
                u2b = [mk.sb(f"u2{i}", [64, 512], F32) for i in range(2)]
                hrot = [mk.sb(f"hrot{i}", [128, 1024], F32) for i in range(2)]
                habs = [mk.sb(f"habs{i}", [128, 1024], F32) for i in range(2)]
                ph = [mk.ps(f"ph{i}", [128, 512], F32) for i in range(2)]
                p3 = [mk.ps(f"p3{i}", [128, 512], F32) for i in range(2)]
                chunks = []
                for st in streams:
                    CW = min(512, st['L'])
                    for pc in range(st['L'] // CW):
                        chunks.append((st, slice(pc * CW, (pc + 1) * CW), CW))
                h1c = [mk.sb(f"h1c{i}", [64, 512], F32) for i in range(len(chunks))]
                for k_ in range(2):
                    wk = (w1, w2)[k_]
                    for ci, (st, cs, CW) in enumerate(chunks):
                        u1, u2, p_ = u1b[ci % 2], u2b[ci % 2], ph[ci % 2]
                        src = st['posT'][:, cs] if k_ == 0 else h1c[ci][:, 0:CW]
                        dst = h1c[ci][:, 0:CW] if k_ == 0 else st['h2T'][:, cs]
                        I('pe', 'matmul', out=p_[0:64, 0:CW], lhsT=wk.v(), rhs=src, start=True, stop=True)
                        I('act', 'activation', out=u1[:, 0:CW], in_=p_[0:64, 0:CW], func=AF.Sin, bias=of[:, k_:k_ + 1], scale=sc[:, k_:k_ + 1])
                        I('dve', 'tensor_tensor', out=u2[:, 0:CW], in0=u1[:, 0:CW], in1=u1[:, 0:CW], op=ALU.mult)
                        I('dve', 'tensor_scalar', out=u2[:, 0:CW], in0=u2[:, 0:CW], scalar1=-4.0, scalar2=3.0, op0=ALU.mult, op1=ALU.add)
                        I('dve', 'tensor_tensor', out=dst, in0=u1[:, 0:CW], in1=u2[:, 0:CW], op=ALU.mult)
                kq = 0
                for st in streams:
                    nT = st['nT']
                    for tc in range(nT):
                        hr, ha = hrot[kq % 2], habs[kq % 2]
                        kq += 1
                        for hf_ in range(2):
                            p = p3[hf_]
                            I('pe', 'matmul', out=p.v(), lhsT=st['h2T'][:, tc * 128:(tc + 1) * 128], rhs=w3[:, hf_ * 512:(hf_ + 1) * 512], start=True, stop=True)
                            I('dve', 'tensor_tensor', out=hr[:, hf_ * 512:(hf_ + 1) * 512].rearrange("p (a c) -> p a c", c=256),
                              in0=p.v().rearrange("p (a c) -> p a c", c=256), in1=st['dec'][:, tc, :].unsq(1).bc([128, 2, 256]), op=ALU.mult)
                        I('act', 'activation', out=ha.v(), in_=hr.v(), func=AF.Abs)
                        for hf_ in range(2):
                            I('pe', 'matmul', out=st['pS'][hf_].v(), lhsT=onesf.v(), rhs=ha[:, hf_ * 512:(hf_ + 1) * 512], start=(tc == 0), stop=(tc == nT - 1))
                        h4 = hr.v().rearrange("p (o d c) -> p o d c", o=2, d=2)
                        if tc == 0:
                            I('dve', 'tensor_scalar', out=h4[:, :, 1, :], in0=h4[:, :, 1, :], scalar1=pmask.v(), scalar2=None, op0=ALU.mult)
                        I('dve', 'tensor_tensor', out=st['Ab'][:, tc, :].rearrange("p (o c) -> p o c", o=2), in0=h4[:, :, 0, :], in1=h4[:, :, 1, :], op=ALU.add)
                        I('pool', 'tensor_tensor', out=st['Bb'][:, tc, :].rearrange("p (o c) -> p o c", o=2), in0=h4[:, :, 0, :], in1=h4[:, :, 1, :], op=ALU.subtract)
                for st in streams:
                    rn = st['rn']
                    for o in range(2):
                        I('dve', 'tensor_copy', out=rn[:, o, :], in_=st['pS'][o][:, 0:256])
                        I('dve', 'tensor_tensor', out=rn[:, o, :], in0=rn[:, o, :], in1=st['pS'][o][:, 256:512], op=ALU.add)
                    I('dve', 'reciprocal', out=rn.v(), in_=rn.v())
                cfb = [mk.sb(f"cfk{i}", [128, 16, 128], BF16) for i in range(2)]
                sfb = [mk.sb(f"sfk{i}", [128, 16, 128], BF16) for i in range(2)]
                kq = 0
                for st in streams:
                    L, nT = st['L'], st['nT']
                    rnf = st['rn'].v().rearrange("p o c -> p (o c)")
                    for fc in range(nT):
                        cf, sf = cfb[kq % 2], sfb[kq % 2]
                        kq += 1
                        mk.dma('sp', cf[:, 0:nT, :].rearrange("p t f -> p (t f)"), cst[f'Cf{L}'][fc])
                        mk.dma('sp', sf[:, 0:nT, :].rearrange("p t f -> p (t f)"), cst[f'Sf{L}'][fc])
                        for tc in range(nT):
                            I('pe', 'matmul', out=ph[0].v(), lhsT=cf[:, tc, :], rhs=st['Ab'][:, tc, :], start=(tc == 0), stop=(tc == nT - 1))
                        for tc in range(nT):
                            I('pe', 'matmul', out=ph[1].v(), lhsT=sf[:, tc, :], rhs=st['Bb'][:, tc, :], start=(tc == 0), stop=(tc == nT - 1))
                        I('dve', 'tensor_tensor', out=st['Kr'][:, fc, :], in0=ph[0].v(), in1=rnf, op=ALU.mult)
                        I('dve', 'tensor_tensor', out=st['Ks'][:, fc, :], in0=ph[1].v(), in1=rnf, op=ALU.mult)
            swb = mk.sb("swb", [128, 3, 768], F32)
            sbb = mk.sb("sbb", [128, 768], F32)
            dbb = mk.sb("dbb", [128, 512], F32)
            mk.dma('sp', swb.v().rearrange("p k n -> p (k n)"), hy_sw[l].rearrange("k n -> (k n)").pbc(128))
            mk.dma('sp', sbb.v(), hy_sb[l, :].pbc(128))
            mk.dma('sp', dbb.v(), hy_bias[l, :].pbc(128))
            for st in streams:
                nT, tg = st['nT'], st['tag']
                st['zf'] = mk.sb("zf" + tg, [128, nT, 256], F32)
                st['zb'] = mk.sb("zb" + tg, [128, nT, 256], BF16)
                st['gts'] = mk.sb("gts" + tg, [128, nT, 512], F32)
                st['Yr'] = mk.sb("Yr" + tg, [128, nT, 256], BF16)
                st['Ys'] = mk.sb("Ys" + tg, [128, nT, 256], BF16)
            a3 = [mk.sb(f"a3{i}", [128, 3, 768], F32) for i in range(3)]
            accb = [mk.sb(f"acc{i}", [128, 768], F32) for i in range(2)]
            kq = 0
            for st in streams:
                nT, row0 = st['nT'], st['row0']
                for tc in range(nT):
                    a_, acc = a3[kq % 3], accb[kq % 2]
                    kq += 1
                    g0 = row0 + tc * 128
                    if tc == 0:
                        I('dve', 'memset', ap=a_[0:1, 0, :], constant=0.0)
                    if tc == nT - 1:
                        I('dve', 'memset', ap=a_[:, 2, :], constant=0.0)
                    mk.dma('sp', a_[:, 1, :], pl[g0:g0 + 128, 2048:2816])
                    if tc == 0:
                        mk.dma('sp', a_[1:128, 0, :], pl[g0:g0 + 127, 2048:2816], nowaw=True)
                    else:
                        mk.dma('sp', a_[:, 0, :], pl[g0 - 1:g0 + 127, 2048:2816], nowaw=True)
                    if tc == nT - 1:
                        mk.dma('sp', a_[0:127, 2, :], pl[g0 + 1:g0 + 128, 2048:2816], nowaw=True)
                    else:
                        mk.dma('sp', a_[:, 2, :], pl[g0 + 1:g0 + 129, 2048:2816], nowaw=True)
                    I('dve', 'tensor_tensor', out=acc.v(), in0=a_[:, 1, :], in1=swb[:, 1, :], op=ALU.mult)
                    I('dve', 'tensor_tensor', out=acc.v(), in0=acc.v(), in1=sbb.v(), op=ALU.add)
                    I('pool', 'tensor_tensor', out=a_[:, 0, :], in0=a_[:, 0, :], in1=swb[:, 0, :], op=ALU.mult)
                    I('pool', 'tensor_tensor', out=a_[:, 2, :], in0=a_[:, 2, :], in1=swb[:, 2, :], op=ALU.mult)
                    I('dve', 'tensor_tensor', out=acc.v(), in0=acc.v(), in1=a_[:, 0, :], op=ALU.add)
                    I('dve', 'tensor_tensor', out=acc.v(), in0=acc.v(), in1=a_[:, 2, :], op=ALU.add)
                    I('act', 'activation', out=st['zf'][:, tc, :], in_=acc[:, 0:256], func=AF.Copy)
                    I('act', 'activation', out=st['zb'][:, tc, :], in_=acc[:, 0:256], func=AF.Copy)
                    I('act', 'activation', out=st['gts'][:, tc, :], in_=acc[:, 256:768], func=AF.Copy)
            cfb = [mk.sb(f"cf{i}", [128, 16, 128], BF16) for i in range(2)]
            sfb = [mk.sb(f"sf{i}", [128, 16, 128], BF16) for i in range(2)]
            zr = [mk.sb(f"zr{i}", [128, 256], F32) for i in range(2)]
            zs = [mk.sb(f"zs{i}", [128, 256], F32) for i in range(2)]
            t1 = mk.sb("t1", [128, 256], F32)
            t2 = mk.sb("t2", [128, 256], F32)
            t3 = mk.sb("t3", [128, 256], F32)
            t4 = mk.sb("t4", [128, 256], F32)
            dzb = [mk.sb(f"dz{i}", [128, 256], F32) for i in range(2)]
            pz = [mk.ps(f"pz{i}", [128, 256], F32) for i in range(4)]
            py = [mk.ps(f"py{i}", [128, 256], F32) for i in range(2)]
            kq = 0
            for n in range(2):
                ksl = slice(n * 256, (n + 1) * 256)
                for st in streams:
                    L, nT = st['L'], st['nT']
                    Kr, Ks, zb, Yr, Ys = st['Kr'], st['Ks'], st['zb'], st['Yr'], st['Ys']
                    for fc in range(nT):
                        cf, sf = cfb[kq % 2], sfb[kq % 2]
                        pzr, pzs = pz[(kq % 2) * 2], pz[(kq % 2) * 2 + 1]
                        zr_, zs_ = zr[kq % 2], zs[kq % 2]
                        kq += 1
                        mk.dma('sp', cf[:, 0:nT, :].rearrange("p t f -> p (t f)"), cst[f'Cf{L}'][fc])
                        mk.dma('sp', sf[:, 0:nT, :].rearrange("p t f -> p (t f)"), cst[f'Sf{L}'][fc])
                        for tc in range(nT):
                            I('pe', 'matmul', out=pzr.v(), lhsT=cf[:, tc, :], rhs=zb[:, tc, :], start=(tc == 0), stop=(tc == nT - 1))
                        for tc in range(nT):
                            I('pe', 'matmul', out=pzs.v(), lhsT=sf[:, tc, :], rhs=zb[:, tc, :], start=(tc == 0), stop=(tc == nT - 1))
                        I('act', 'activation', out=zr_.v(), in_=pzr.v(), func=AF.Copy)
                        I('act', 'activation', out=zs_.v(), in_=pzs.v(), func=AF.Copy)
                        I('pool', 'tensor_tensor', out=t1.v(), in0=zr_.v(), in1=Kr[:, fc, ksl], op=ALU.mult)
                        I('pool', 'tensor_tensor', out=t2.v(), in0=zs_.v(), in1=Ks[:, fc, ksl], op=ALU.mult)
                        I('pool', 'tensor_tensor', out=Yr[:, fc, :], in0=t1.v(), in1=t2.v(), op=ALU.subtract)
                        I('dve', 'tensor_tensor', out=t3.v(), in0=zr_.v(), in1=Ks[:, fc, ksl], op=ALU.mult)
                        I('dve', 'tensor_tensor', out=t4.v(), in0=zs_.v(), in1=Kr[:, fc, ksl], op=ALU.mult)
                        I('dve', 'tensor_tensor', out=Ys[:, fc, :], in0=t3.v(), in1=t4.v(), op=ALU.add)
                for st in streams:
                    L, nT = st['L'], st['nT']
                    zf, zb, gts, Yr, Ys = st['zf'], st['zb'], st['gts'], st['Yr'], st['Ys']
                    for tc in range(nT):
                        ci, si = cfb[kq % 2], sfb[kq % 2]
                        p, dz = py[kq % 2], dzb[kq % 2]
                        kq += 1
                        mk.dma('sp', ci[:, 0:nT, :].rearrange("p t f -> p (t f)"), cst[f'Ci{L}'][tc])
                        mk.dma('sp', si[:, 0:nT, :].rearrange("p t f -> p (t f)"), cst[f'Si{L}'][tc])
                        for fc in range(nT):
                            I('pe', 'matmul', out=p.v(), lhsT=ci[:, fc, :], rhs=Yr[:, fc, :], start=(fc == 0), stop=False)
                        for fc in range(nT):
                            I('pe', 'matmul', out=p.v(), lhsT=si[:, fc, :], rhs=Ys[:, fc, :], start=False, stop=(fc == nT - 1))
                        I('pool', 'tensor_tensor', out=dz.v(), in0=zf[:, tc, :], in1=dbb[:, ksl], op=ALU.mult)
                        I('dve', 'scalar_tensor_tensor', out=zf[:, tc, :], in0=p.v(), scalar=1.0 / L, in1=dz.v(), op0=ALU.mult, op1=ALU.add)
                        I('dve', 'tensor_tensor', out=zf[:, tc, :], in0=zf[:, tc, :], in1=gts[:, tc, ksl], op=ALU.mult)
                        I('act', 'activation', out=zb[:, tc, :], in_=zf[:, tc, :], func=AF.Copy)
            for st in streams:
                L, row0 = st['L'], st['row0']
                mk.dma('sp', mix[row0:row0 + L, 768:1024].rearrange("(t p) c -> p t c", p=128), st['zb'].v())
        if stop == f"p6_{l}":
            return done()

        with mk.phase():
            wo = mk.sb("wo", [128, 8, D], BF16)
            wst = [mk.sb(f"wst{i}", [128, 2, D], F32) for i in range(2)]
            for c2 in range(4):
                mk.dma('sp', wst[c2 % 2].v(), w_out[l, c2 * 256:(c2 + 1) * 256, :].rearrange("(c p) n -> p c n", p=128))
                if c2 % 2 == 0:
                    I('act', 'activation', out=wo[:, c2 * 2:(c2 + 1) * 2, :], in_=wst[c2 % 2].v(), func=AF.Copy)
                else:
                    I('dve', 'tensor_copy', out=wo[:, c2 * 2:(c2 + 1) * 2, :], in_=wst[c2 % 2].v())
            g2 = [mk.sb(f"g2{s}", [128, D], F32) for s in range(2)]
            for s in range(2):
                load_mod(g2[s].v(), s, 2)
            xb = [mk.sb(f"xb{i}", [128, D], F32) for i in range(4)]
            mb = [mk.sb(f"mb{i}", [128, D], BF16) for i in range(4)]
            mT = [mk.sb(f"mT{i}", [128, 8, 128], BF16) for i in range(2)]
            tmp = mk.sb("tmp", [128, D], F32)
            pt = [mk.ps(f"pt{i}", [128, 1024], BF16) for i in range(2)]
            po = [mk.ps(f"po{i}", [128, 512], F32) for i in range(4)]
            def p7_front(tt):
                x_, m_, mT_, pt_ = xb[tt % 4], mb[tt % 4], mT[tt % 2], pt[tt % 2]
                mk.dma('sp', x_.v(), xsrc[tt * 128:(tt + 1) * 128, :])
                mk.dma('sp', m_.v(), mix[tt * 128:(tt + 1) * 128, :])
                for c in range(8):
                    I('pe', 'transpose', out=pt_[:, c * 128:(c + 1) * 128], in_=m_[:, c * 128:(c + 1) * 128], identity=identb.v())
                I('act', 'activation', out=mT_.v().rearrange("p c t -> p (c t)"), in_=pt_.v(), func=AF.Copy)

            def p7_back(tt):
                s = 0 if tt < 16 else 1
                x_, mT_ = xb[tt % 4], mT[tt % 2]
                for hf_ in range(2):
                    p = po[(tt % 2) * 2 + hf_]
                    for c in range(8):
                        I('pe', 'matmul', out=p.v(), lhsT=mT_[:, c, :], rhs=wo[:, c, hf_ * 512:(hf_ + 1) * 512], start=(c == 0), stop=(c == 7))
                    hs = slice(hf_ * 512, (hf_ + 1) * 512)
                    I('dve', 'tensor_tensor', out=tmp[:, hs], in0=p.v(), in1=g2[s][:, hs], op=ALU.mult)
                    I('dve', 'tensor_tensor', out=x_[:, hs], in0=x_[:, hs], in1=tmp[:, hs], op=ALU.add)
                mk.dma('sp', xs[tt * 128:(tt + 1) * 128, :], x_.v())

            for tt in range(n_tiles + 1):
                if tt < n_tiles:
                    p7_front(tt)
                if tt >= 1:
                    p7_back(tt - 1)
        if stop == f"p7_{l}":
            return done()

        ntok = n_tiles * 128
        with mk.phase():
            hTa = mk.sb("hTa", [128, 8, ntok], BF16)
            oacc = mk.sb("oacc", [128, n_tiles, D], F32)
            with mk.phase():
                gsb = [mk.sb(f"gsb{s}", [128, D], F32) for s in range(2)]
                shb = [mk.sb(f"shb{s}", [128, D], F32) for s in range(2)]
                for s in range(2):
                    load_mod(gsb[s].v(), s, 4)
                    load_mod(shb[s].v(), s, 3)
                wr = mk.sb("wr", [128, 8, 36], F32)
                brb = mk.sb("brb", [128, 36], F32)
                mk.dma('sp', wr.v(), moe_wr[l].rearrange("(c p) n -> p c n", p=128))
                mk.dma('sp', brb.v(), moe_br[l, :].pbc(128))
                xb = [mk.sb(f"xb{i}", [128, D], F32) for i in range(2)]
                junk = mk.sb("junk", [128, D], BF16)
                ssb = [mk.sb(f"ss{i}", [128, 1], F32) for i in range(2)]
                hf = [mk.sb(f"hf{i}", [128, D], F32) for i in range(2)]
                hTf = [mk.sb(f"hTf{i}", [128, 8, 128], F32) for i in range(2)]
                ptf = [mk.ps(f"ptf{i}", [128, 512], F32) for i in range(4)]
                pr = [mk.ps(f"pr{i}", [128, 36], F32) for i in range(2)]
                pct = [mk.ps(f"pct{i}", [32, 128], F32) for i in range(2)]
                combT = mk.sb("combT", [32, ntok], BF16)
                lgall = mk.sb("lgall", [128, n_tiles, 36], F32)
                cmball = mk.sb("cmball", [128, n_tiles, 32], F32)
                def p8_front(tt):
                    s = 0 if tt < 16 else 1
                    i2 = tt % 2
                    x_, ss_, hf_, hTf_ = xb[i2], ssb[i2], hf[i2], hTf[i2]
                    mk.dma('sp', x_.v(), xs[tt * 128:(tt + 1) * 128, :])
                    I('act', 'activation', out=junk.v(), in_=x_.v(), func=AF.Square, accum_out=ss_.v())
                    rstd_from_ss(ss_.v(), 1.0 / D, RMS_EPS, 1)
                    I('dve', 'scalar_tensor_tensor', out=hf_.v(), in0=x_.v(), scalar=ss_.v(), in1=gsb[s].v(), op0=ALU.mult, op1=ALU.mult)
                    I('pool', 'tensor_tensor', out=hf_.v(), in0=hf_.v(), in1=shb[s].v(), op=ALU.add)
                    for hh in range(2):
                        p = ptf[i2 * 2 + hh]
                        for c in range(4):
                            cc = hh * 4 + c
                            I('pe', 'transpose', out=p[:, c * 128:(c + 1) * 128], in_=hf_[:, cc * 128:(cc + 1) * 128], identity=identf.v())
                        I('act', 'activation', out=hTf_[:, hh * 4:(hh + 1) * 4, :].rearrange("p c t -> p (c t)"), in_=p.v(), func=AF.Copy)
                        I('dve', 'tensor_copy', out=hTa[:, hh * 4:(hh + 1) * 4, tt * 128:(tt + 1) * 128],
                          in_=hTf_[:, hh * 4:(hh + 1) * 4, :])
                def p8_back(tt):
                    i2 = tt % 2
                    hTf_ = hTf[i2]
                    pr_ = pr[i2]
                    for c in range(8):
                        I('pe', 'matmul', out=pr_.v(), lhsT=hTf_[:, c, :], rhs=wr[:, c, :], start=(c == 0), stop=(c == 7))
                    I('dve', 'tensor_tensor', out=lgall[:, tt, :], in0=pr_.v(), in1=brb.v(), op=ALU.add)
                for tt in range(n_tiles + 1):
                    if tt < n_tiles:
                        p8_front(tt)
                    if tt >= 1:
                        p8_back(tt - 1)
                routing_batched(mk, lgall.v(), n_tiles, cmball.v())
                for tt in range(n_tiles):
                    pc_ = pct[tt % 2]
                    I('pe', 'transpose', out=pc_.v(), in_=cmball[:, tt, :], identity=identf.v())
                    I('act', 'activation', out=combT[:, tt * 128:(tt + 1) * 128], in_=pc_.v(), func=AF.Copy)
                mk.dma('sp', cbd[:, 0:ntok], combT.v())
            with mk.phase():
                wgb = [mk.sb(f"wg{i}", [128, 2, 8, FH], BF16) for i in range(2)]
                wub = [mk.sb(f"wu{i}", [128, 2, 8, FH], BF16) for i in range(2)]
                wdb = [mk.sb(f"wd{i}", [128, 2, 2, D], BF16) for i in range(2)]
                actb = [mk.sb(f"act{i}", [128, 2, 2, 512], BF16) for i in range(2)]
                sgb = [mk.sb(f"sg{i}", [128, 512], F32) for i in range(2)]
                tb = [mk.sb(f"tb{i}", [128, 512], F32) for i in range(2)]
                pgp = [mk.ps(f"pgp{i}", [128, 512], F32) for i in range(2)]
                pup = [mk.ps(f"pup{i}", [128, 512], F32) for i in range(2)]
                cbb = [mk.sb(f"cbb{i}", [128, ntok], BF16) for i in range(3)]
                pdp = [mk.ps(f"pdp{i}", [128, 512], F32) for i in range(4)]
                groups = [(g0, min(512, ntok - g0)) for g0 in range(0, ntok, 512)]
                g5 = [mk.sb(f"g5{s}", [128, D], F32) for s in range(2)]
                for s in range(2):
                    load_mod(g5[s].v(), s, 5)
                xrb = [mk.sb(f"xr{i}", [128, D], F32) for i in range(2)]
                res_dst = out if (l == n_layers - 1 and not debug) or last else xs

                def res_load(tt):
                    if tt < n_tiles:
                        mk.dma('sp', xrb[tt % 2].v(), xs[tt * 128:(tt + 1) * 128, :])

                def res_finish(tt):
                    s = 0 if tt < 16 else 1
                    x_ = xrb[tt % 2]
                    I('dve', 'tensor_tensor', out=oacc[:, tt, :], in0=oacc[:, tt, :], in1=g5[s].v(), op=ALU.mult)
                    I('dve', 'tensor_tensor', out=x_.v(), in0=x_.v(), in1=oacc[:, tt, :], op=ALU.add)
                    if not (res_dst is out and tt >= 16):
                        mk.dma('sp', res_dst[tt * 128:(tt + 1) * 128, :], x_.v())
                kk = 0
                kd = 0
                gi = 0
                for pair in range(0 if stop == f"p8a_{l}" else NEXP // 2):
                    wg, wu, wd = wgb[pair % 2], wub[pair % 2], wdb[pair % 2]
                    for e in range(2):
                        ge = pair * 2 + e
                        mk.dma('sp', cbb[ge % 3].v(), cbd[ge, 0:ntok].pbc(128))
                        mk.dma('pool', wg[:, e, :, :], moe_wg[l, ge].rearrange("(c p) f -> p c f", p=128))
                        mk.dma('pool', wu[:, e, :, :], moe_wu[l, ge].rearrange("(c p) f -> p c f", p=128))
                        mk.dma('pool', wd[:, e, :, :], moe_wd[l, ge].rearrange("(c p) n -> p c n", p=128))
                    last_pair = (pair == NEXP // 2 - 1)
                    if last_pair:
                        res_load(0)
                    for (g0, gw) in groups:
                        at = actb[gi % 2]
                        gi += 1
                        for e in range(2):
                            ge = pair * 2 + e
                            for fc in range(2):
                                pg_, pu_, sg_, tb_ = pgp[kk % 2], pup[kk % 2], sgb[kk % 2], tb[kk % 2]
                                cb_ = cbb[ge % 3]
                                kk += 1
                                for c in range(8):
                                    I('pe', 'matmul', out=pg_[:, 0:gw], lhsT=wg[:, e, c, fc * 128:(fc + 1) * 128], rhs=hTa[:, c, g0:g0 + gw], start=(c == 0), stop=(c == 7))
                                for c in range(8):
                                    I('pe', 'matmul', out=pu_[:, 0:gw], lhsT=wu[:, e, c, fc * 128:(fc + 1) * 128], rhs=hTa[:, c, g0:g0 + gw], start=(c == 0), stop=(c == 7))
                                I('act', 'activation', out=sg_[:, 0:gw], in_=pg_[:, 0:gw], func=AF.Silu)
                                I('dve', 'tensor_tensor', out=tb_[:, 0:gw], in0=sg_[:, 0:gw], in1=pu_[:, 0:gw], op=ALU.mult)
                                I('dve', 'tensor_tensor', out=at[:, e, fc, 0:gw], in0=tb_[:, 0:gw], in1=cb_[:, g0:g0 + gw], op=ALU.mult)
                        for ti in range(gw // 128):
                            tt = g0 // 128 + ti
                            if last_pair:
                                res_load(tt + 1)
                            for hf_ in range(2):
                                pd_ = pdp[kd % 4]
                                kd += 1
                                hs = slice(hf_ * 512, (hf_ + 1) * 512)
                                n_ = 0
                                for e in range(2):
                                    for fc in range(2):
                                        I('pe', 'matmul', out=pd_.v(), lhsT=at[:, e, fc, ti * 128:(ti + 1) * 128], rhs=wd[:, e, fc, hs], start=(n_ == 0), stop=(n_ == 3))
                                        n_ += 1
                                if pair == 0:
                                    I('dve', 'tensor_copy', out=oacc[:, tt, hs], in_=pd_.v())
                                else:
                                    I('dve', 'tensor_tensor', out=oacc[:, tt, hs], in0=oacc[:, tt, hs], in1=pd_.v(), op=ALU.add)
                            if last_pair:
                                res_finish(tt)
        if stop in (f"p8_{l}", f"p8a_{l}"):
            return done()
    return done()


def make_in_maps(inputs):
    c = _consts()
    f32 = np.float32
    x = np.asarray(inputs['x'], f32)
    B = x.shape[0]
    ctx = np.asarray(inputs['ctx'], f32)
    cc = np.asarray(inputs['c'], f32)
    c_ctx = np.asarray(inputs['c_ctx'], f32)
    info = _na_tables()
    rpb = np.asarray(inputs['na_rpb'], f32)
    nab = np.empty((DEPTH, 16, 128, NH, 4, 128), f32)
    for b_, (r0, c0, qr0, qc0, mask, dr, dc) in enumerate(info):
        g = rpb[:, :, dr, dc]
        g = np.where(mask[None, None], g, f32(NEG_MASK))
        nab[:, b_] = g.transpose(0, 3, 1, 2, 4)
    nab = nab.reshape(DEPTH, 16, 128, NH * 4 * 128)
    fb = np.stack([np.asarray(inputs['hy_freq'], f32)[:, 0], np.asarray(inputs['hy_freq'], f32)[:, 1],
                   np.asarray(inputs['hy_b1'], f32), np.asarray(inputs['hy_b2'], f32)], axis=-1)
    shared = {
        'w_ada': np.asarray(inputs['w_ada'], f32), 'b_ada': np.asarray(inputs['b_ada'], f32),
        'g_mix': np.asarray(inputs['g_mix'], f32), 'g_ffn': np.asarray(inputs['g_ffn'], f32),
        'w_in': np.asarray(inputs['w_in'], f32), 'w_out': np.asarray(inputs['w_out'], f32),
        'gmlp_v_gain': np.asarray(inputs['gmlp_v_gain'], f32), 'gmlp_ws': np.asarray(inputs['gmlp_ws'], f32),
        'gmlp_bsT': np.ascontiguousarray(np.asarray(inputs['gmlp_bs'], f32).transpose(0, 2, 1)),
        'na_q_gain': np.asarray(inputs['na_q_gain'], f32), 'na_k_gain': np.asarray(inputs['na_k_gain'], f32),
        'na_bias': nab,
        'hy_short_w': np.asarray(inputs['hy_short_w'], f32), 'hy_short_b': np.asarray(inputs['hy_short_b'], f32),
        'hy_w1': np.asarray(inputs['hy_w1'], f32), 'hy_w2': np.asarray(inputs['hy_w2'], f32), 'hy_w3': np.asarray(inputs['hy_w3'], f32),
        'hy_fb': np.ascontiguousarray(fb), 'hy_bias': np.asarray(inputs['hy_bias'], f32).reshape(DEPTH, 512),
        'moe_wr': np.ascontiguousarray(np.concatenate([np.asarray(inputs['moe_w_rg'], f32), np.asarray(inputs['moe_w_re'], f32)], axis=-1)),
        'moe_br': np.ascontiguousarray(np.concatenate([np.asarray(inputs['moe_b_rg'], f32), np.asarray(inputs['moe_b_re'], f32)], axis=-1)),
        'moe_w_gate': np.asarray(inputs['moe_w_gate'], f32).reshape(DEPTH, NEXP, D, FH),
        'moe_w_up': np.asarray(inputs['moe_w_up'], f32).reshape(DEPTH, NEXP, D, FH),
        'moe_w_down': np.asarray(inputs['moe_w_down'], f32).reshape(DEPTH, NEXP, FH, D),
        'ident': c['ident'],
    }
    for L in (S, CTX):
        nT = L // 128
        shared[f'posT{L}'] = c[f'posT{L}']
        shared[f'decay{L}'] = c[f'decay{L}']
        for nm in ('Cf', 'Sf', 'Ci', 'Si'):
            shared[f'{nm}{L}'] = c[f'{nm}{L}'].reshape(nT, 128, nT * 128)
    maps = []
    for b in range(B):
        m = dict(shared)
        m['xin'] = np.ascontiguousarray(np.concatenate([x[b], ctx[b]], axis=0))
        cT = np.concatenate([cc[b].reshape(8, 128).T, c_ctx.reshape(8, 128).T], axis=1)
        m['cT'] = np.ascontiguousarray(cT)
        maps.append(m)
    return maps


_PROG = {}


def kernel(**inputs):
    if 'nc' not in _PROG:
        _PROG['nc'] = build_program()[0]
    maps = make_in_maps(inputs)
    res = run_bass_kernel_spmd(_PROG['nc'], maps, core_ids=list(range(len(maps))))
    return np.stack([np.asarray(r['out'], np.float32) for r in res.results], axis=0)
```
